# Optimizing a Trainium2 kernel written in Bass

```python
import math
import jax
import jax.numpy as jnp
from jax import lax
import numpy as np

D_MODEL = 2048
BATCH = 2
SEQ = 4096
DEPTH = 2

GRID_W = 64
CTX_LEN = 256

GLA_HEADS = 4
GLA_DK = 64
GLA_DV = 128
GLA_GATE_RANK = 16
GLA_GATE_NORM = 16.0
GDN_HEADS = 6
GDN_DK = 128
GDN_DV = 128
GDN_CONV = 5
DIFF_HEADS = 6
DIFF_DH = 64
DIFF_DV = 2 * DIFF_DH
DIFF_EPS = 1e-5
ROPE_THETA = 10000.0
D_MIX = GLA_HEADS * GLA_DV + GDN_HEADS * GDN_DV + DIFF_HEADS * DIFF_DV
CHUNK = 64
Q_BLOCK = 128
IN_SPLITS = (
    GLA_HEADS * GLA_DK, GLA_HEADS * GLA_DK, GLA_HEADS * GLA_DV, GLA_HEADS * GLA_DV, 2 * GLA_GATE_RANK,
    GDN_HEADS * GDN_DK, GDN_HEADS * GDN_DK, GDN_HEADS * GDN_DV, GDN_HEADS * GDN_DV, 2 * GDN_HEADS, 2 * GDN_HEADS,
    DIFF_HEADS * 2 * DIFF_DH, DIFF_HEADS * 2 * DIFF_DH, DIFF_HEADS * DIFF_DV,
)
D_IN_PROJ = sum(IN_SPLITS)
GDN_QKV = GDN_HEADS * (2 * GDN_DK + GDN_DV)
N_EXPERTS = 32
TOP_K = 4
D_EXPERT = D_MODEL
SWIGLU_ALPHA = 1.702
SWIGLU_LIMIT = 7.0
MOE_BLOCK = 128
NORM_EPS = 1e-6

kernel_name = "hybrid_gla_gdn_diffattn_moe_dit"

F32 = jnp.float32


def _rmsnorm(x, w, eps=NORM_EPS):
    xf = x.astype(F32)
    y = xf * lax.rsqrt(jnp.mean(xf * xf, axis=-1, keepdims=True) + eps)
    return (y * w.astype(F32)).astype(x.dtype)


def _l2norm(x, eps=1e-6):
    return x * lax.rsqrt(jnp.sum(x * x, axis=-1, keepdims=True) + eps)


def _centred_dwconv(x, w):
    pad = (GDN_CONV - 1) // 2
    return lax.conv_general_dilated(
        x, w[:, None, :].astype(x.dtype), window_strides=(1,), padding=[(pad, pad)],
        dimension_numbers=("NWC", "WIO", "NWC"), feature_group_count=x.shape[-1])


def _axial_rope(x):
    n = x.shape[1]
    rows = n // GRID_W
    row = jnp.repeat(jnp.arange(rows, dtype=F32), GRID_W)
    col = jnp.tile(jnp.arange(GRID_W, dtype=F32), rows)
    half = DIFF_DH // 2
    inv = 1.0 / (ROPE_THETA ** (jnp.arange(0, half, 2, dtype=F32) / half))

    def rot(xa, pos):
        ang = pos[:, None] * inv[None, :]
        cos = jnp.cos(ang)[None, :, None, None, :]
        sin = jnp.sin(ang)[None, :, None, None, :]
        x1, x2 = xa[..., : half // 2], xa[..., half // 2:]
        return jnp.concatenate([x1 * cos - x2 * sin, x2 * cos + x1 * sin], axis=-1)

    xf = x.astype(F32)
    return jnp.concatenate([rot(xf[..., :half], row), rot(xf[..., half:], col)], axis=-1)


def _gla_chunked(q, k, v, log_a, s0):
    B, H, T, dk = q.shape
    dv = v.shape[-1]
    n = T // CHUNK
    q = q.reshape(B, H, n, CHUNK, dk)
    k = k.reshape(B, H, n, CHUNK, dk)
    v = v.reshape(B, H, n, CHUNK, dv)
    b = jnp.cumsum(log_a.reshape(B, H, n, CHUNK, dk), axis=3)
    b_last = b[:, :, :, -1:, :]
    qd = q * jnp.exp(b)
    att = jnp.einsum("bhncd,bhnsd->bhncs", qd, k * jnp.exp(-b))
    att = jnp.where(jnp.tril(jnp.ones((CHUNK, CHUNK), bool)), att, 0.0)
    o_intra = jnp.einsum("bhncs,bhnsv->bhncv", att, v)
    u = jnp.einsum("bhncd,bhncv->bhndv", k * jnp.exp(b_last - b), v)
    decay = jnp.exp(b_last[:, :, :, 0, :])

    def step(S, xs):
        u_n, d_n = xs
        return d_n[..., None] * S + u_n, S

    s_fin, s_in = lax.scan(step, s0, (jnp.moveaxis(u, 2, 0), jnp.moveaxis(decay, 2, 0)))
    o_inter = jnp.einsum("bhncd,nbhdv->bhncv", qd, s_in)
    return (o_intra + o_inter).reshape(B, H, T, dv), s_fin


def _gdn_chunked(q, k, v, beta, g, s0):
    B, H, T, dk = q.shape
    dv = v.shape[-1]
    n = T // CHUNK
    q = q.reshape(B, H, n, CHUNK, dk)
    k = k.reshape(B, H, n, CHUNK, dk)
    v = v.reshape(B, H, n, CHUNK, dv)
    beta = beta.reshape(B, H, n, CHUNK)
    g = jnp.cumsum(g.reshape(B, H, n, CHUNK), axis=-1)
    incl = jnp.tril(jnp.ones((CHUNK, CHUNK), bool))
    strict = jnp.tril(jnp.ones((CHUNK, CHUNK), bool), -1)
    decay = jnp.exp(jnp.where(incl, g[..., :, None] - g[..., None, :], -jnp.inf))
    kb = k * beta[..., None]
    vb = v * beta[..., None]
    lower = jnp.where(strict, jnp.einsum("bhncd,bhnsd->bhncs", kb, k) * decay, 0.0)
    eye = jnp.eye(CHUNK, dtype=q.dtype)
    tmat = lax.linalg.triangular_solve(eye + lower, jnp.broadcast_to(eye, lower.shape),
                                       left_side=True, lower=True, unit_diagonal=True)
    u = tmat @ vb
    w = tmat @ (kb * jnp.exp(g)[..., None])
    a_qk = jnp.einsum("bhncd,bhnsd->bhncs", q, k) * decay
    qg = q * jnp.exp(g)[..., None]
    g_last = g[..., -1]
    kd = k * jnp.exp(g_last[..., None] - g)[..., None]

    def step(S, xs):
        w_n, u_n, qg_n, a_n, kd_n, gl_n = xs
        v_new = u_n - w_n @ S
        o = qg_n @ S + a_n @ v_new
        S = S * jnp.exp(gl_n)[..., None, None] + jnp.einsum("bhcd,bhcv->bhdv", kd_n, v_new)
        return S, o

    xs = tuple(jnp.moveaxis(t, 2, 0) for t in (w, u, qg, a_qk, kd, g_last))
    s_fin, o = lax.scan(step, s0, xs)
    return jnp.moveaxis(o, 0, 2).reshape(B, H, T, dv), s_fin


def _flip(args):
    return tuple(jnp.flip(a, axis=2) for a in args)


def _bidirectional(scan_fn, ctx_f, ctx_b, lat_f, lat_b, s0, need_ctx):
    o_cf, s_cf = scan_fn(*ctx_f, s0)
    o_cb, s_cb = scan_fn(*_flip(ctx_b), s0)
    o_lf, _ = scan_fn(*lat_f, s_cf)
    o_lb, _ = scan_fn(*_flip(lat_b), s_cb)
    o_lat = o_lf + jnp.flip(o_lb, axis=2)
    o_ctx = (o_cf + jnp.flip(o_cb, axis=2)) if need_ctx else None
    return o_lat, o_ctx


def _gated_head_norm(o, og, norm_w):
    B, H, T, dv = o.shape
    o = _rmsnorm(o.transpose(0, 2, 1, 3), norm_w)
    return (o * jax.nn.silu(og.astype(F32).reshape(B, T, H, dv))).reshape(B, T, H * dv)


def _gla_prep(parts, gate_w, gate_b):
    q, k, v, og, glr = parts
    B, T, _ = q.shape
    heads = lambda t, d: t.reshape(B, T, GLA_HEADS, d).transpose(0, 2, 1, 3).astype(F32)
    q = heads(q, GLA_DK) * GLA_DK ** -0.5
    k = heads(k, GLA_DK)
    v = heads(v, GLA_DV)
    glr = glr.reshape(B, T, 2, GLA_GATE_RANK).astype(F32)
    la = jax.nn.log_sigmoid(jnp.einsum("btzr,zrk->zbtk", glr, gate_w.astype(F32))
                            + gate_b.astype(F32)[:, None, None, :]) / GLA_GATE_NORM
    la = la.reshape(2, B, T, GLA_HEADS, GLA_DK).transpose(0, 1, 3, 2, 4)
    return (q, k, v, la[0]), (q, k, v, la[1]), og


def _gla_group(parts_lat, parts_ctx, gate_w, gate_b, norm_w, need_ctx):
    lf, lb, og_l = _gla_prep(parts_lat, gate_w, gate_b)
    cf, cb, og_c = _gla_prep(parts_ctx, gate_w, gate_b)
    s0 = jnp.zeros((og_l.shape[0], GLA_HEADS, GLA_DK, GLA_DV), F32)
    o_l, o_c = _bidirectional(_gla_chunked, cf, cb, lf, lb, s0, need_ctx)
    y_l = _gated_head_norm(o_l, og_l, norm_w)
    y_c = _gated_head_norm(o_c, og_c, norm_w) if need_ctx else None
    return y_l, y_c


def _gdn_prep(parts, conv_w, a_log, dt_bias):
    q, k, v, og, blr, alr = parts
    B, T, _ = q.shape
    qkv = jax.nn.silu(_centred_dwconv(jnp.concatenate([q, k, v], axis=-1), conv_w)).astype(F32)
    q, k, v = jnp.split(qkv, [GDN_HEADS * GDN_DK, 2 * GDN_HEADS * GDN_DK], axis=-1)
    heads = lambda t, d: t.reshape(B, T, GDN_HEADS, d).transpose(0, 2, 1, 3)
    q = _l2norm(heads(q, GDN_DK)) * GDN_DK ** -0.5
    k = _l2norm(heads(k, GDN_DK))
    v = heads(v, GDN_DV)
    beta = jax.nn.sigmoid(blr.astype(F32).reshape(B, T, 2, GDN_HEADS)).transpose(2, 0, 3, 1)
    g = (-jnp.exp(a_log.astype(F32))
         * jax.nn.softplus(alr.astype(F32).reshape(B, T, 2, GDN_HEADS) + dt_bias.astype(F32))
         ).transpose(2, 0, 3, 1)
    return (q, k, v, beta[0], g[0]), (q, k, v, beta[1], g[1]), og


def _gdn_group(parts_lat, parts_ctx, conv_w, a_log, dt_bias, norm_w, need_ctx):
    lf, lb, og_l = _gdn_prep(parts_lat, conv_w, a_log, dt_bias)
    cf, cb, og_c = _gdn_prep(parts_ctx, conv_w, a_log, dt_bias)
    s0 = jnp.zeros((og_l.shape[0], GDN_HEADS, GDN_DK, GDN_DV), F32)
    o_l, o_c = _bidirectional(_gdn_chunked, cf, cb, lf, lb, s0, need_ctx)
    y_l = _gated_head_norm(o_l, og_l, norm_w)
    y_c = _gated_head_norm(o_c, og_c, norm_w) if need_ctx else None
    return y_l, y_c


def _diff_attend(q, k, v, lam):
    s = jnp.einsum("bqhmd,bkhmd->bhmqk", q, k) * DIFF_DH ** -0.5
    p = jax.nn.softmax(s, axis=-1)
    w = p[:, :, 0] - lam * p[:, :, 1]
    return jnp.einsum("bhqk,bkhv->bqhv", w, v)


def _diff_group(parts_lat, parts_ctx, lam_vecs, norm_w, lambda_init, need_ctx):
    def heads(parts):
        q, k, v = parts
        B, T, _ = q.shape
        return (q.reshape(B, T, DIFF_HEADS, 2, DIFF_DH).astype(F32),
                k.reshape(B, T, DIFF_HEADS, 2, DIFF_DH).astype(F32),
                v.reshape(B, T, DIFF_HEADS, DIFF_DV).astype(F32))

    q_l, k_l, v_l = heads(parts_lat)
    q_c, k_c, v_c = heads(parts_ctx)
    B, S = q_l.shape[:2]
    q_l = _axial_rope(q_l)
    k_l = _axial_rope(k_l)
    lf = lam_vecs.astype(F32)
    lam = jnp.exp(jnp.sum(lf[0] * lf[1])) - jnp.exp(jnp.sum(lf[2] * lf[3])) + lambda_init
    k_all = jnp.concatenate([k_l, k_c], axis=1)
    v_all = jnp.concatenate([v_l, v_c], axis=1)
    nb = S // Q_BLOCK
    qb = jnp.moveaxis(q_l.reshape(B, nb, Q_BLOCK, DIFF_HEADS, 2, DIFF_DH), 1, 0)
    ob = lax.map(lambda blk: _diff_attend(blk, k_all, v_all, lam), qb)
    o_l = jnp.moveaxis(ob, 0, 1).reshape(B, S, DIFF_HEADS, DIFF_DV)
    post = lambda o: (_rmsnorm(o, norm_w, DIFF_EPS) * (1.0 - lambda_init)).reshape(o.shape[0], o.shape[1], -1)
    y_l = post(o_l)
    y_c = post(_diff_attend(q_c, k_c, v_c, lam)) if need_ctx else None
    return y_l, y_c


def _head_group_mixer(h_lat, h_ctx, w_in, gla_gate_w, gla_gate_b, gla_norm_w, gdn_conv_w,
                      gdn_a_log, gdn_dt_bias, gdn_norm_w, diff_lambda, diff_norm_w, w_out,
                      lambda_init, need_ctx):
    cuts = [int(v) for v in np.cumsum(IN_SPLITS)[:-1]]
    pl = jnp.split(h_lat @ w_in, cuts, axis=-1)
    pc = jnp.split(h_ctx @ w_in, cuts, axis=-1)
    a_l, a_c = _gla_group(pl[0:5], pc[0:5], gla_gate_w, gla_gate_b, gla_norm_w, need_ctx)
    b_l, b_c = _gdn_group(pl[5:11], pc[5:11], gdn_conv_w, gdn_a_log, gdn_dt_bias, gdn_norm_w, need_ctx)
    d_l, d_c = _diff_group(pl[11:14], pc[11:14], diff_lambda, diff_norm_w, lambda_init, need_ctx)
    out_l = jnp.concatenate([a_l, b_l, d_l], axis=-1).astype(h_lat.dtype) @ w_out
    out_c = (jnp.concatenate([a_c, b_c, d_c], axis=-1).astype(h_ctx.dtype) @ w_out) if need_ctx else None
    return out_l, out_c


def _moe(h, router_w, router_b, w1, b1, w2, b2):
    n_tok, d = h.shape
    logits = h.astype(F32) @ router_w.astype(F32) + router_b.astype(F32)
    top_logit, top_e = lax.top_k(logits, TOP_K)
    gate = jax.nn.softmax(top_logit, axis=-1)
    n_assign = n_tok * TOP_K
    flat_e = top_e.reshape(-1)
    flat_tok = jnp.arange(n_assign, dtype=jnp.int32) // TOP_K
    flat_gate = gate.reshape(-1)
    order = jnp.argsort(flat_e)
    sorted_e = flat_e[order]
    counts = jnp.bincount(flat_e, length=N_EXPERTS)
    padded = (counts + MOE_BLOCK - 1) // MOE_BLOCK * MOE_BLOCK
    start = jnp.cumsum(counts) - counts
    pad_end = jnp.cumsum(padded)
    pad_start = pad_end - padded
    dest = pad_start[sorted_e] + jnp.arange(n_assign, dtype=jnp.int32) - start[sorted_e]
    n_rows = -(-(n_assign + N_EXPERTS * (MOE_BLOCK - 1)) // MOE_BLOCK) * MOE_BLOCK
    n_blocks = n_rows // MOE_BLOCK
    row_tok = jnp.full((n_rows,), n_tok, jnp.int32).at[dest].set(flat_tok[order])
    row_gate = jnp.zeros((n_rows,), F32).at[dest].set(flat_gate[order])
    block_e = jnp.minimum(jnp.searchsorted(pad_end, jnp.arange(n_blocks) * MOE_BLOCK, side="right"),
                          N_EXPERTS - 1)
    h_pad = jnp.concatenate([h, jnp.zeros((1, d), h.dtype)], axis=0)
    xb = h_pad[row_tok].reshape(n_blocks, MOE_BLOCK, d)

    def expert_block(args):
        xe, e = args
        hid = xe @ w1[e] + b1[e]
        x_glu = jnp.minimum(hid[:, ::2], SWIGLU_LIMIT)
        x_lin = jnp.clip(hid[:, 1::2], -SWIGLU_LIMIT, SWIGLU_LIMIT)
        act = x_glu * jax.nn.sigmoid(SWIGLU_ALPHA * x_glu) * (x_lin + 1.0)
        return act @ w2[e] + b2[e]

    yb = lax.map(expert_block, (xb, block_e))
    y = yb.reshape(n_rows, d) * row_gate[:, None].astype(h.dtype)
    return jnp.zeros_like(h_pad).at[row_tok].add(y)[:n_tok]


def setup_inputs(seed: int = 0) -> dict:
    key = jax.random.key(seed)
    ks = jax.random.split(key, 32)
    D = D_MODEL
    nrm = lambda k, shape, s: jax.random.normal(k, shape, F32) * s
    dt = jnp.exp(jax.random.uniform(ks[13], (DEPTH, 2, GDN_HEADS), F32, math.log(1e-3), math.log(1e-1)))
    return {
        "x": nrm(ks[0], (BATCH, SEQ, D), 1.0),
        "c": nrm(ks[1], (BATCH, D), 1.0),
        "ctx": nrm(ks[2], (BATCH, CTX_LEN, D), 1.0),
        "c_ctx": nrm(ks[3], (D,), 1.0),
        "w_mod": nrm(ks[4], (DEPTH, D, 6 * D), 0.5 * D ** -0.5),
        "b_mod": nrm(ks[5], (DEPTH, 6 * D), 0.02),
        "norm1_w": 1.0 + nrm(ks[6], (DEPTH, D), 0.1),
        "w_in": nrm(ks[7], (DEPTH, D, D_IN_PROJ), D ** -0.5),
        "gla_gate_w": nrm(ks[8], (DEPTH, 2, GLA_GATE_RANK, GLA_HEADS * GLA_DK), GLA_GATE_RANK ** -0.5),
        "gla_gate_b": nrm(ks[9], (DEPTH, 2, GLA_HEADS * GLA_DK), 0.1),
        "gla_norm_w": 1.0 + nrm(ks[10], (DEPTH, GLA_DV), 0.1),
        "gdn_conv_w": nrm(ks[11], (DEPTH, GDN_CONV, GDN_QKV), GDN_CONV ** -0.5),
        "gdn_a_log": jnp.log(jax.random.uniform(ks[12], (DEPTH, 2, GDN_HEADS), F32, 1.0, 16.0)),
        "gdn_dt_bias": dt + jnp.log(-jnp.expm1(-dt)),
        "gdn_norm_w": 1.0 + nrm(ks[14], (DEPTH, GDN_DV), 0.1),
        "diff_lambda": nrm(ks[15], (DEPTH, 4, DIFF_DH), 0.1),
        "diff_norm_w": 1.0 + nrm(ks[16], (DEPTH, DIFF_DV), 0.1),
        "w_out": nrm(ks[17], (DEPTH, D_MIX, D), D_MIX ** -0.5),
        "norm2_w": 1.0 + nrm(ks[18], (DEPTH, D), 0.1),
        "router_w": nrm(ks[19], (DEPTH, D, N_EXPERTS), D ** -0.5),
        "router_b": nrm(ks[20], (DEPTH, N_EXPERTS), 0.01),
        "expert_w1": nrm(ks[21], (DEPTH, N_EXPERTS, D, 2 * D_EXPERT), D ** -0.5),
        "expert_b1": nrm(ks[22], (DEPTH, N_EXPERTS, 2 * D_EXPERT), 0.01),
        "expert_w2": nrm(ks[23], (DEPTH, N_EXPERTS, D_EXPERT, D), D_EXPERT ** -0.5),
        "expert_b2": nrm(ks[24], (DEPTH, N_EXPERTS, D), 0.01),
        "final_norm_w": 1.0 + nrm(ks[25], (D,), 0.1),
    }


def reference(x, c, ctx, c_ctx, w_mod, b_mod, norm1_w, w_in, gla_gate_w, gla_gate_b, gla_norm_w,
              gdn_conv_w, gdn_a_log, gdn_dt_bias, gdn_norm_w, diff_lambda, diff_norm_w, w_out,
              norm2_w, router_w, router_b, expert_w1, expert_b1, expert_w2, expert_b2, final_norm_w):
    B, S, D = x.shape
    L = ctx.shape[1]
    for l in range(DEPTH):
        last = l == DEPTH - 1
        lambda_init = 0.8 - 0.6 * math.exp(-0.3 * l)
        mod = jax.nn.silu(c) @ w_mod[l] + b_mod[l]
        mod_c = jax.nn.silu(c_ctx) @ w_mod[l] + b_mod[l]
        sh1, sc1, g1, sh2, sc2, g2 = jnp.split(mod, 6, axis=-1)
        sh1c, sc1c, g1c, sh2c, sc2c, g2c = jnp.split(mod_c, 6, axis=-1)
        h_lat = _rmsnorm(x, norm1_w[l]) * (1.0 + sc1[:, None]) + sh1[:, None]
        h_ctx = _rmsnorm(ctx, norm1_w[l]) * (1.0 + sc1c) + sh1c
        a_lat, a_ctx = _head_group_mixer(
            h_lat, h_ctx, w_in[l], gla_gate_w[l], gla_gate_b[l], gla_norm_w[l], gdn_conv_w[l],
            gdn_a_log[l], gdn_dt_bias[l], gdn_norm_w[l], diff_lambda[l], diff_norm_w[l], w_out[l],
            lambda_init, not last)
        x = x + g1[:, None] * a_lat
        h2 = _rmsnorm(x, norm2_w[l]) * (1.0 + sc2[:, None]) + sh2[:, None]
        if last:
            y = _moe(h2.reshape(B * S, D), router_w[l], router_b[l], expert_w1[l], expert_b1[l],
                     expert_w2[l], expert_b2[l]).reshape(B, S, D)
            x = x + g2[:, None] * y
        else:
            ctx = ctx + g1c * a_ctx
            h2c = _rmsnorm(ctx, norm2_w[l]) * (1.0 + sc2c) + sh2c
            tokens = jnp.concatenate([h2.reshape(B * S, D), h2c.reshape(B * L, D)], axis=0)
            y_all = _moe(tokens, router_w[l], router_b[l], expert_w1[l], expert_b1[l],
                         expert_w2[l], expert_b2[l])
            x = x + g2[:, None] * y_all[: B * S].reshape(B, S, D)
            ctx = ctx + g2c * y_all[B * S:].reshape(B, L, D)
    return _rmsnorm(x, final_norm_w)
```

```python
import contextlib
import math
import os
import time
import numpy as np
import concourse.bass as bass
import concourse.mybir as mybir
from concourse.bass_utils import run_bass_kernel_spmd

F32 = mybir.dt.float32
BF16 = mybir.dt.bfloat16
AF = mybir.ActivationFunctionType
ALU = mybir.AluOpType
AX = mybir.AxisListType

NCORES = 8


class Prog:
    ENG = ("pe", "act", "dve", "pool", "sp")

    def __init__(self):
        self.nc = bass.Bass("TRN2", target_bir_lowering=False)
        self.stack = contextlib.ExitStack()
        self.q = {e: [] for e in self.ENG}
        self.cnt = {e: 0 for e in self.ENG}
        self.pending = {e: False for e in self.ENG}
        self.esem = {e: self.stack.enter_context(self.nc.semaphore("s_" + e)) for e in self.ENG}
        self.dsem = {}
        self.dcnt = {}
        self.waited = {}
        self.lastw = {}
        self.readers = {}
        self.out_final = {}
        self.nbuf = 0

    def dram(self, name, shape, dt, kind):
        return self.nc.dram_tensor(name, list(shape), dt, kind=kind).ap()

    def sb(self, name, shape, dt=F32):
        return self.stack.enter_context(self.nc.sbuf_tensor("sb_" + name, list(shape), dt))

    def ps(self, name, shape, dt=F32):
        return self.stack.enter_context(self.nc.psum_tensor("ps_" + name, list(shape), dt))

    def _deps(self, eng, reads, writes):
        need = {}
        for b in reads:
            w = self.lastw.get(b)
            if w is not None:
                need[w[0]] = max(need.get(w[0], 0), w[1])
        for b in writes:
            w = self.lastw.get(b)
            if w is not None:
                need[w[0]] = max(need.get(w[0], 0), w[1])
            for r in self.readers.get(b, ()):
                need[r[0]] = max(need.get(r[0], 0), r[1])
        out = []
        for key, val in need.items():
            if key == ("e", eng) and val > self.cnt[eng]:
                continue
            if self.waited.get((eng, key), 0) >= val:
                continue
            self.waited[(eng, key)] = val
            sem = self.esem[key[1]] if key[0] == "e" else self.dsem[key[1]]
            out.append((sem, val))
        return out

    def _mark(self, tag, reads, writes):
        for b in reads:
            self.readers.setdefault(b, []).append(tag)
        for b in writes:
            self.lastw[b] = tag
            self.readers[b] = []

    def op(self, eng, fn, reads=(), writes=(), inc=True):
        waits = self._deps(eng, reads, writes)
        tag = (("e", eng), self.cnt[eng] + 1)
        self._mark(tag, reads, writes)
        sem = self.esem[eng]
        if inc:
            self.cnt[eng] += 1
            self.pending[eng] = False
        else:
            self.pending[eng] = True

        def thunk(e, fn=fn, waits=waits, inc=inc, sem=sem):
            for s, v in waits:
                e.wait_ge(s, v)
            ins = fn(e)
            if inc:
                ins.then_inc(sem, 1)
        self.q[eng].append(thunk)

    def dma(self, eng, out, in_, reads=(), writes=(), key=None, is_output=False, **kw):
        if key is None:
            key = (writes[0] if writes else reads[0])
        if key not in self.dsem:
            self.dsem[key] = self.stack.enter_context(self.nc.semaphore("d%d" % len(self.dsem)))
            self.dcnt[key] = 0
        waits = self._deps(eng, reads, writes)
        self.dcnt[key] += 16
        tag = (("d", key), self.dcnt[key])
        self._mark(tag, reads, writes)
        sem = self.dsem[key]
        if is_output:
            self.out_final[key] = self.dcnt[key]

        def thunk(e, waits=waits, sem=sem, out=out, in_=in_, kw=kw):
            for s, v in waits:
                e.wait_ge(s, v)
            e.dma_start(out=out, in_=in_, **kw).then_inc(sem, 16)
        self.q[eng].append(thunk)

    def finish(self):
        nc = self.nc
        finals = [(self.dsem[k], v) for k, v in self.out_final.items()]

        def fin(e, finals=finals):
            for s, v in finals:
                e.wait_ge(s, v)
        self.q["sp"].append(fin)
        with nc.Block() as block:
            @block.tensor
            def _(e):
                for t in self.q["pe"]:
                    t(e)

            @block.scalar
            def _(e):
                for t in self.q["act"]:
                    t(e)

            @block.vector
            def _(e):
                for t in self.q["dve"]:
                    t(e)

            @block.gpsimd
            def _(e):
                for t in self.q["pool"]:
                    t(e)

            @block.sync
            def _(e):
                for t in self.q["sp"]:
                    t(e)
        self.stack.close()
        return nc

    def mm(self, out, lhsT, rhs, start, stop, reads=(), writes=(), inc=None):
        if inc is None:
            inc = stop
        self.op("pe", lambda e: e.matmul(out, lhsT, rhs, start=start, stop=stop),
                reads=reads, writes=writes, inc=inc)


MOD_COLS = 12288 // NCORES


def build_p0():
    p = Prog()
    wm = p.dram("wm", [2, 2048, MOD_COLS], F32, "ExternalInput")
    cT = p.dram("cT", [128, 16, 3], F32, "ExternalInput")
    bm = p.dram("bm", [3, 2, MOD_COLS], F32, "ExternalInput")
    mo = p.dram("mo", [3, 2, MOD_COLS], F32, "ExternalOutput")
    ct = p.sb("ct", [128, 16, 3])
    sct = p.sb("sct", [128, 16, 3])
    bmt = p.sb("bmt", [3, 2, MOD_COLS])
    res = p.sb("res", [3, 2, MOD_COLS])
    wbuf = [p.sb("wb%d" % i, [128, 16, 512]) for i in range(2)]
    pst = [p.ps("ps%d" % i, [128, 512]) for i in range(2)]
    p.dma("sp", ct[:], cT, writes=["ct"])
    p.dma("sp", bmt[:], bm, writes=["bmt"])
    p.op("act", lambda e: e.activation(out=sct[:], in_=ct[:], func=AF.Silu), reads=["ct"], writes=["sct"])
    it = 0
    for l in range(2):
        for n in range(MOD_COLS // 512):
            wb = wbuf[it % 2]
            wn = "wb%d" % (it % 2)
            pt = pst[it % 2]
            pn = "ps%d" % (it % 2)
            src = wm[l, :, n * 512:(n + 1) * 512].rearrange("(kc p) j -> p kc j", p=128)
            p.dma("sp" if it % 2 == 0 else "pool", wb[:], src, writes=[wn])
            for kc in range(16):
                p.mm(pt[0:3, :], sct[:, kc, :], wb[:, kc, :], start=(kc == 0), stop=(kc == 15),
                     reads=["sct", wn], writes=[pn])
            p.op("dve", lambda e, pt=pt, l=l, n=n: e.tensor_tensor(
                out=res[:, l, n * 512:(n + 1) * 512], in0=pt[0:3, :], in1=bmt[:, l, n * 512:(n + 1) * 512],
                op=ALU.add), reads=[pn, "bmt"], writes=["res"])
            it += 1
    p.dma("sp", mo, res[:], reads=["res"], key="mo", is_output=True)
    return p.finish()


def run_p0(c, c_ctx, w_mod, b_mod):
    cv = np.concatenate([c, c_ctx[None, :]], axis=0).astype(np.float32)
    cT = np.ascontiguousarray(cv.T.reshape(16, 128, 3).transpose(1, 0, 2))
    in_maps = []
    for i in range(NCORES):
        sl = slice(i * MOD_COLS, (i + 1) * MOD_COLS)
        in_maps.append({
            "wm": np.ascontiguousarray(w_mod[:, :, sl]),
            "cT": cT,
            "bm": np.ascontiguousarray(np.broadcast_to(b_mod[None, :, sl], (3, 2, MOD_COLS))),
        })
    nc = build_p0()
    res = run_bass_kernel_spmd(nc, in_maps, core_ids=list(range(NCORES)))
    mod = np.concatenate([r["mo"] for r in res.results], axis=2)
    return mod


EPS = 1e-6


def row_tiles(RL, RC):
    tiles = []
    r = 0
    while r < RL:
        n = min(128, RL - r)
        tiles.append((r, n, 0))
        r += n
    while r < RL + RC:
        n = min(128, RL + RC - r)
        tiles.append((r, n, 1))
        r += n
    return tiles


def emit_norm_mod_T(p, xr, tiles, A, Bv, hT, pT, tagp):
    xt = [p.sb(tagp + "xt%d" % i, [128, 2048]) for i in range(2)]
    tmp = p.sb(tagp + "tmp", [128, 2048])
    sq = p.sb(tagp + "sq", [128, 2048], BF16)
    hb = [p.sb(tagp + "hb%d" % i, [128, 2048], BF16) for i in range(2)]
    ss = p.sb(tagp + "ss", [128, 2 * len(tiles)])
    ident = p.sb(tagp + "ident", [128, 128], BF16)
    identf = p.sb(tagp + "identf", [128, 128])
    p.op("pool", lambda e: e.memset(identf[:], 1.0), writes=[tagp + "identf"])
    p.op("pool", lambda e: e.affine_select(out=identf[:], in_=identf[:], pattern=[[-1, 128]],
                                           compare_op=ALU.is_equal, fill=0.0, base=0, channel_multiplier=1),
         reads=[tagp + "identf"], writes=[tagp + "identf"])
    p.op("pool", lambda e: e.tensor_copy(out=ident[:], in_=identf[:]), reads=[tagp + "identf"], writes=[tagp + "ident"])
    for ti, (r0, n, g) in enumerate(tiles):
        x_ = xt[ti % 2]
        xn = tagp + "xt%d" % (ti % 2)
        h_ = hb[ti % 2]
        hn = tagp + "hb%d" % (ti % 2)
        p.dma("sp", x_[0:n, :], xr[r0:r0 + n, :], writes=[xn])
        c0 = 2 * ti
        p.op("act", lambda e, x_=x_, n=n, c0=c0: e.activation(out=sq[0:n, :], in_=x_[0:n, :], func=AF.Square,
                                                            accum_out=ss[0:n, c0:c0 + 1]),
             reads=[xn], writes=[tagp + "sq", tagp + "ss"])
        p.op("act", lambda e, n=n, c0=c0: e.activation(out=ss[0:n, c0 + 1:c0 + 2], in_=ss[0:n, c0:c0 + 1],
                                                     func=AF.Sqrt, scale=1.0 / 2048.0, bias=epsb[0:n, :]),
             reads=[tagp + "ss", "epsb"], writes=[tagp + "ss"]) if False else None
        p.op("dve", lambda e, n=n, c0=c0: e.tensor_scalar(out=ss[0:n, c0 + 1:c0 + 2], in0=ss[0:n, c0:c0 + 1],
                                                        scalar1=1.0 / 2048.0, scalar2=EPS, op0=ALU.mult, op1=ALU.add),
             reads=[tagp + "ss"], writes=[tagp + "ss"])
        p.op("act", lambda e, n=n, c0=c0: e.activation(out=ss[0:n, c0:c0 + 1], in_=ss[0:n, c0 + 1:c0 + 2], func=AF.Sqrt),
             reads=[tagp + "ss"], writes=[tagp + "ss"])
        p.op("dve", lambda e, n=n, c0=c0: e.reciprocal(out=ss[0:n, c0 + 1:c0 + 2], in_=ss[0:n, c0:c0 + 1]),
             reads=[tagp + "ss"], writes=[tagp + "ss"])
        p.op("dve", lambda e, x_=x_, n=n, c0=c0, g=g: e.scalar_tensor_tensor(
            out=tmp[0:n, :], in0=x_[0:n, :], scalar=ss[0:n, c0 + 1:c0 + 2], in1=A[g][0:n, :],
            op0=ALU.mult, op1=ALU.mult), reads=[xn, tagp + "ss", "A%d" % g], writes=[tagp + "tmp"])
        p.op("dve", lambda e, h_=h_, n=n, g=g: e.tensor_tensor(out=h_[0:n, :], in0=tmp[0:n, :], in1=Bv[g][0:n, :], op=ALU.add),
             reads=[tagp + "tmp", "B%d" % g], writes=[hn])
        for k4 in range(4):
            pt = pT[(ti * 4 + k4) % 2]
            pn = tagp + "pT%d" % ((ti * 4 + k4) % 2)
            for kk in range(4):
                kc = k4 * 4 + kk
                p.op("pe", lambda e, pt=pt, kk=kk, h_=h_, n=n, kc=kc: e.transpose(
                    out=pt[:, kk, 0:n], in_=h_[0:n, kc * 128:(kc + 1) * 128], identity=ident[0:n, 0:n]),
                    reads=[hn, tagp + "ident"], writes=[pn], inc=(kk == 3))
            eng = "act" if k4 % 2 == 0 else "dve"
            if eng == "act":
                p.op("act", lambda e, pt=pt, k4=k4, r0=r0, n=n: e.copy(out=hT[:, k4 * 4:(k4 + 1) * 4, r0:r0 + n], in_=pt[:, :, 0:n]),
                     reads=[pn], writes=["hT"])
            else:
                p.op("dve", lambda e, pt=pt, k4=k4, r0=r0, n=n: e.tensor_copy(out=hT[:, k4 * 4:(k4 + 1) * 4, r0:r0 + n], in_=pt[:, :, 0:n]),
                     reads=[pn], writes=["hT"])


def load_mod_vectors(p, mvec, idx_w, idx_sc, idx_sh, names):
    nw = p.sb("nw", [128, 2048])
    p.dma("sp", nw[:], mvec[idx_w, :].partition_broadcast(128), writes=["nw"])
    A, Bv = [], []
    for g in range(len(idx_sc)):
        a = p.sb("A%d" % g, [128, 2048])
        b = p.sb("B%d" % g, [128, 2048])
        p.dma("sp", a[:], mvec[idx_sc[g], :].partition_broadcast(128), writes=["A%d" % g])
        p.dma("sp", b[:], mvec[idx_sh[g], :].partition_broadcast(128), writes=["B%d" % g])
        p.op("dve", lambda e, a=a: e.scalar_tensor_tensor(out=a[:], in0=a[:], scalar=1.0, in1=nw[:], op0=ALU.add, op1=ALU.mult),
             reads=["A%d" % g, "nw"], writes=["A%d" % g])
        A.append(a)
        Bv.append(b)
    return A, Bv


def build_p1(RL, RC, NCOL):
    p = Prog()
    R = RL + RC
    xr = p.dram("xr", [R, 2048], F32, "ExternalInput")
    mvec = p.dram("mvec", [5, 2048], F32, "ExternalInput")
    w = p.dram("w", [2048, NCOL], F32, "ExternalInput")
    proj = p.dram("proj", [R, NCOL], F32, "ExternalOutput")
    tiles = row_tiles(RL, RC)
    A, Bv = load_mod_vectors(p, mvec, 0, [1, 3], [2, 4], None)
    hT = p.sb("hT", [128, 16, R], BF16)
    pT = [p.ps("pT%d" % i, [128, 4, 128], BF16) for i in range(2)]
    emit_norm_mod_T(p, xr, tiles, A, Bv, hT, pT, "")
    wt = [p.sb("wt%d" % i, [128, 16, 512], BF16) for i in range(2)]
    nt = len(tiles)
    st = [p.sb("st%d" % i, [128, nt, 512]) for i in range(2)]
    pacc = [p.ps("pacc%d" % i, [128, 512]) for i in range(4)]
    ncol_t = (NCOL + 511) // 512
    ev = 0
    for j in range(ncol_t):
        c0 = j * 512
        cw = min(512, NCOL - c0)
        w_ = wt[j % 2]
        wn = "wt%d" % (j % 2)
        s_ = st[j % 2]
        sn = "st%d" % (j % 2)
        p.dma("pool", w_[:, :, 0:cw], w[:, c0:c0 + cw].rearrange("(kc p) c -> p kc c", p=128), writes=[wn])
        for ti, (r0, n, g) in enumerate(tiles):
            pa = pacc[ev % 4]
            pn = "pacc%d" % (ev % 4)
            for kc in range(16):
                p.mm(pa[0:n, 0:cw], hT[:, kc, r0:r0 + n], w_[:, kc, 0:cw], start=(kc == 0), stop=(kc == 15),
                     reads=["hT", wn], writes=[pn])
            if ev % 2 == 0:
                p.op("act", lambda e, pa=pa, s_=s_, ti=ti, n=n, cw=cw: e.copy(out=s_[0:n, ti, 0:cw], in_=pa[0:n, 0:cw]),
                     reads=[pn], writes=[sn])
            else:
                p.op("dve", lambda e, pa=pa, s_=s_, ti=ti, n=n, cw=cw: e.tensor_copy(out=s_[0:n, ti, 0:cw], in_=pa[0:n, 0:cw]),
                     reads=[pn], writes=[sn])
            ev += 1
        nfull = sum(1 for (_, n, _) in tiles if n == 128)
        full_ok = all(tiles[i][1] == 128 for i in range(nfull))
        assert full_ok
        if nfull:
            p.dma("sp", proj[0:nfull * 128, c0:c0 + cw].rearrange("(t p) c -> p t c", p=128), s_[:, 0:nfull, 0:cw],
                  reads=[sn], key=sn + "_o", is_output=True)
        for ti in range(nfull, nt):
            r0, n, g = tiles[ti]
            p.dma("sp", proj[r0:r0 + n, c0:c0 + cw], s_[0:n, ti, 0:cw], reads=[sn], key=sn + "_o", is_output=True)
    return p.finish()


def make_consts64(p):
    c = {}
    ones = p.sb("c_ones", [128, 128])
    p.op("pool", lambda e: e.memset(ones[:], 1.0), writes=["c_ones"])
    negones = p.sb("c_negones", [64, 64])
    p.op("pool", lambda e: e.memset(negones[:], -1.0), writes=["c_negones"])
    tri = p.sb("c_tri", [64, 64])
    p.op("pool", lambda e: e.affine_select(out=tri[:], in_=ones[0:64, 0:64], pattern=[[1, 64]], compare_op=ALU.is_ge,
                                           fill=0.0, base=0, channel_multiplier=-1), reads=["c_ones"], writes=["c_tri"])
    stri = p.sb("c_stri", [64, 64])
    p.op("pool", lambda e: e.affine_select(out=stri[:], in_=ones[0:64, 0:64], pattern=[[1, 64]], compare_op=ALU.is_gt,
                                           fill=0.0, base=0, channel_multiplier=-1), reads=["c_ones"], writes=["c_stri"])
    ident = p.sb("c_ident", [128, 128])
    p.op("pool", lambda e: e.affine_select(out=ident[:], in_=ones[:], pattern=[[-1, 128]], compare_op=ALU.is_equal,
                                           fill=0.0, base=0, channel_multiplier=1), reads=["c_ones"], writes=["c_ident"])
    lowI = p.sb("c_lowI", [64, 64])
    p.op("pool", lambda e: e.affine_select(out=lowI[:], in_=ones[0:64, 0:64], pattern=[[-1, 64]], compare_op=ALU.is_ge,
                                           fill=0.0, base=0, channel_multiplier=1), reads=["c_ones"], writes=["c_lowI"])
    lowS = p.sb("c_lowS", [64, 64])
    p.op("pool", lambda e: e.affine_select(out=lowS[:], in_=ones[0:64, 0:64], pattern=[[-1, 64]], compare_op=ALU.is_gt,
                                           fill=0.0, base=0, channel_multiplier=1), reads=["c_ones"], writes=["c_lowS"])
    c.update(ones=ones, negones=negones, tri=tri, stri=stri, ident=ident, lowI=lowI, lowS=lowS)
    c["names"] = ["c_ones", "c_negones", "c_tri", "c_stri", "c_ident", "c_lowI", "c_lowS"]
    return c


def build_gla(T):
    NCH = T // 64
    p = Prog()
    qT_d = p.dram("qT", [2, 64, T], F32, "ExternalInput")
    kT_d = p.dram("kT", [2, 64, T], F32, "ExternalInput")
    kt_d = p.dram("kt", [2, 64, NCH, 64], F32, "ExternalInput")
    vt_d = p.dram("vt", [2, 64, NCH, 128], F32, "ExternalInput")
    gl_d = p.dram("glrT", [2, 17, T], F32, "ExternalInput")
    gw_d = p.dram("gwb", [2, 17, 64], F32, "ExternalInput")
    o_d = p.dram("o", [2, 64, NCH, 128], F32, "ExternalOutput")
    C = make_consts64(p)
    cn = C["names"]
    qT = p.sb("qT", [64, T])
    kT = p.sb("kT", [64, T])
    kt = p.sb("kt", [64, NCH, 64])
    vt = p.sb("vt", [64, NCH, 128])
    glrT = p.sb("glrT", [17, T])
    gwb = p.sb("gwb", [17, 64])
    ost = p.sb("ost", [64, NCH, 128])
    S = [p.sb("S%d" % i, [64, 128]) for i in range(2)]
    NR = 3
    tmp = [p.sb("tm%d" % i, [64, 10, 64]) for i in range(NR)]
    bLA = p.ps("bLA", [128, 512])
    bBT = p.ps("bBT", [128, 512])
    bAT = p.ps("bAT", [128, 512])
    bO = [p.ps("bO%d" % i, [128, 512]) for i in range(2)]
    bU = [p.ps("bU%d" % i, [128, 512]) for i in range(2)]
    for z in range(2):
        p.dma("sp", qT[:], qT_d[z], writes=["qT"])
        p.dma("sp", kT[:], kT_d[z], writes=["kT"])
        p.dma("sp", kt[:], kt_d[z], writes=["kt"])
        p.dma("sp", vt[:], vt_d[z], writes=["vt"])
        p.dma("sp", glrT[:], gl_d[z], writes=["glrT"])
        p.dma("sp", gwb[:], gw_d[z], writes=["gwb"])
        p.op("dve", lambda e: e.memset(S[0][:], 0.0), writes=["S0"])
        for n in range(NCH):
            r = n % NR
            t_ = tmp[r]
            tn = lambda k, r=r: "tm%d_%s" % (r, k)
            _pn = {"la": "bLA", "BT": "bBT", "blT": "bBT", "Bm": "bBT", "att": "bAT"}
            an = lambda k: _pn[k]
            bn = lambda k, n=n: ("bO%d" % (n % 2)) if k == "o" else ("bU%d" % (n % 2))
            Sc, Sn_ = S[n % 2], S[(n + 1) % 2]
            Scn, Snn = "S%d" % (n % 2), "S%d" % ((n + 1) % 2)
            cs = slice(n * 64, (n + 1) * 64)
            e1, sp_, E1T, E2T, E3, qdT, kdT, kg, attT = [t_[:, i, :] for i in range(9)]
            dec = t_[:, 9, 0:1]
            la_ps, BT_ps, blT_ps, Bm_ps, att_ps = bLA[0:64, 0:64], bBT[0:64, 64:128], bBT[0:64, 128:130], bBT[0:64, 192:256], bAT[0:64, 256:320]
            o_ps, u_ps = bO[n % 2][0:64, 0:128], bU[n % 2][0:64, 128:256]
            p.mm(la_ps, glrT[:, cs], gwb[:], True, True, reads=["glrT", "gwb"], writes=[an("la")])
            p.op("act", lambda e, e1=e1, la_ps=la_ps: e.activation(out=e1, in_=la_ps, func=AF.Exp, scale=-1.0),
                 reads=[an("la")], writes=[tn("e1")])
            p.op("act", lambda e, e1=e1, sp_=sp_: e.activation(out=sp_, in_=e1, func=AF.Ln, bias=1.0),
                 reads=[tn("e1")], writes=[tn("sp")])
            p.mm(BT_ps, sp_, C["tri"][:], True, True, reads=[tn("sp"), "c_tri"], writes=[an("BT")])
            p.mm(blT_ps, sp_, C["ones"][0:64, 0:2], True, True, reads=[tn("sp"), "c_ones"], writes=[an("blT")])
            p.mm(Bm_ps, C["tri"][:], sp_, True, False, reads=[tn("sp"), "c_tri"], writes=[an("Bm")])
            p.mm(Bm_ps, C["negones"][:], sp_, False, True, reads=[tn("sp"), "c_negones"], writes=[an("Bm")])
            p.op("act", lambda e, E1T=E1T, BT_ps=BT_ps: e.activation(out=E1T, in_=BT_ps, func=AF.Exp, scale=-1.0 / 16),
                 reads=[an("BT")], writes=[tn("E1T")])
            p.op("act", lambda e, E2T=E2T, BT_ps=BT_ps: e.activation(out=E2T, in_=BT_ps, func=AF.Exp, scale=1.0 / 16),
                 reads=[an("BT")], writes=[tn("E2T")])
            p.op("act", lambda e, E3=E3, Bm_ps=Bm_ps: e.activation(out=E3, in_=Bm_ps, func=AF.Exp, scale=1.0 / 16),
                 reads=[an("Bm")], writes=[tn("E3")])
            p.op("act", lambda e, dec=dec, blT_ps=blT_ps: e.activation(out=dec, in_=blT_ps[:, 0:1], func=AF.Exp, scale=-1.0 / 16),
                 reads=[an("blT")], writes=[tn("dec")])
            p.op("dve", lambda e, qdT=qdT, E1T=E1T, cs=cs: e.scalar_tensor_tensor(
                out=qdT, in0=qT[:, cs], scalar=0.125, in1=E1T, op0=ALU.mult, op1=ALU.mult),
                reads=["qT", tn("E1T")], writes=[tn("qdT")])
            p.op("dve", lambda e, kdT=kdT, E2T=E2T, cs=cs: e.tensor_tensor(out=kdT, in0=kT[:, cs], in1=E2T, op=ALU.mult),
                 reads=["kT", tn("E2T")], writes=[tn("kdT")])
            p.op("dve", lambda e, kg=kg, E3=E3, n=n: e.tensor_tensor(out=kg, in0=kt[:, n, :], in1=E3, op=ALU.mult),
                 reads=["kt", tn("E3")], writes=[tn("kg")])
            p.mm(att_ps, kdT, qdT, True, True, reads=[tn("kdT"), tn("qdT")], writes=[an("att")])
            p.op("dve", lambda e, attT=attT, att_ps=att_ps: e.tensor_tensor(out=attT, in0=att_ps, in1=C["tri"][:], op=ALU.mult),
                 reads=[an("att"), "c_tri"], writes=[tn("attT")])
            p.mm(o_ps, attT, vt[:, n, :], True, False, reads=[tn("attT"), "vt"], writes=[bn("o")])
            p.mm(o_ps, qdT, Sc[:], False, True, reads=[tn("qdT"), Scn], writes=[bn("o")])
            p.op("act", lambda e, o_ps=o_ps, n=n: e.copy(out=ost[:, n, :], in_=o_ps), reads=[bn("o")], writes=["ost"])
            p.mm(u_ps, kg, vt[:, n, :], True, True, reads=[tn("kg"), "vt"], writes=[bn("u")])
            p.op("dve", lambda e, Sn_=Sn_, Sc=Sc, dec=dec, u_ps=u_ps: e.scalar_tensor_tensor(
                out=Sn_[:], in0=Sc[:], scalar=dec, in1=u_ps, op0=ALU.mult, op1=ALU.add),
                reads=[Scn, tn("dec"), bn("u")], writes=[Snn])
        p.dma("sp", o_d[z], ost[:], reads=["ost"], key="o_out", is_output=True)
    return p.finish()


def build_gdn(TC, TL, NI):
    T = TC + TL
    NCH = T // 64
    p = Prog()
    x_d = p.dram("x", [NI, 3, 128, T], F32, "ExternalInput")
    cw_d = p.dram("cw", [NI, 128, 3, 5], F32, "ExternalInput")
    ba_d = p.dram("ba", [NI, 64, 2, NCH], F32, "ExternalInput")
    sc_d = p.dram("sc", [NI, 64, 2], F32, "ExternalInput")
    o_d = p.dram("o", [NI, 64, NCH, 128], F32, "ExternalOutput")
    C = make_consts64(p)
    ones, negones, tri, ident, lowI, lowS = C["ones"], C["negones"], C["tri"], C["ident"], C["lowI"], C["lowS"]
    X = [p.sb("X%d" % i, [128, T]) for i in range(3)]
    Y = [p.sb("Y%d" % i, [128, T]) for i in range(3)]
    cw = p.sb("cw", [128, 3, 5])
    ba = p.sb("ba", [64, 2, NCH])
    sc = p.sb("sc", [64, 2])
    beta = p.sb("beta", [64, NCH])
    nbeta = p.sb("nbeta", [64, NCH])
    graw = p.sb("graw", [64, NCH + 1])
    gtmp = p.sb("gtmp", [64, NCH])
    ea = p.sb("ea", [64, 1])
    ost = p.sb("ost", [64, NCH, 128])
    S = [p.sb("S%d" % i, [128, 128]) for i in range(2)]
    NR = 2
    t64 = [p.sb("t64_%d" % i, [64, 1424]) for i in range(NR)]
    t128 = [p.sb("t128_%d" % i, [128, 200]) for i in range(NR)]
    bT = p.ps("bT", [128, 512])
    bG = p.ps("bG", [128, 512])
    bK = p.ps("bK", [128, 512])
    bX = p.ps("bX", [128, 512])
    bSq = p.ps("bSq", [128, 512])
    bW = p.ps("bW", [128, 512])
    bV = p.ps("bV", [128, 512])
    bO = p.ps("bO", [128, 512])
    segs = [(0, TC), (TC, T)]
    for i in range(NI):
        for j in range(3):
            p.dma("sp", X[j][:], x_d[i, j], writes=["X%d" % j])
        p.dma("sp", cw[:], cw_d[i], writes=["cw"])
        p.dma("sp", ba[:], ba_d[i], writes=["ba"])
        p.dma("sp", sc[:], sc_d[i], writes=["sc"])
        for j in range(3):
            xj, yj, xn, yn = X[j], Y[j], "X%d" % j, "Y%d" % j
            p.op("dve", lambda e, xj=xj, yj=yj, j=j: e.tensor_scalar(out=yj[:], in0=xj[:], scalar1=cw[:, j, 2:3], scalar2=None,
                                                                    op0=ALU.mult), reads=[xn, "cw"], writes=[yn])
            for k in (0, 1, 3, 4):
                sh = k - 2
                for (a, b) in segs:
                    d0, d1 = max(a, a - sh), min(b, b - sh)
                    if d1 <= d0:
                        continue
                    p.op("dve", lambda e, xj=xj, yj=yj, j=j, k=k, d0=d0, d1=d1, sh=sh: e.scalar_tensor_tensor(
                        out=yj[:, d0:d1], in0=xj[:, d0 + sh:d1 + sh], scalar=cw[:, j, k:k + 1], in1=yj[:, d0:d1],
                        op0=ALU.mult, op1=ALU.add), reads=[xn, yn, "cw"], writes=[yn])
            p.op("act", lambda e, yj=yj: e.activation(out=yj[:], in_=yj[:], func=AF.Silu), reads=[yn], writes=[yn])
        for j in range(2):
            yj, yn, sq, sqn = Y[j], "Y%d" % j, X[j], "X%d" % j
            scale = (128.0 ** -0.5) if j == 0 else 1.0
            p.op("act", lambda e, yj=yj, sq=sq: e.activation(out=sq[:], in_=yj[:], func=AF.Square), reads=[yn], writes=[sqn])
            for c0 in range(0, T, 512):
                c1 = min(T, c0 + 512)
                w_ = c1 - c0
                p.mm(bT[:, 0:w_], ones[:, :], sq[:, c0:c1], True, True, reads=["c_ones", sqn], writes=["bT"])
                p.op("dve", lambda e, sq=sq, c0=c0, c1=c1, w_=w_: e.tensor_scalar(out=sq[:, c0:c1], in0=bT[:, 0:w_], scalar1=1e-6,
                                                                                 scalar2=None, op0=ALU.add),
                     reads=["bT"], writes=[sqn])
            p.op("act", lambda e, sq=sq: e.activation(out=sq[:], in_=sq[:], func=AF.Sqrt), reads=[sqn], writes=[sqn])
            p.op("dve", lambda e, sq=sq: e.reciprocal(out=sq[:], in_=sq[:]), reads=[sqn], writes=[sqn])
            p.op("dve", lambda e, yj=yj, sq=sq, scale=scale: e.scalar_tensor_tensor(
                out=yj[:], in0=yj[:], scalar=scale, in1=sq[:], op0=ALU.mult, op1=ALU.mult), reads=[yn, sqn], writes=[yn])
        p.op("act", lambda e: e.activation(out=beta[:], in_=ba[:, 0, :], func=AF.Sigmoid), reads=["ba"], writes=["beta"])
        p.op("dve", lambda e: e.tensor_scalar(out=nbeta[:], in0=beta[:], scalar1=-1.0, scalar2=None, op0=ALU.mult),
             reads=["beta"], writes=["nbeta"])
        p.op("act", lambda e: e.activation(out=gtmp[:], in_=ba[:, 1, :], func=AF.Exp, bias=sc[:, 1:2]), reads=["ba", "sc"], writes=["gtmp"])
        p.op("act", lambda e: e.activation(out=gtmp[:], in_=gtmp[:], func=AF.Ln, bias=1.0), reads=["gtmp"], writes=["gtmp"])
        p.op("act", lambda e: e.activation(out=ea[:], in_=sc[:, 0:1], func=AF.Exp), reads=["sc"], writes=["ea"])
        p.op("dve", lambda e: e.memset(graw[:], 0.0), writes=["graw"])
        p.op("dve", lambda e: e.tensor_scalar(out=graw[:, 0:NCH], in0=gtmp[:], scalar1=ea[:, 0:1], scalar2=-1.0, op0=ALU.mult, op1=ALU.mult),
             reads=["gtmp", "ea"], writes=["graw"])
        p.op("dve", lambda e: e.memset(S[0][:], 0.0), writes=["S0"])
        for n in range(NCH):
            r = n % NR
            a_, b_ = t64[r], t128[r]
            tn = lambda k, r=r: "t%d_%s" % (r, k)
            W1, ngd, Dm0, Dm, Dms, aqk, AT = [a_[:, 64 * q:64 * (q + 1)] for q in range(7)]
            XY = [a_[:, 448:576], a_[:, 576:704]]
            PP = [a_[:, 704:768], a_[:, 768:832]]
            kbgn, kd, vb, vnew = a_[:, 832:960], a_[:, 960:1088], a_[:, 1088:1216], a_[:, 1216:1344]
            gcol_sb, eg, ekd = a_[:, 1344:1345], a_[:, 1345:1346], a_[:, 1346:1347]
            egrow, qgT, wTn, egl = b_[:, 0:64], b_[:, 64:128], b_[:, 128:192], b_[:, 192:193]
            cs = slice(n * 64, (n + 1) * 64)
            qTn, kTn, vTn = Y[0][:, cs], Y[1][:, cs], Y[2][:, cs]
            gr2 = graw[:, n:n + 2]
            Sc, Sn_ = S[n % 2], S[(n + 1) % 2]
            Scn, Snn = "S%d" % (n % 2), "S%d" % ((n + 1) % 2)
            p.op("pe", lambda e, kTn=kTn: e.transpose(out=bT[0:64, 0:128], in_=kTn, identity=ident[:, :]),
                 reads=["Y1", "c_ident"], writes=["bT"], inc=False)
            p.op("pe", lambda e, vTn=vTn: e.transpose(out=bT[0:64, 128:256], in_=vTn, identity=ident[:, :]),
                 reads=["Y2", "c_ident"], writes=["bT"])
            p.mm(bG[0:64, 0:2], tri[:], gr2, True, True, reads=["c_tri", "graw"], writes=["bG"], inc=False)
            p.mm(bG[0:64, 2:4], tri[:], gr2, True, False, reads=["c_tri", "graw"], writes=["bG"], inc=False)
            p.mm(bG[0:64, 2:4], negones[:], gr2, False, True, reads=["c_negones", "graw"], writes=["bG"], inc=False)
            p.mm(bG[0:128, 4:6], ones[0:64, 0:128], gr2, True, True, reads=["c_ones", "graw"], writes=["bG"], inc=False)
            p.op("dve", lambda e, W1=W1, n=n: e.tensor_scalar(out=W1, in0=tri[:], scalar1=graw[:, n:n + 1], scalar2=None, op0=ALU.mult),
                 reads=["c_tri", "graw"], writes=[tn("W1")])
            p.mm(bG[0:64, 64:128], ones[0:64, 0:64], W1, True, True, reads=["c_ones", tn("W1")], writes=["bG"], inc=False)
            p.mm(bG[0:128, 128:192], ones[0:64, 0:128], W1, True, True, reads=["c_ones", tn("W1")], writes=["bG"])
            p.op("act", lambda e, gcol_sb=gcol_sb: e.copy(out=gcol_sb, in_=bG[0:64, 0:1]), reads=["bG"], writes=[tn("gcol")])
            p.op("act", lambda e, eg=eg: e.activation(out=eg, in_=bG[0:64, 0:1], func=AF.Exp), reads=["bG"], writes=[tn("eg")])
            p.op("act", lambda e, ekd=ekd: e.activation(out=ekd, in_=bG[0:64, 2:3], func=AF.Exp, scale=-1.0), reads=["bG"], writes=[tn("ekd")])
            p.op("act", lambda e, egl=egl: e.activation(out=egl, in_=bG[0:128, 4:5], func=AF.Exp), reads=["bG"], writes=[tn("egl")])
            p.op("act", lambda e, egrow=egrow: e.activation(out=egrow, in_=bG[0:128, 128:192], func=AF.Exp), reads=["bG"], writes=[tn("egrow")])
            p.op("dve", lambda e, ngd=ngd, gcol_sb=gcol_sb: e.scalar_tensor_tensor(
                out=ngd, in0=bG[0:64, 64:128], scalar=gcol_sb, in1=lowI[:], op0=ALU.subtract, op1=ALU.mult),
                reads=["bG", tn("gcol"), "c_lowI"], writes=[tn("ngd")])
            p.op("act", lambda e, Dm0=Dm0, ngd=ngd: e.activation(out=Dm0, in_=ngd, func=AF.Exp, scale=-1.0), reads=[tn("ngd")], writes=[tn("Dm0")])
            p.op("dve", lambda e, Dm=Dm, Dm0=Dm0: e.tensor_tensor(out=Dm, in0=Dm0, in1=lowI[:], op=ALU.mult), reads=[tn("Dm0"), "c_lowI"], writes=[tn("Dm")])
            p.op("dve", lambda e, Dms=Dms, Dm0=Dm0: e.tensor_tensor(out=Dms, in0=Dm0, in1=lowS[:], op=ALU.mult), reads=[tn("Dm0"), "c_lowS"], writes=[tn("Dms")])
            p.op("dve", lambda e, kbgn=kbgn, eg=eg, n=n: e.tensor_scalar(out=kbgn, in0=bT[0:64, 0:128], scalar1=nbeta[:, n:n + 1], scalar2=eg,
                                                                    op0=ALU.mult, op1=ALU.mult), reads=["bT", "nbeta", tn("eg")], writes=[tn("kbgn")])
            p.op("dve", lambda e, kd=kd, ekd=ekd: e.tensor_scalar(out=kd, in0=bT[0:64, 0:128], scalar1=ekd, scalar2=None, op0=ALU.mult),
                 reads=["bT", tn("ekd")], writes=[tn("kd")])
            p.op("dve", lambda e, vb=vb, n=n: e.tensor_scalar(out=vb, in0=bT[0:64, 128:256], scalar1=beta[:, n:n + 1], scalar2=None, op0=ALU.mult),
                 reads=["bT", "beta"], writes=[tn("vb")])
            p.mm(bK[0:64, 0:64], kTn, kTn, True, True, reads=["Y1"], writes=["bK"], inc=False)
            p.mm(bK[0:64, 64:128], qTn, kTn, True, True, reads=["Y0", "Y1"], writes=["bK"])
            xy0 = XY[0]
            p.op("dve", lambda e, xy0=xy0, Dms=Dms, n=n: e.scalar_tensor_tensor(
                out=xy0[:, 64:128], in0=bK[0:64, 0:64], scalar=nbeta[:, n:n + 1], in1=Dms, op0=ALU.mult, op1=ALU.mult),
                reads=["bK", "nbeta", tn("Dms")], writes=[tn("XY0")])
            p.op("dve", lambda e, aqk=aqk, Dm=Dm: e.tensor_tensor(out=aqk, in0=bK[0:64, 64:128], in1=Dm, op=ALU.mult),
                 reads=["bK", tn("Dm")], writes=[tn("aqk")])
            p.op("pe", lambda e, xy0=xy0: e.transpose(out=bX[0:64, 0:64], in_=xy0[:, 64:128], identity=ident[0:64, 0:64]),
                 reads=[tn("XY0"), "c_ident"], writes=["bX"], inc=False)
            p.op("pe", lambda e, aqk=aqk: e.transpose(out=bX[0:64, 64:128], in_=aqk, identity=ident[0:64, 0:64]),
                 reads=[tn("aqk"), "c_ident"], writes=["bX"])
            p.op("act", lambda e, xy0=xy0: e.copy(out=xy0[:, 0:64], in_=bX[0:64, 0:64]), reads=["bX"], writes=[tn("XY0")])
            p.op("act", lambda e, AT=AT: e.copy(out=AT, in_=bX[0:64, 64:128]), reads=["bX"], writes=[tn("AT")])
            p.op("dve", lambda e, xy0=xy0, P0=PP[0]: e.tensor_tensor(out=P0, in0=xy0[:, 0:64], in1=ident[0:64, 0:64], op=ALU.add),
                 reads=[tn("XY0"), "c_ident"], writes=[tn("P0")])
            for j in range(5):
                xc, xn_ = XY[j % 2], XY[(j + 1) % 2]
                xcn, xnn = tn("XY%d" % (j % 2)), tn("XY%d" % ((j + 1) % 2))
                pc, pn_ = PP[j % 2], PP[(j + 1) % 2]
                pcn, pnn = tn("P%d" % (j % 2)), tn("P%d" % ((j + 1) % 2))
                if j < 4:
                    p.mm(bSq[0:64, 0:64], xc[:, 64:128], xc[:, 0:64], True, True, reads=[xcn], writes=["bSq"], inc=False)
                p.mm(bSq[0:64, 64:128], xc[:, 0:64], xc[:, 64:128], True, True, reads=[xcn], writes=["bSq"])
                lo = 0 if j < 4 else 64
                if j % 2 == 0:
                    p.op("act", lambda e, xn_=xn_, lo=lo: e.copy(out=xn_[:, lo:128], in_=bSq[0:64, lo:128]), reads=["bSq"], writes=[xnn])
                else:
                    p.op("dve", lambda e, xn_=xn_, lo=lo: e.tensor_copy(out=xn_[:, lo:128], in_=bSq[0:64, lo:128]), reads=["bSq"], writes=[xnn])
                p.mm(bSq[0:64, 128:192], ident[0:64, 0:64], pc, True, False, reads=["c_ident", pcn], writes=["bSq"], inc=False)
                p.mm(bSq[0:64, 128:192], xn_[:, 64:128], pc, False, True, reads=[xnn, pcn], writes=["bSq"])
                if j % 2 == 0:
                    p.op("dve", lambda e, pn_=pn_: e.tensor_copy(out=pn_, in_=bSq[0:64, 128:192]), reads=["bSq"], writes=[pnn])
                else:
                    p.op("act", lambda e, pn_=pn_: e.copy(out=pn_, in_=bSq[0:64, 128:192]), reads=["bSq"], writes=[pnn])
            TT, TTn = PP[1], tn("P1")
            p.mm(bW[0:128, 0:64], kbgn, TT, True, True, reads=[tn("kbgn"), TTn], writes=["bW"])
            p.op("act", lambda e, wTn=wTn: e.copy(out=wTn, in_=bW[0:128, 0:64]), reads=["bW"], writes=[tn("wTn")])
            p.mm(bV[0:64, 0:128], TT, vb, True, False, reads=[TTn, tn("vb")], writes=["bV"], inc=False)
            p.mm(bV[0:64, 0:128], wTn, Sc[:], False, True, reads=[tn("wTn"), Scn], writes=["bV"])
            p.op("dve", lambda e, vnew=vnew: e.tensor_copy(out=vnew, in_=bV[0:64, 0:128]), reads=["bV"], writes=[tn("vnew")])
            p.op("dve", lambda e, qgT=qgT, qTn=qTn, egrow=egrow: e.tensor_tensor(out=qgT, in0=qTn, in1=egrow, op=ALU.mult),
                 reads=["Y0", tn("egrow")], writes=[tn("qgT")])
            p.mm(bO[0:64, 0:128], qgT, Sc[:], True, False, reads=[tn("qgT"), Scn], writes=["bO"], inc=False)
            p.mm(bO[0:64, 0:128], AT, vnew, False, True, reads=[tn("AT"), tn("vnew")], writes=["bO"])
            p.op("act", lambda e, n=n: e.copy(out=ost[:, n, :], in_=bO[0:64, 0:128]), reads=["bO"], writes=["ost"])
            p.mm(bW[0:128, 64:192], kd, vnew, True, True, reads=[tn("kd"), tn("vnew")], writes=["bW"])
            p.op("dve", lambda e, Sn_=Sn_, Sc=Sc, egl=egl: e.scalar_tensor_tensor(
                out=Sn_[:], in0=Sc[:], scalar=egl, in1=bW[0:128, 64:192], op0=ALU.mult, op1=ALU.add),
                reads=[Scn, tn("egl"), "bW"], writes=[Snn])
        p.dma("sp", o_d[i], ost[:], reads=["ost"], key="o_out", is_output=True)
    return p.finish()


def build_diff(TQ, TL, TC, NI, need_ctx, lambda_init):
    TK = TC + TL
    NKT = TK // 128
    NKC = TC // 128
    p = Prog()
    q_d = p.dram("q", [NI, 2, 2, 64, TQ], F32, "ExternalInput")
    k_d = p.dram("k", [NI, 2, 2, 64, TL], F32, "ExternalInput")
    kc_d = p.dram("kc", [NI, 2, 64, TC], F32, "ExternalInput")
    qc_d = p.dram("qc", [NI, 2, 64, TC], F32, "ExternalInput")
    v_d = p.dram("v", [NI, 128, NKT, 128], F32, "ExternalInput")
    csq_d = p.dram("csq", [NI, 2, 64, TQ], F32, "ExternalInput")
    csk_d = p.dram("csk", [2, 64, TL], F32, "ExternalInput")
    dl_d = p.dram("dl", [1, 386], F32, "ExternalInput")
    y_d = p.dram("y", [NI, 128, TQ // 128, 128], F32, "ExternalOutput")
    yc_d = p.dram("yc", [NI, 128, max(NKC, 1), 128], F32, "ExternalOutput")
    stA = p.sb("stA", [64, TL])
    stB = p.sb("stB", [64, TL])
    csk = p.sb("csk", [64, 2, TL])
    csq = p.sb("csq", [64, 2, TQ])
    kall = [p.sb("kall%d" % m, [64, TK], BF16) for m in range(2)]
    qr = [p.sb("qr%d" % m, [64, TQ], BF16) for m in range(2)]
    qcb = [p.sb("qcb%d" % m, [64, TC], BF16) for m in range(2)]
    vst = p.sb("vst", [128, NKT, 128])
    vaug = p.sb("vaug", [128, NKT, 129], BF16)
    eT = [p.sb("eT%d" % i, [128, 512], BF16) for i in range(3)]
    osb = [p.sb("osb%d" % m, [128, 4, 129]) for m in range(2)]
    yst = p.sb("yst", [128, TQ // 128, 128])
    ycst = p.sb("ycst", [128, max(NKC, 1), 128])
    dl = p.sb("dl", [128, 386])
    sm = p.sb("sm", [128, 16])
    scr = p.sb("scr", [128, 128])
    osc = p.sb("osc", [128, 128])
    nwd = p.sb("nwd", [128, 128])
    bS = [p.ps("bS%d" % i, [128, 512]) for i in range(2)]
    bA = [p.ps("bA%d" % i, [128, 512]) for i in range(4)]
    p.dma("sp", dl[:], dl_d[0, :].partition_broadcast(128), writes=["dl"])
    p.dma("sp", csk[:], csk_d.rearrange("c d t -> d c t"), writes=["csk"])
    p.op("dve", lambda e: e.scalar_tensor_tensor(out=scr[:, 0:64], in0=dl[:, 0:64], scalar=1.0, in1=dl[:, 64:128],
                                                 op0=ALU.mult, op1=ALU.mult, accum_out=sm[:, 0:1]), reads=["dl"], writes=["scr", "sm"])
    p.op("dve", lambda e: e.scalar_tensor_tensor(out=scr[:, 0:64], in0=dl[:, 128:192], scalar=1.0, in1=dl[:, 192:256],
                                                 op0=ALU.mult, op1=ALU.mult, accum_out=sm[:, 1:2]), reads=["dl"], writes=["scr", "sm"])
    p.op("act", lambda e: e.activation(out=sm[:, 2:4], in_=sm[:, 0:2], func=AF.Exp), reads=["sm"], writes=["sm"])
    p.op("dve", lambda e: e.tensor_tensor(out=sm[:, 4:5], in0=sm[:, 3:4], in1=sm[:, 2:3], op=ALU.subtract), reads=["sm"], writes=["sm"])
    p.op("dve", lambda e: e.tensor_scalar(out=sm[:, 5:6], in0=sm[:, 4:5], scalar1=dl[:, 384:385], scalar2=None, op0=ALU.subtract),
         reads=["sm"], writes=["sm"])
    nlam = sm[:, 5:6]
    p.op("dve", lambda e: e.tensor_scalar(out=nwd[:], in0=dl[:, 256:384], scalar1=dl[:, 385:386], scalar2=None, op0=ALU.mult),
         reads=["dl"], writes=["nwd"])

    def rope(dst, dstn, src_ap, ncol, cs, csn, eng="dve"):
        p.dma("sp", stA[:, 0:ncol], src_ap[0], writes=["stA"])
        p.dma("sp", stB[:, 0:ncol], src_ap[1], writes=["stB"])
        p.op(eng, lambda e: e.tensor_tensor(out=stA[:, 0:ncol], in0=stA[:, 0:ncol], in1=cs[:, 0, 0:ncol], op=ALU.mult),
             reads=["stA", csn], writes=["stA"])
        p.op(eng, lambda e: e.tensor_tensor(out=stB[:, 0:ncol], in0=stB[:, 0:ncol], in1=cs[:, 1, 0:ncol], op=ALU.mult),
             reads=["stB", csn], writes=["stB"])
        p.op(eng, lambda e: e.tensor_tensor(out=dst, in0=stA[:, 0:ncol], in1=stB[:, 0:ncol], op=ALU.add),
             reads=["stA", "stB"], writes=[dstn])

    cnt = {"s": 0}

    def attend(qsrc, qn, nq, nkt, outt, outn, ob):
        nj = nq // 128
        for m in range(2):
            for kt in range(nkt):
                s = cnt["s"]
                cnt["s"] += 1
                bs, bsn = bS[s % 2], "bS%d" % (s % 2)
                et, etn = eT[s % 3], "eT%d" % (s % 3)
                p.mm(bs[:, 0:nq], kall[m][:, kt * 128:(kt + 1) * 128], qsrc[m], True, True,
                     reads=["kall%d" % m, qn[m]], writes=[bsn])
                p.op("act", lambda e, et=et, bs=bs: e.activation(out=et[:, 0:nq], in_=bs[:, 0:nq], func=AF.Exp, scale=0.125),
                     reads=[bsn], writes=[etn])
                for j in range(nj):
                    p.mm(bA[j][:, 0:129], et[:, j * 128:(j + 1) * 128], vaug[:, kt, :], kt == 0, kt == nkt - 1,
                         reads=[etn, "vaug"], writes=["bA%d" % j])
            for j in range(nj):
                p.op("dve", lambda e, m=m, j=j: e.tensor_copy(out=osb[m][:, j, :], in_=bA[j][:, 0:129]),
                     reads=["bA%d" % j], writes=["osb%d" % m])
        for j in range(nj):
            p.op("dve", lambda e, j=j: e.reciprocal(out=sm[:, 6:7], in_=osb[0][:, j, 128:129]), reads=["osb0"], writes=["sm"])
            p.op("dve", lambda e, j=j: e.reciprocal(out=sm[:, 7:8], in_=osb[1][:, j, 128:129]), reads=["osb1"], writes=["sm"])
            p.op("dve", lambda e: e.tensor_tensor(out=sm[:, 8:9], in0=sm[:, 7:8], in1=nlam, op=ALU.mult), reads=["sm"], writes=["sm"])
            p.op("dve", lambda e, j=j: e.tensor_scalar(out=osc[:], in0=osb[0][:, j, 0:128], scalar1=sm[:, 6:7], scalar2=None, op0=ALU.mult),
                 reads=["osb0", "sm"], writes=["osc"])
            p.op("dve", lambda e, j=j: e.scalar_tensor_tensor(out=osc[:], in0=osb[1][:, j, 0:128], scalar=sm[:, 8:9], in1=osc[:],
                                                              op0=ALU.mult, op1=ALU.add), reads=["osb1", "sm", "osc"], writes=["osc"])
            p.op("dve", lambda e: e.scalar_tensor_tensor(out=scr[:], in0=osc[:], scalar=1.0, in1=osc[:], op0=ALU.mult, op1=ALU.mult,
                                                         accum_out=sm[:, 9:10]), reads=["osc"], writes=["scr", "sm"])
            p.op("dve", lambda e: e.tensor_scalar(out=sm[:, 10:11], in0=sm[:, 9:10], scalar1=1.0 / 128.0, scalar2=1e-5, op0=ALU.mult, op1=ALU.add),
                 reads=["sm"], writes=["sm"])
            p.op("act", lambda e: e.activation(out=sm[:, 11:12], in_=sm[:, 10:11], func=AF.Sqrt), reads=["sm"], writes=["sm"])
            p.op("dve", lambda e: e.reciprocal(out=sm[:, 12:13], in_=sm[:, 11:12]), reads=["sm"], writes=["sm"])
            p.op("dve", lambda e, j=j: e.scalar_tensor_tensor(out=outt[:, ob + j, :], in0=osc[:], scalar=sm[:, 12:13], in1=nwd[:],
                                                              op0=ALU.mult, op1=ALU.mult), reads=["osc", "sm", "nwd"], writes=[outn])

    for i in range(NI):
        p.dma("sp", csq[:], csq_d[i].rearrange("c d t -> d c t"), writes=["csq"])
        p.dma("sp", vst[:], v_d[i], writes=["vst"])
        p.op("pool", lambda e: e.memset(vaug[:, :, 128:129], 1.0), writes=["vaug"])
        p.op("pool", lambda e: e.tensor_copy(out=vaug[:, :, 0:128], in_=vst[:]), reads=["vst"], writes=["vaug"])
        for m in range(2):
            rope(kall[m][:, TC:TK], "kall%d" % m, k_d[i, m], TL, csk, "csk")
            p.dma("sp", stA[:, 0:TC], kc_d[i, m], writes=["stA"])
            p.op("dve", lambda e, m=m: e.tensor_copy(out=kall[m][:, 0:TC], in_=stA[:, 0:TC]), reads=["stA"], writes=["kall%d" % m])
            rope(qr[m][:], "qr%d" % m, q_d[i, m], TQ, csq, "csq")
            if need_ctx:
                p.dma("sp", stA[:, 0:TC], qc_d[i, m], writes=["stA"])
                p.op("dve", lambda e, m=m: e.tensor_copy(out=qcb[m][:], in_=stA[:, 0:TC]), reads=["stA"], writes=["qcb%d" % m])
        QB = min(512, TQ)
        for qb in range(TQ // QB):
            attend([qr[m][:, qb * QB:(qb + 1) * QB] for m in range(2)], ["qr0", "qr1"], QB, NKT, yst, "yst", qb * (QB // 128))
        p.dma("sp", y_d[i], yst[:], reads=["yst"], key="y_out", is_output=True)
        if need_ctx:
            attend([qcb[m][:] for m in range(2)], ["qcb0", "qcb1"], TC, NKC, ycst, "ycst", 0)
            p.dma("sp", yc_d[i], ycst[:], reads=["ycst"], key="yc_out", is_output=True)
    return p.finish()


def build_p3(RL, RC):
    R = RL + RC
    p = Prog()
    xr = p.dram("xr", [R, 2048], F32, "ExternalInput")
    go_d = p.dram("go", [2, R, 512], F32, "ExternalInput")
    do_d = p.dram("do", [2, R, 768], F32, "ExternalInput")
    og_d = p.dram("og", [R, 1280], F32, "ExternalInput")
    df_d = p.dram("df", [R, 768], F32, "ExternalInput")
    mT_d = p.dram("mT", [128, 16, 5], F32, "ExternalInput")
    g1_d = p.dram("g1", [2, 2048], F32, "ExternalInput")
    hn_d = p.dram("hn", [1, 256], F32, "ExternalInput")
    wo_d = p.dram("wo", [2048, 2048], F32, "ExternalInput")
    rw_d = p.dram("rw", [2048, 32], F32, "ExternalInput")
    rb_d = p.dram("rb", [1, 32], F32, "ExternalInput")
    xn_d = p.dram("xn", [R, 2048], F32, "ExternalOutput")
    h2T_d = p.dram("h2T", [2048, R], BF16, "ExternalOutput")
    G_d = p.dram("G", [R, 32], F32, "ExternalOutput")
    tiles = row_tiles(RL, RC)
    wo = p.sb("wo", [128, 16, 2048], BF16)
    for j in range(4):
        p.dma("pool", wo[:, :, j * 512:(j + 1) * 512], wo_d[:, j * 512:(j + 1) * 512].rearrange("(kc p) c -> p kc c", p=128),
              writes=["wo"], key="wo%d" % j)
    ng = 2 if RC > 0 else 1
    g1b = [p.sb("g1b%d" % g, [128, 2048]) for g in range(ng)]
    for g in range(ng):
        p.dma("sp", g1b[g][:], g1_d[g, :].partition_broadcast(128), writes=["g1b%d" % g])
    hnb = p.sb("hnb", [128, 256])
    p.dma("sp", hnb[:], hn_d[0, :].partition_broadcast(128), writes=["hnb"])
    rbb = p.sb("rbb", [128, 32])
    p.dma("sp", rbb[:], rb_d[0, :].partition_broadcast(128), writes=["rbb"])
    rwt = p.sb("rwt", [128, 16, 32])
    p.dma("sp", rwt[:], rw_d.rearrange("(kc p) c -> p kc c", p=128), writes=["rwt"])
    mT = p.sb("mT", [128, 16, 5])
    p.dma("sp", mT[:], mT_d, writes=["mT"])
    AT = p.sb("AT2", [128, 2, 16])
    for g in range(ng):
        p.op("dve", lambda e, g=g: e.scalar_tensor_tensor(out=AT[:, g, :], in0=mT[:, :, 1 + 2 * g], scalar=1.0, in1=mT[:, :, 0],
                                                          op0=ALU.add, op1=ALU.mult), reads=["mT"], writes=["AT2"])
    identf = p.sb("identf", [128, 128])
    identb = p.sb("identb", [128, 128], BF16)
    onesf = p.sb("onesf", [128, 128])
    p.op("pool", lambda e: e.memset(onesf[:], 1.0), writes=["onesf"])
    p.op("pool", lambda e: e.affine_select(out=identf[:], in_=onesf[:], pattern=[[-1, 128]], compare_op=ALU.is_equal,
                                           fill=0.0, base=0, channel_multiplier=1), reads=["onesf"], writes=["identf"])
    p.op("pool", lambda e: e.tensor_copy(out=identb[:], in_=identf[:]), reads=["identf"], writes=["identb"])
    xt = [p.sb("xt%d" % i, [128, 2048]) for i in range(2)]
    xn = [p.sb("xn%d" % i, [128, 2048]) for i in range(2)]
    sqs = p.sb("sqs", [128, 2048], BF16)
    yn = p.sb("yn", [128, 2048])
    h2T = p.sb("h2T", [128, 16, 128])
    h2Tb = p.sb("h2Tb", [128, 16, 128], BF16)
    go = p.sb("go", [128, 2, 512])
    do = p.sb("do", [128, 2, 768])
    ogt = p.sb("ogt", [128, 1280])
    dft = p.sb("dft", [128, 768])
    osq = p.sb("osq", [128, 1280])
    ymix = p.sb("ymix", [128, 2048], BF16)
    ymT = p.sb("ymT", [128, 16, 128], BF16)
    sm = p.sb("sm", [128, 64])
    lg = p.sb("lg", [128, 32])
    ex = p.sb("ex", [128, 32])
    mk = p.sb("mk", [128, 32])
    Gt = p.sb("Gt", [128, 32])
    pT = [p.ps("pT%d" % i, [128, 4, 128], BF16) for i in range(2)]
    pacc = [p.ps("pacc%d" % i, [128, 512]) for i in range(2)]
    pF = [p.ps("pF%d" % i, [128, 4, 128]) for i in range(2)]
    pR = p.ps("pR", [128, 512])
    ev = 0
    for ti, (r0, n, g) in enumerate(tiles):
        x_, xnm = xt[ti % 2], "xt%d" % (ti % 2)
        xo, xon = xn[ti % 2], "xn%d" % (ti % 2)
        rs = slice(r0, r0 + n)
        p.dma("sp", x_[0:n, :], xr[rs, :], writes=[xnm])
        for z in range(2):
            p.dma("sp", go[0:n, z, :], go_d[z, rs, :], writes=["go"])
            p.dma("sp", do[0:n, z, :], do_d[z, rs, :], writes=["do"])
        p.dma("sp", ogt[0:n, :], og_d[rs, :], writes=["ogt"])
        p.dma("sp", dft[0:n, :], df_d[rs, :], writes=["dft"])
        p.op("act", lambda e, n=n: e.activation(out=ogt[0:n, :], in_=ogt[0:n, :], func=AF.Silu), reads=["ogt"], writes=["ogt"])
        p.op("dve", lambda e, n=n: e.tensor_tensor(out=go[0:n, 0, :], in0=go[0:n, 0, :], in1=go[0:n, 1, :], op=ALU.add), reads=["go"], writes=["go"])
        p.op("dve", lambda e, n=n: e.tensor_tensor(out=do[0:n, 0, :], in0=do[0:n, 0, :], in1=do[0:n, 1, :], op=ALU.add), reads=["do"], writes=["do"])
        p.op("dve", lambda e, n=n: e.tensor_tensor(out=osq[0:n, 0:512], in0=go[0:n, 0, :], in1=go[0:n, 0, :], op=ALU.mult), reads=["go"], writes=["osq"])
        p.op("dve", lambda e, n=n: e.tensor_tensor(out=osq[0:n, 512:1280], in0=do[0:n, 0, :], in1=do[0:n, 0, :], op=ALU.mult), reads=["do"], writes=["osq"])
        p.op("dve", lambda e, n=n: e.tensor_reduce(out=sm[0:n, 0:10], in_=osq[0:n, :].rearrange("p (h d) -> p h d", d=128), axis=AX.X, op=ALU.add),
             reads=["osq"], writes=["sm"])
        p.op("dve", lambda e, n=n: e.tensor_scalar(out=sm[0:n, 10:20], in0=sm[0:n, 0:10], scalar1=1.0 / 128.0, scalar2=EPS, op0=ALU.mult, op1=ALU.add),
             reads=["sm"], writes=["sm"])
        p.op("act", lambda e, n=n: e.activation(out=sm[0:n, 0:10], in_=sm[0:n, 10:20], func=AF.Sqrt), reads=["sm"], writes=["sm"])
        p.op("dve", lambda e, n=n: e.reciprocal(out=sm[0:n, 10:20], in_=sm[0:n, 0:10]), reads=["sm"], writes=["sm"])
        for h in range(10):
            src = go[0:n, 0, h * 128:(h + 1) * 128] if h < 4 else do[0:n, 0, (h - 4) * 128:(h - 3) * 128]
            nwc = hnb[0:n, 0:128] if h < 4 else hnb[0:n, 128:256]
            p.op("dve", lambda e, n=n, h=h, src=src, nwc=nwc: e.scalar_tensor_tensor(
                out=osq[0:n, h * 128:(h + 1) * 128], in0=src, scalar=sm[0:n, 10 + h:11 + h], in1=nwc, op0=ALU.mult, op1=ALU.mult),
                reads=["go", "do", "sm", "hnb"], writes=["osq"])
        p.op("dve", lambda e, n=n: e.tensor_tensor(out=ymix[0:n, 0:1280], in0=osq[0:n, :], in1=ogt[0:n, :], op=ALU.mult),
             reads=["osq", "ogt"], writes=["ymix"])
        p.op("pool", lambda e, n=n: e.tensor_copy(out=ymix[0:n, 1280:2048], in_=dft[0:n, :]), reads=["dft"], writes=["ymix"])
        for k4 in range(4):
            pt, pn = pT[k4 % 2], "pT%d" % (k4 % 2)
            for kk in range(4):
                kc = k4 * 4 + kk
                p.op("pe", lambda e, pt=pt, kk=kk, kc=kc, n=n: e.transpose(out=pt[:, kk, 0:n], in_=ymix[0:n, kc * 128:(kc + 1) * 128],
                                                                         identity=identb[0:n, 0:n]),
                     reads=["ymix", "identb"], writes=[pn], inc=(kk == 3))
            if k4 % 2 == 0:
                p.op("act", lambda e, pt=pt, k4=k4, n=n: e.copy(out=ymT[:, k4 * 4:(k4 + 1) * 4, 0:n], in_=pt[:, :, 0:n]), reads=[pn], writes=["ymT"])
            else:
                p.op("dve", lambda e, pt=pt, k4=k4, n=n: e.tensor_copy(out=ymT[:, k4 * 4:(k4 + 1) * 4, 0:n], in_=pt[:, :, 0:n]), reads=[pn], writes=["ymT"])
        for j in range(4):
            pa, pan = pacc[ev % 2], "pacc%d" % (ev % 2)
            ev += 1
            cs = slice(j * 512, (j + 1) * 512)
            for kc in range(16):
                p.mm(pa[0:n, :], ymT[:, kc, 0:n], wo[:, kc, cs], kc == 0, kc == 15, reads=["ymT", "wo"], writes=[pan])
            p.op("dve", lambda e, pa=pa, xo=xo, n=n, cs=cs, g=g: e.tensor_tensor(out=xo[0:n, cs], in0=pa[0:n, :], in1=g1b[g][0:n, cs], op=ALU.mult),
                 reads=[pan, "g1b%d" % g], writes=[xon])
            p.op("pool", lambda e, xo=xo, x_=x_, n=n, cs=cs: e.tensor_tensor(out=xo[0:n, cs], in0=xo[0:n, cs], in1=x_[0:n, cs], op=ALU.add),
                 reads=[xon, xnm], writes=[xon])
        p.dma("sp", xn_d[rs, :], xo[0:n, :], reads=[xon], key=xon + "_o", is_output=True)
        p.op("act", lambda e, xo=xo, n=n: e.activation(out=sqs[0:n, :], in_=xo[0:n, :], func=AF.Square, accum_out=sm[0:n, 20:21]),
             reads=[xon], writes=["sqs", "sm"])
        p.op("dve", lambda e, n=n: e.tensor_scalar(out=sm[0:n, 21:22], in0=sm[0:n, 20:21], scalar1=1.0 / 2048.0, scalar2=EPS, op0=ALU.mult, op1=ALU.add),
             reads=["sm"], writes=["sm"])
        p.op("act", lambda e, n=n: e.activation(out=sm[0:n, 22:23], in_=sm[0:n, 21:22], func=AF.Sqrt), reads=["sm"], writes=["sm"])
        p.op("dve", lambda e, n=n: e.reciprocal(out=sm[0:n, 23:24], in_=sm[0:n, 22:23]), reads=["sm"], writes=["sm"])
        p.op("dve", lambda e, xo=xo, n=n: e.tensor_scalar(out=yn[0:n, :], in0=xo[0:n, :], scalar1=sm[0:n, 23:24], scalar2=None, op0=ALU.mult),
             reads=[xon, "sm"], writes=["yn"])
        for k4 in range(4):
            pf, pfn = pF[k4 % 2], "pF%d" % (k4 % 2)
            for kk in range(4):
                kc = k4 * 4 + kk
                p.op("pe", lambda e, pf=pf, kk=kk, kc=kc, n=n: e.transpose(out=pf[:, kk, 0:n], in_=yn[0:n, kc * 128:(kc + 1) * 128],
                                                                         identity=identf[0:n, 0:n]),
                     reads=["yn", "identf"], writes=[pfn], inc=(kk == 3))
            for kk in range(4):
                kc = k4 * 4 + kk
                p.op("dve", lambda e, pf=pf, kk=kk, kc=kc, n=n, g=g: e.tensor_scalar(
                    out=h2T[:, kc, 0:n], in0=pf[:, kk, 0:n], scalar1=AT[:, g, kc:kc + 1], scalar2=mT[:, kc, 2 + 2 * g:3 + 2 * g],
                    op0=ALU.mult, op1=ALU.add), reads=[pfn, "AT2", "mT"], writes=["h2T"])
        p.op("act", lambda e, n=n: e.copy(out=h2Tb[:, :, 0:n], in_=h2T[:, :, 0:n]), reads=["h2T"], writes=["h2Tb"])
        p.dma("sp", h2T_d[:, rs].rearrange("(kc p) r -> p kc r", p=128), h2Tb[:, :, 0:n], reads=["h2Tb"], key="h2Tb_o", is_output=True)
        for kc in range(16):
            p.mm(pR[0:n, 0:32], h2T[:, kc, 0:n], rwt[:, kc, :], kc == 0, kc == 15, reads=["h2T", "rwt"], writes=["pR"])
        p.op("dve", lambda e, n=n: e.tensor_tensor(out=lg[0:n, :], in0=pR[0:n, 0:32], in1=rbb[0:n, :], op=ALU.add), reads=["pR", "rbb"], writes=["lg"])
        p.op("dve", lambda e, n=n: e.max(out=sm[0:n, 24:32], in_=lg[0:n, :]), reads=["lg"], writes=["sm"])
        p.op("dve", lambda e, n=n: e.tensor_scalar(out=sm[0:n, 32:33], in0=sm[0:n, 24:25], scalar1=-1.0, scalar2=None, op0=ALU.mult), reads=["sm"], writes=["sm"])
        p.op("dve", lambda e, n=n: e.tensor_scalar(out=mk[0:n, :], in0=lg[0:n, :], scalar1=sm[0:n, 27:28], scalar2=None, op0=ALU.is_ge),
             reads=["lg", "sm"], writes=["mk"])
        p.op("act", lambda e, n=n: e.activation(out=ex[0:n, :], in_=lg[0:n, :], func=AF.Exp, bias=sm[0:n, 32:33]), reads=["lg", "sm"], writes=["ex"])
        p.op("dve", lambda e, n=n: e.scalar_tensor_tensor(out=ex[0:n, :], in0=ex[0:n, :], scalar=1.0, in1=mk[0:n, :], op0=ALU.mult, op1=ALU.mult,
                                                          accum_out=sm[0:n, 33:34]), reads=["ex", "mk"], writes=["ex", "sm"])
        p.op("dve", lambda e, n=n: e.reciprocal(out=sm[0:n, 34:35], in_=sm[0:n, 33:34]), reads=["sm"], writes=["sm"])
        p.op("dve", lambda e, n=n: e.tensor_scalar(out=Gt[0:n, :], in0=ex[0:n, :], scalar1=sm[0:n, 34:35], scalar2=None, op0=ALU.mult),
             reads=["ex", "sm"], writes=["Gt"])
        p.dma("sp", G_d[rs, :], Gt[0:n, :], reads=["Gt"], key="Gt_o", is_output=True)
    return p.finish()


def build_p4(NT, TB=1024, NE=4, FA=2048):
    NJ = FA // 128
    p = Prog()
    h2T_d = p.dram("h2T", [2048, NT], BF16, "ExternalInput")
    Gl_d = p.dram("Gl", [NT, NE], F32, "ExternalInput")
    GlT_d = p.dram("GlT", [NE, NT], F32, "ExternalInput")
    w1_d = p.dram("w1", [NE, 2048, 2 * FA], F32, "ExternalInput")
    b1_d = p.dram("b1c", [128, NE, 2 * NJ], F32, "ExternalInput")
    w2_d = p.dram("w2", [NE, FA, 2048], F32, "ExternalInput")
    b2_d = p.dram("b2", [NE, 2048], F32, "ExternalInput")
    part_d = p.dram("part", [NT, 2048], F32, "ExternalOutput")
    blocks = [(t0, min(TB, NT - t0)) for t0 in range(0, NT, TB)]
    hT = p.sb("hT", [128, 16, TB], BF16)
    actT = p.sb("actT", [128, NJ, TB], BF16)
    acc = p.sb("acc", [128, TB // 128, 2048])
    wt1 = [p.sb("wt1_%d" % i, [128, 16, 256], BF16) for i in range(2)]
    wt2 = [p.sb("wt2_%d" % i, [128, NJ, 256], BF16) for i in range(2)]
    NRT = 2
    tg = [p.sb("tg%d" % i, [128, 512]) for i in range(NRT)]
    tl = [p.sb("tl%d" % i, [128, 512]) for i in range(NRT)]
    ts_ = [p.sb("ts%d" % i, [128, 512], BF16) for i in range(NRT)]
    tt_ = [p.sb("tt%d" % i, [128, 512], BF16) for i in range(NRT)]
    glt = p.sb("glt", [128, TB // 128, NE])
    glT = p.sb("glT", [NE, TB])
    b1c = p.sb("b1c", [128, NE, 2 * NJ])
    b2s = p.sb("b2s", [NE, 2048])
    pG = [p.ps("pG%d" % i, [128, 512]) for i in range(2)]
    pL = [p.ps("pL%d" % i, [128, 512]) for i in range(2)]
    pY = [p.ps("pY%d" % i, [128, 512]) for i in range(2)]
    pB = p.ps("pB", [128, 512])
    p.dma("sp", b1c[:], b1_d, writes=["b1c"])
    p.dma("sp", b2s[:], b2_d, writes=["b2s"])
    p.op("dve", lambda e: e.tensor_scalar(out=b1c[:, :, 1::2], in0=b1c[:, :, 1::2], scalar1=1.0, scalar2=None, op0=ALU.add),
         reads=["b1c"], writes=["b1c"])
    w1i = 0
    w2i = 0
    ev = 0
    ri = 0
    for (t0, TBn) in blocks:
        ntile = TBn // 128
        nN = (TBn + 511) // 512
        p.dma("sp", hT[:, :, 0:TBn], h2T_d[:, t0:t0 + TBn].rearrange("(kc p) t -> p kc t", p=128), writes=["hT"])
        p.dma("sp", glt[:, 0:ntile, :], Gl_d[t0:t0 + TBn, :].rearrange("(t p) e -> p t e", p=128), writes=["glt"],
              allow_slow_non_contiguous=True)
        p.dma("sp", glT[:, 0:TBn], GlT_d[:, t0:t0 + TBn], writes=["glT"])
        for tt in range(ntile):
            for dt in range(4):
                p.mm(pB[:, :], glT[:, tt * 128:(tt + 1) * 128], b2s[:, dt * 512:(dt + 1) * 512], True, True, reads=["glT", "b2s"], writes=["pB"])
                if (tt * 4 + dt) % 2 == 0:
                    p.op("act", lambda e, tt=tt, dt=dt: e.copy(out=acc[:, tt, dt * 512:(dt + 1) * 512], in_=pB[:, :]), reads=["pB"], writes=["acc"])
                else:
                    p.op("dve", lambda e, tt=tt, dt=dt: e.tensor_copy(out=acc[:, tt, dt * 512:(dt + 1) * 512], in_=pB[:, :]), reads=["pB"], writes=["acc"])
        for ex in range(NE):
            for j in range(NJ):
                w_, wn = wt1[w1i % 2], "wt1_%d" % (w1i % 2)
                w1i += 1
                p.dma("pool", w_[:], w1_d[ex][:, j * 256:(j + 1) * 256].rearrange("(kc p) c -> p kc c", p=128), writes=[wn])
                for nt in range(nN):
                    c0 = nt * 512
                    cw = min(512, TBn - c0)
                    k = ev % 2
                    ev += 1
                    g_ps, gn = pG[k], "pG%d" % k
                    l_ps, ln = pL[k], "pL%d" % k
                    for kc in range(16):
                        p.mm(g_ps[:, 0:cw], w_[:, kc, 0:128], hT[:, kc, c0:c0 + cw], kc == 0, kc == 15, reads=[wn, "hT"], writes=[gn])
                    for kc in range(16):
                        p.mm(l_ps[:, 0:cw], w_[:, kc, 128:256], hT[:, kc, c0:c0 + cw], kc == 0, kc == 15, reads=[wn, "hT"], writes=[ln])
                    r = ri % NRT
                    ri += 1
                    g_, l_, s_, t_ = tg[r], tl[r], ts_[r], tt_[r]
                    p.op("dve", lambda e, g_=g_, g_ps=g_ps, ex=ex, j=j, cw=cw: e.tensor_scalar(
                        out=g_[:, 0:cw], in0=g_ps[:, 0:cw], scalar1=b1c[:, ex, 2 * j:2 * j + 1], scalar2=7.0, op0=ALU.add, op1=ALU.min),
                        reads=[gn, "b1c"], writes=["tg%d" % r])
                    p.op("act", lambda e, g_=g_, s_=s_, cw=cw: e.activation(out=s_[:, 0:cw], in_=g_[:, 0:cw], func=AF.Sigmoid, scale=1.702),
                         reads=["tg%d" % r], writes=["ts%d" % r])
                    p.op("dve", lambda e, l_=l_, l_ps=l_ps, ex=ex, j=j, cw=cw: e.tensor_scalar(
                        out=l_[:, 0:cw], in0=l_ps[:, 0:cw], scalar1=b1c[:, ex, 2 * j + 1:2 * j + 2], scalar2=8.0, op0=ALU.add, op1=ALU.min),
                        reads=[ln, "b1c"], writes=["tl%d" % r])
                    p.op("pool", lambda e, g_=g_, s_=s_, t_=t_, cw=cw: e.tensor_tensor(out=t_[:, 0:cw], in0=g_[:, 0:cw], in1=s_[:, 0:cw], op=ALU.mult),
                         reads=["tg%d" % r, "ts%d" % r], writes=["tt%d" % r])
                    p.op("dve", lambda e, l_=l_, t_=t_, j=j, c0=c0, cw=cw: e.scalar_tensor_tensor(
                        out=actT[:, j, c0:c0 + cw], in0=l_[:, 0:cw], scalar=-6.0, in1=t_[:, 0:cw], op0=ALU.max, op1=ALU.mult),
                        reads=["tl%d" % r, "tt%d" % r], writes=["actT"])
            for dt in range(8):
                w_, wn = wt2[w2i % 2], "wt2_%d" % (w2i % 2)
                w2i += 1
                p.dma("pool", w_[:], w2_d[ex][:, dt * 256:(dt + 1) * 256].rearrange("(fc p) c -> p fc c", p=128), writes=[wn])
                for tt in range(ntile):
                    k = ev % 2
                    ev += 1
                    y_ps, yn_ = pY[k], "pY%d" % k
                    for fc in range(NJ):
                        p.mm(y_ps[:, 0:256], actT[:, fc, tt * 128:(tt + 1) * 128], w_[:, fc, :], fc == 0, fc == NJ - 1,
                             reads=["actT", wn], writes=[yn_])
                    p.op("dve", lambda e, y_ps=y_ps, tt=tt, dt=dt, ex=ex: e.scalar_tensor_tensor(
                        out=acc[:, tt, dt * 256:(dt + 1) * 256], in0=y_ps[:, 0:256], scalar=glt[:, tt, ex:ex + 1],
                        in1=acc[:, tt, dt * 256:(dt + 1) * 256], op0=ALU.mult, op1=ALU.add),
                        reads=[yn_, "glt", "acc"], writes=["acc"])
        p.dma("sp", part_d[t0:t0 + TBn, :].rearrange("(t p) d -> p t d", p=128), acc[:, 0:ntile, :], reads=["acc"], key="acc_o", is_output=True)
    return p.finish()


def build_p5(RL, RC, final):
    R = RL + RC
    p = Prog()
    xn_d = p.dram("xn", [R, 2048], F32, "ExternalInput")
    pt_d = p.dram("parts", [NCORES, R, 2048], F32, "ExternalInput")
    g2_d = p.dram("g2", [2, 2048], F32, "ExternalInput")
    fw_d = p.dram("fw", [1, 2048], F32, "ExternalInput")
    xo_d = p.dram("xo", [R, 2048], F32, "ExternalOutput")
    tiles = row_tiles(RL, RC)
    ng = 2 if RC > 0 else 1
    g2b = [p.sb("g2b%d" % g, [128, 2048]) for g in range(ng)]
    for g in range(ng):
        p.dma("sp", g2b[g][:], g2_d[g, :].partition_broadcast(128), writes=["g2b%d" % g])
    fwb = p.sb("fwb", [128, 2048])
    p.dma("sp", fwb[:], fw_d[0, :].partition_broadcast(128), writes=["fwb"])
    xt = [p.sb("xt%d" % i, [128, 2048]) for i in range(2)]
    acc = [p.sb("acc%d" % i, [128, 2048]) for i in range(2)]
    pb = [p.sb("pb%d" % i, [128, 2048]) for i in range(4)]
    sqs = p.sb("sqs", [128, 2048], BF16)
    sm = p.sb("sm", [128, 8])
    k = 0
    for ti, (r0, n, g) in enumerate(tiles):
        rs = slice(r0, r0 + n)
        x_, xnm = xt[ti % 2], "xt%d" % (ti % 2)
        a_, an = acc[ti % 2], "acc%d" % (ti % 2)
        p.dma("sp", x_[0:n, :], xn_d[rs, :], writes=[xnm])
        p.dma("sp", a_[0:n, :], pt_d[0, rs, :], writes=[an])
        for c in range(1, NCORES):
            b_, bn = pb[k % 4], "pb%d" % (k % 4)
            k += 1
            p.dma("sp", b_[0:n, :], pt_d[c, rs, :], writes=[bn])
            eng = "dve" if c % 2 == 1 else "pool"
            p.op(eng, lambda e, a_=a_, b_=b_, n=n: e.tensor_tensor(out=a_[0:n, :], in0=a_[0:n, :], in1=b_[0:n, :], op=ALU.add),
                 reads=[an, bn], writes=[an])
        p.op("dve", lambda e, a_=a_, n=n, g=g: e.tensor_tensor(out=a_[0:n, :], in0=a_[0:n, :], in1=g2b[g][0:n, :], op=ALU.mult),
             reads=[an, "g2b%d" % g], writes=[an])
        p.op("pool", lambda e, a_=a_, x_=x_, n=n: e.tensor_tensor(out=a_[0:n, :], in0=a_[0:n, :], in1=x_[0:n, :], op=ALU.add),
             reads=[an, xnm], writes=[an])
        if final:
            p.op("act", lambda e, a_=a_, n=n: e.activation(out=sqs[0:n, :], in_=a_[0:n, :], func=AF.Square, accum_out=sm[0:n, 0:1]),
                 reads=[an], writes=["sqs", "sm"])
            p.op("dve", lambda e, n=n: e.tensor_scalar(out=sm[0:n, 1:2], in0=sm[0:n, 0:1], scalar1=1.0 / 2048.0, scalar2=EPS, op0=ALU.mult, op1=ALU.add),
                 reads=["sm"], writes=["sm"])
            p.op("act", lambda e, n=n: e.activation(out=sm[0:n, 2:3], in_=sm[0:n, 1:2], func=AF.Sqrt), reads=["sm"], writes=["sm"])
            p.op("dve", lambda e, n=n: e.reciprocal(out=sm[0:n, 3:4], in_=sm[0:n, 2:3]), reads=["sm"], writes=["sm"])
            p.op("dve", lambda e, a_=a_, n=n: e.scalar_tensor_tensor(out=a_[0:n, :], in0=a_[0:n, :], scalar=sm[0:n, 3:4], in1=fwb[0:n, :],
                                                                    op0=ALU.mult, op1=ALU.mult), reads=[an, "sm", "fwb"], writes=[an])
        p.dma("sp", xo_d[rs, :], a_[0:n, :], reads=[an], key=an + "_o", is_output=True)
    return p.finish()


B_, S_, L_, D_ = 2, 4096, 256, 2048
T_ = S_ + L_
RL_, RC_ = S_ // 4, L_ // 4
NT_ = NCORES * (RL_ + RC_)
C_GLA_Q, C_GLA_K, C_GLA_V, C_GLA_OG, C_GLA_GLR = 0, 256, 512, 1024, 1536
C_GDN_Q, C_GDN_K, C_GDN_V, C_GDN_OG, C_GDN_BLR, C_GDN_ALR = 1568, 2336, 3104, 3872, 4640, 4652
C_DF_Q, C_DF_K, C_DF_V = 4664, 5432, 6200
NCOL_ = 6968
GRID_W_ = 64
_PERM = np.concatenate([np.arange(16, 32), np.arange(0, 16), np.arange(48, 64), np.arange(32, 48)])

_prog_cache = {}


def _get(name, fn, *args):
    key = (name,) + tuple(args)
    if key not in _prog_cache:
        _prog_cache[key] = fn(*args)
    return _prog_cache[key]


_DBG = {}
_KDEBUG = bool(os.environ.get("KDEBUG"))


def _run(nc, in_maps):
    t0 = time.time()
    res = run_bass_kernel_spmd(nc, in_maps, core_ids=list(range(NCORES)))
    if _KDEBUG:
        print("  launch %.1fs" % (time.time() - t0), flush=True)
    return res.results


def _f32(a):
    return np.ascontiguousarray(a, dtype=np.float32)


def _rope_tables(pos):
    row = (pos // GRID_W_).astype(np.float64)
    col = (pos % GRID_W_).astype(np.float64)
    inv = 1.0 / (10000.0 ** (np.arange(0, 32, 2) / 32.0))
    cos = np.zeros((64, len(pos)))
    sin = np.zeros((64, len(pos)))
    for half, pp in ((0, row), (1, col)):
        ang = pp[None, :] * inv[:, None]
        b = half * 32
        cos[b:b + 16] = np.cos(ang)
        cos[b + 16:b + 32] = np.cos(ang)
        sin[b:b + 16] = -np.sin(ang)
        sin[b + 16:b + 32] = np.sin(ang)
    return np.stack([cos, sin]).astype(np.float32)


def _scan_order(ctx_part, lat_part, z):
    if z == 1:
        ctx_part, lat_part = ctx_part[::-1], lat_part[::-1]
    return np.concatenate([ctx_part, lat_part], axis=0)


def _unscan(o, z):
    oc, ol = o[:L_], o[L_:]
    if z == 1:
        oc, ol = oc[::-1], ol[::-1]
    return oc, ol


def _rows(lat_b, ctx_b, qd):
    return np.concatenate([lat_b[qd * RL_:(qd + 1) * RL_], ctx_b[qd * RC_:(qd + 1) * RC_]], axis=0)


def _tT(v):
    return v.reshape(16, 128).T


def run_layer(l, x, ctx, mod, P, final):
    D = D_
    msl = lambda i: slice(i * D, (i + 1) * D)
    SH1, SC1, G1, SH2, SC2, G2 = [msl(i) for i in range(6)]
    mlat = [mod[b, l] for b in range(B_)]
    mctx = mod[2, l]
    nc = _get("p1", build_p1, RL_, RC_, NCOL_)
    w_in = _f32(P["w_in"][l])
    in_maps = []
    for c in range(NCORES):
        b, qd = c // 4, c % 4
        mvec = np.stack([P["norm1_w"][l], mlat[b][SC1], mlat[b][SH1], mctx[SC1], mctx[SH1]])
        in_maps.append({"xr": _f32(_rows(x[b], ctx[b], qd)), "mvec": _f32(mvec), "w": w_in})
    res = _run(nc, in_maps)
    proj_lat = [np.concatenate([res[4 * b + qd]["proj"][:RL_] for qd in range(4)], 0) for b in range(B_)]
    proj_ctx = [np.concatenate([res[4 * b + qd]["proj"][RL_:] for qd in range(4)], 0) for b in range(B_)]
    del res
    if _KDEBUG:
        _DBG["proj_lat%d" % l] = proj_lat
        _DBG["proj_ctx%d" % l] = proj_ctx
    NCH = T_ // 64
    nc = _get("gla", build_gla, T_)
    in_maps = []
    for c in range(NCORES):
        b, h = c // 4, c % 4
        pl, pc = proj_lat[b], proj_ctx[b]
        qs, ks, vs, gs, gw = [], [], [], [], []
        for z in range(2):
            sl = lambda c0, w: _scan_order(pc[:, c0:c0 + w], pl[:, c0:c0 + w], z)
            qs.append(sl(C_GLA_Q + h * 64, 64))
            ks.append(sl(C_GLA_K + h * 64, 64))
            vs.append(sl(C_GLA_V + h * 128, 128))
            gs.append(sl(C_GLA_GLR + z * 16, 16))
            gw.append(np.concatenate([P["gla_gate_w"][l][z][:, h * 64:(h + 1) * 64], P["gla_gate_b"][l][z][None, h * 64:(h + 1) * 64]], 0))
        q, k, v, glr = np.stack(qs), np.stack(ks), np.stack(vs), np.stack(gs)
        in_maps.append({
            "qT": _f32(q.transpose(0, 2, 1)), "kT": _f32(k.transpose(0, 2, 1)),
            "kt": _f32(k.reshape(2, NCH, 64, 64).transpose(0, 2, 1, 3)),
            "vt": _f32(v.reshape(2, NCH, 64, 128).transpose(0, 2, 1, 3)),
            "glrT": _f32(np.concatenate([glr.transpose(0, 2, 1), np.ones((2, 1, T_), np.float32)], 1)),
            "gwb": _f32(np.stack(gw)),
        })
    res = _run(nc, in_maps)
    gla_lat = np.zeros((B_, 2, S_, 512), np.float32)
    gla_ctx = np.zeros((B_, 2, L_, 512), np.float32)
    for c in range(NCORES):
        b, h = c // 4, c % 4
        o = res[c]["o"].transpose(0, 2, 1, 3).reshape(2, T_, 128)
        for z in range(2):
            oc, ol = _unscan(o[z], z)
            gla_lat[b, z, :, h * 128:(h + 1) * 128] = ol
            gla_ctx[b, z, :, h * 128:(h + 1) * 128] = oc
    del res
    nc = _get("gdn", build_gdn, L_, S_, 1)
    gdn_lat = np.zeros((B_, 2, S_, 768), np.float32)
    gdn_ctx = np.zeros((B_, 2, L_, 768), np.float32)
    for i in range(3):
        in_maps = []
        for c in range(NCORES):
            b = c // 4
            pl, pc = proj_lat[b], proj_ctx[b]
            kk = (c % 4) * 3 + i
            h, z = kk // 2, kk % 2
            sl = lambda c0, w: _scan_order(pc[:, c0:c0 + w], pl[:, c0:c0 + w], z)
            xs = np.stack([sl(C_GDN_Q + h * 128, 128).T, sl(C_GDN_K + h * 128, 128).T, sl(C_GDN_V + h * 128, 128).T])
            cw = np.stack([P["gdn_conv_w"][l][:, j * 768 + h * 128: j * 768 + (h + 1) * 128].T for j in range(3)], 1)
            if z == 1:
                cw = cw[:, :, ::-1]
            blr = sl(C_GDN_BLR + z * 6 + h, 1)[:, 0]
            alr = sl(C_GDN_ALR + z * 6 + h, 1)[:, 0]
            ba = np.stack([blr.reshape(NCH, 64).T, alr.reshape(NCH, 64).T], 1)
            sc = np.stack([np.full(64, P["gdn_a_log"][l][z, h]), np.full(64, P["gdn_dt_bias"][l][z, h])], 1)
            in_maps.append({"x": _f32(xs[None]), "cw": _f32(cw[None]), "ba": _f32(ba[None]), "sc": _f32(sc[None])})
        res = _run(nc, in_maps)
        for c in range(NCORES):
            b = c // 4
            kk = (c % 4) * 3 + i
            h, z = kk // 2, kk % 2
            o = res[c]["o"][0].transpose(1, 0, 2).reshape(T_, 128)
            oc, ol = _unscan(o, z)
            gdn_lat[b, z, :, h * 128:(h + 1) * 128] = ol
            gdn_ctx[b, z, :, h * 128:(h + 1) * 128] = oc
        del res
    TQ = S_ // 2
    NKT = T_ // 128
    nc = _get("diff", build_diff, TQ, S_, L_, 3, True, -1.0)
    lam0 = 0.8 - 0.6 * math.exp(-0.3 * l)
    csk = _rope_tables(np.arange(S_))
    csq = [_rope_tables(np.arange(S_)[hf * TQ:(hf + 1) * TQ]) for hf in range(2)]
    dl = _f32(np.concatenate([P["diff_lambda"][l].reshape(-1), P["diff_norm_w"][l], [lam0, 1.0 - lam0]])[None])
    in_maps = []
    for c in range(NCORES):
        b = c // 4
        pl, pc = proj_lat[b], proj_ctx[b]
        qi, ki, kci, qci, vi, csi = [], [], [], [], [], []
        for i in range(3):
            kk = (c % 4) * 3 + i
            h, hf = kk // 2, kk % 2
            ql = pl[:, C_DF_Q + h * 128: C_DF_Q + (h + 1) * 128].reshape(S_, 2, 64)
            kl = pl[:, C_DF_K + h * 128: C_DF_K + (h + 1) * 128].reshape(S_, 2, 64)
            qc = pc[:, C_DF_Q + h * 128: C_DF_Q + (h + 1) * 128].reshape(L_, 2, 64)
            kc = pc[:, C_DF_K + h * 128: C_DF_K + (h + 1) * 128].reshape(L_, 2, 64)
            vl = pl[:, C_DF_V + h * 128: C_DF_V + (h + 1) * 128]
            vc = pc[:, C_DF_V + h * 128: C_DF_V + (h + 1) * 128]
            qT = ql[hf * TQ:(hf + 1) * TQ].transpose(1, 2, 0)
            kT = kl.transpose(1, 2, 0)
            qi.append(np.stack([qT, qT[:, _PERM, :]], 1))
            ki.append(np.stack([kT, kT[:, _PERM, :]], 1))
            kci.append(kc.transpose(1, 2, 0))
            qci.append(qc.transpose(1, 2, 0))
            vi.append(np.concatenate([vc, vl], 0).reshape(NKT, 128, 128).transpose(1, 0, 2))
            csi.append(csq[hf])
        in_maps.append({"q": _f32(np.stack(qi)), "k": _f32(np.stack(ki)), "kc": _f32(np.stack(kci)), "qc": _f32(np.stack(qci)),
                        "v": _f32(np.stack(vi)), "csq": _f32(np.stack(csi)), "csk": csk, "dl": dl})
    res = _run(nc, in_maps)
    df_lat = np.zeros((B_, S_, 768), np.float32)
    df_ctx = np.zeros((B_, L_, 768), np.float32)
    for c in range(NCORES):
        b = c // 4
        for i in range(3):
            kk = (c % 4) * 3 + i
            h, hf = kk // 2, kk % 2
            y = res[c]["y"][i].transpose(1, 0, 2).reshape(TQ, 128)
            df_lat[b, hf * TQ:(hf + 1) * TQ, h * 128:(h + 1) * 128] = y
            if hf == 0:
                df_ctx[b, :, h * 128:(h + 1) * 128] = res[c]["yc"][i].transpose(1, 0, 2).reshape(L_, 128)
    del res
    if _KDEBUG:
        _DBG["gla_lat%d" % l], _DBG["gla_ctx%d" % l] = gla_lat, gla_ctx
        _DBG["gdn_lat%d" % l], _DBG["gdn_ctx%d" % l] = gdn_lat, gdn_ctx
        _DBG["df_lat%d" % l], _DBG["df_ctx%d" % l] = df_lat, df_ctx
    nc = _get("p3", build_p3, RL_, RC_)
    wo, rw, rb = _f32(P["w_out"][l]), _f32(P["router_w"][l]), _f32(P["router_b"][l][None])
    hn = _f32(np.concatenate([P["gla_norm_w"][l], P["gdn_norm_w"][l]])[None])
    in_maps = []
    for c in range(NCORES):
        b, qd = c // 4, c % 4
        og_l = np.concatenate([proj_lat[b][:, C_GLA_OG:C_GLA_OG + 512], proj_lat[b][:, C_GDN_OG:C_GDN_OG + 768]], 1)
        og_c = np.concatenate([proj_ctx[b][:, C_GLA_OG:C_GLA_OG + 512], proj_ctx[b][:, C_GDN_OG:C_GDN_OG + 768]], 1)
        mT = np.stack([_tT(P["norm2_w"][l]), _tT(mlat[b][SC2]), _tT(mlat[b][SH2]), _tT(mctx[SC2]), _tT(mctx[SH2])], 2)
        in_maps.append({
            "xr": _f32(_rows(x[b], ctx[b], qd)),
            "go": _f32(np.stack([_rows(gla_lat[b, z], gla_ctx[b, z], qd) for z in range(2)])),
            "do": _f32(np.stack([_rows(gdn_lat[b, z], gdn_ctx[b, z], qd) for z in range(2)])),
            "og": _f32(_rows(og_l, og_c, qd)), "df": _f32(_rows(df_lat[b], df_ctx[b], qd)),
            "mT": _f32(mT), "g1": _f32(np.stack([mlat[b][G1], mctx[G1]])), "hn": hn, "wo": wo, "rw": rw, "rb": rb,
        })
    res = _run(nc, in_maps)
    xn = [res[c]["xn"] for c in range(NCORES)]
    h2T = np.concatenate([res[c]["h2T"] for c in range(NCORES)], axis=1)
    G = np.concatenate([res[c]["G"] for c in range(NCORES)], axis=0)
    del res, proj_lat, proj_ctx
    if _KDEBUG:
        _DBG["xn%d" % l], _DBG["h2T%d" % l], _DBG["G%d" % l] = xn, h2T, G
    nc = _get("p4", build_p4, NT_, 1024, 4, 2048)
    in_maps = []
    for c in range(NCORES):
        es = slice(4 * c, 4 * c + 4)
        w1 = P["expert_w1"][l][es].reshape(4, 2048, 16, 128, 2).transpose(0, 1, 2, 4, 3).reshape(4, 2048, 4096)
        b1c = P["expert_b1"][l][es].reshape(4, 16, 128, 2).transpose(2, 0, 1, 3).reshape(128, 4, 32)
        Gl = G[:, es]
        in_maps.append({"h2T": h2T, "Gl": _f32(Gl), "GlT": _f32(Gl.T), "w1": _f32(w1), "b1c": _f32(b1c),
                        "w2": _f32(P["expert_w2"][l][es]), "b2": _f32(P["expert_b2"][l][es])})
    res = _run(nc, in_maps)
    del in_maps
    parts = [res[c]["part"] for c in range(NCORES)]
    del res
    if _KDEBUG:
        _DBG["parts%d" % l] = parts
    nc = _get("p5", build_p5, RL_, RC_, bool(final))
    R = RL_ + RC_
    fw = _f32(P["final_norm_w"][None])
    in_maps = []
    for c in range(NCORES):
        b = c // 4
        in_maps.append({"xn": xn[c], "parts": _f32(np.stack([parts[k][c * R:(c + 1) * R] for k in range(NCORES)])),
                        "g2": _f32(np.stack([mlat[b][G2], mctx[G2]])), "fw": fw})
    res = _run(nc, in_maps)
    x_new = np.stack([np.concatenate([res[4 * b + qd]["xo"][:RL_] for qd in range(4)], 0) for b in range(B_)])
    ctx_new = np.stack([np.concatenate([res[4 * b + qd]["xo"][RL_:] for qd in range(4)], 0) for b in range(B_)])
    return x_new, ctx_new


def kernel(**inputs):
    P = {k: np.asarray(v) for k, v in inputs.items()}
    x = _f32(P["x"])
    ctx = _f32(P["ctx"])
    mod = run_p0(_f32(P["c"]), _f32(P["c_ctx"]), _f32(P["w_mod"]), _f32(P["b_mod"]))
    for l in range(2):
        x, ctx = run_layer(l, x, ctx, mod, P, final=(l == 1))
    return np.ascontiguousarray(x, dtype=np.float32)
```

```python
import contextlib
import math
import os
import time
import numpy as np
import concourse.bass as bass
import concourse.mybir as mybir
from concourse.bass_utils import run_bass_kernel_spmd

F32 = mybir.dt.float32
BF16 = mybir.dt.bfloat16
AF = mybir.ActivationFunctionType
ALU = mybir.AluOpType
AX = mybir.AxisListType

NCORES = 8


class Prog:
    ENG = ("pe", "act", "dve", "pool", "sp")

    def __init__(self):
        self.nc = bass.Bass("TRN2", target_bir_lowering=False)
        self.stack = contextlib.ExitStack()
        self.q = {e: [] for e in self.ENG}
        self.cnt = {e: 0 for e in self.ENG}
        self.pending = {e: False for e in self.ENG}
        self.esem = {e: self.stack.enter_context(self.nc.semaphore("s_" + e)) for e in self.ENG}
        self.dsem = {}
        self.dcnt = {}
        self.waited = {}
        self.lastw = {}
        self.readers = {}
        self.out_final = {}
        self.nbuf = 0

    def dram(self, name, shape, dt, kind):
        return self.nc.dram_tensor(name, list(shape), dt, kind=kind).ap()

    def sb(self, name, shape, dt=F32):
        return self.stack.enter_context(self.nc.sbuf_tensor("sb_" + name, list(shape), dt))

    def ps(self, name, shape, dt=F32):
        return self.stack.enter_context(self.nc.psum_tensor("ps_" + name, list(shape), dt))

    def _deps(self, eng, reads, writes):
        need = {}
        for b in reads:
            w = self.lastw.get(b)
            if w is not None:
                need[w[0]] = max(need.get(w[0], 0), w[1])
        for b in writes:
            w = self.lastw.get(b)
            if w is not None:
                need[w[0]] = max(need.get(w[0], 0), w[1])
            for r in self.readers.get(b, ()):
                need[r[0]] = max(need.get(r[0], 0), r[1])
        out = []
        for key, val in need.items():
            if key == ("e", eng) and val > self.cnt[eng]:
                continue
            if self.waited.get((eng, key), 0) >= val:
                continue
            self.waited[(eng, key)] = val
            sem = self.esem[key[1]] if key[0] == "e" else self.dsem[key[1]]
            out.append((sem, val))
        return out

    def _mark(self, tag, reads, writes):
        for b in reads:
            self.readers.setdefault(b, []).append(tag)
        for b in writes:
            self.lastw[b] = tag
            self.readers[b] = []

    def op(self, eng, fn, reads=(), writes=(), inc=True):
        waits = self._deps(eng, reads, writes)
        tag = (("e", eng), self.cnt[eng] + 1)
        self._mark(tag, reads, writes)
        sem = self.esem[eng]
        if inc:
            self.cnt[eng] += 1
            self.pending[eng] = False
        else:
            self.pending[eng] = True

        def thunk(e, fn=fn, waits=waits, inc=inc, sem=sem):
            for s, v in waits:
                e.wait_ge(s, v)
            ins = fn(e)
            if inc:
                ins.then_inc(sem, 1)
        self.q[eng].append(thunk)

    def dma(self, eng, out, in_, reads=(), writes=(), key=None, is_output=False, **kw):
        if key is None:
            key = (writes[0] if writes else reads[0])
        if key not in self.dsem:
            self.dsem[key] = self.stack.enter_context(self.nc.semaphore("d%d" % len(self.dsem)))
            self.dcnt[key] = 0
        waits = self._deps(eng, reads, writes)
        self.dcnt[key] += 16
        tag = (("d", key), self.dcnt[key])
        self._mark(tag, reads, writes)
        sem = self.dsem[key]
        if is_output:
            self.out_final[key] = self.dcnt[key]

        def thunk(e, waits=waits, sem=sem, out=out, in_=in_, kw=kw):
            for s, v in waits:
                e.wait_ge(s, v)
            e.dma_start(out=out, in_=in_, **kw).then_inc(sem, 16)
        self.q[eng].append(thunk)

    def finish(self):
        nc = self.nc
        finals = [(self.dsem[k], v) for k, v in self.out_final.items()]

        def fin(e, finals=finals):
            for s, v in finals:
                e.wait_ge(s, v)
        self.q["sp"].append(fin)
        with nc.Block() as block:
            @block.tensor
            def _(e):
                for t in self.q["pe"]:
                    t(e)

            @block.scalar
            def _(e):
                for t in self.q["act"]:
                    t(e)

            @block.vector
            def _(e):
                for t in self.q["dve"]:
                    t(e)

            @block.gpsimd
            def _(e):
                for t in self.q["pool"]:
                    t(e)

            @block.sync
            def _(e):
                for t in self.q["sp"]:
                    t(e)
        self.stack.close()
        return nc

    def mm(self, out, lhsT, rhs, start, stop, reads=(), writes=(), inc=None):
        if inc is None:
            inc = stop
        self.op("pe", lambda e: e.matmul(out, lhsT, rhs, start=start, stop=stop),
                reads=reads, writes=writes, inc=inc)


MOD_COLS = 12288 // NCORES


def build_p0():
    p = Prog()
    wm = p.dram("wm", [2, 2048, MOD_COLS], F32, "ExternalInput")
    cT = p.dram("cT", [128, 16, 3], F32, "ExternalInput")
    bm = p.dram("bm", [3, 2, MOD_COLS], F32, "ExternalInput")
    mo = p.dram("mo", [3, 2, MOD_COLS], F32, "ExternalOutput")
    ct = p.sb("ct", [128, 16, 3])
    sct = p.sb("sct", [128, 16, 3])
    bmt = p.sb("bmt", [3, 2, MOD_COLS])
    res = p.sb("res", [3, 2, MOD_COLS])
    wbuf = [p.sb("wb%d" % i, [128, 16, 512]) for i in range(2)]
    pst = [p.ps("ps%d" % i, [128, 512]) for i in range(2)]
    p.dma("sp", ct[:], cT, writes=["ct"])
    p.dma("sp", bmt[:], bm, writes=["bmt"])
    p.op("act", lambda e: e.activation(out=sct[:], in_=ct[:], func=AF.Silu), reads=["ct"], writes=["sct"])
    it = 0
    for l in range(2):
        for n in range(MOD_COLS // 512):
            wb = wbuf[it % 2]
            wn = "wb%d" % (it % 2)
            pt = pst[it % 2]
            pn = "ps%d" % (it % 2)
            src = wm[l, :, n * 512:(n + 1) * 512].rearrange("(kc p) j -> p kc j", p=128)
            p.dma("sp" if it % 2 == 0 else "pool", wb[:], src, writes=[wn])
            for kc in range(16):
                p.mm(pt[0:3, :], sct[:, kc, :], wb[:, kc, :], start=(kc == 0), stop=(kc == 15),
                     reads=["sct", wn], writes=[pn])
            p.op("dve", lambda e, pt=pt, l=l, n=n: e.tensor_tensor(
                out=res[:, l, n * 512:(n + 1) * 512], in0=pt[0:3, :], in1=bmt[:, l, n * 512:(n + 1) * 512],
                op=ALU.add), reads=[pn, "bmt"], writes=["res"])
            it += 1
    p.dma("sp", mo, res[:], reads=["res"], key="mo", is_output=True)
    return p.finish()


def run_p0(c, c_ctx, w_mod, b_mod):
    cv = np.concatenate([c, c_ctx[None, :]], axis=0).astype(np.float32)
    cT = np.ascontiguousarray(cv.T.reshape(16, 128, 3).transpose(1, 0, 2))
    in_maps = []
    for i in range(NCORES):
        sl = slice(i * MOD_COLS, (i + 1) * MOD_COLS)
        in_maps.append({
            "wm": np.ascontiguousarray(w_mod[:, :, sl]),
            "cT": cT,
            "bm": np.ascontiguousarray(np.broadcast_to(b_mod[None, :, sl], (3, 2, MOD_COLS))),
        })
    nc = build_p0()
    res = run_bass_kernel_spmd(nc, in_maps, core_ids=list(range(NCORES)))
    mod = np.concatenate([r["mo"] for r in res.results], axis=2)
    return mod


EPS = 1e-6


def row_tiles(RL, RC):
    tiles = []
    r = 0
    while r < RL:
        n = min(128, RL - r)
        tiles.append((r, n, 0))
        r += n
    while r < RL + RC:
        n = min(128, RL + RC - r)
        tiles.append((r, n, 1))
        r += n
    return tiles


def emit_norm_mod_T(p, xr, tiles, A, Bv, hT, pT, tagp):
    xt = [p.sb(tagp + "xt%d" % i, [128, 2048]) for i in range(2)]
    tmp = p.sb(tagp + "tmp", [128, 2048])
    sq = p.sb(tagp + "sq", [128, 2048], BF16)
    hb = [p.sb(tagp + "hb%d" % i, [128, 2048], BF16) for i in range(2)]
    ss = p.sb(tagp + "ss", [128, 2 * len(tiles)])
    ident = p.sb(tagp + "ident", [128, 128], BF16)
    identf = p.sb(tagp + "identf", [128, 128])
    p.op("pool", lambda e: e.memset(identf[:], 1.0), writes=[tagp + "identf"])
    p.op("pool", lambda e: e.affine_select(out=identf[:], in_=identf[:], pattern=[[-1, 128]],
                                           compare_op=ALU.is_equal, fill=0.0, base=0, channel_multiplier=1),
         reads=[tagp + "identf"], writes=[tagp + "identf"])
    p.op("pool", lambda e: e.tensor_copy(out=ident[:], in_=identf[:]), reads=[tagp + "identf"], writes=[tagp + "ident"])
    for ti, (r0, n, g) in enumerate(tiles):
        x_ = xt[ti % 2]
        xn = tagp + "xt%d" % (ti % 2)
        h_ = hb[ti % 2]
        hn = tagp + "hb%d" % (ti % 2)
        p.dma("sp", x_[0:n, :], xr[r0:r0 + n, :], writes=[xn])
        c0 = 2 * ti
        p.op("act", lambda e, x_=x_, n=n, c0=c0: e.activation(out=sq[0:n, :], in_=x_[0:n, :], func=AF.Square,
                                                            accum_out=ss[0:n, c0:c0 + 1]),
             reads=[xn], writes=[tagp + "sq", tagp + "ss"])
        p.op("act", lambda e, n=n, c0=c0: e.activation(out=ss[0:n, c0 + 1:c0 + 2], in_=ss[0:n, c0:c0 + 1],
                                                     func=AF.Sqrt, scale=1.0 / 2048.0, bias=epsb[0:n, :]),
             reads=[tagp + "ss", "epsb"], writes=[tagp + "ss"]) if False else None
        p.op("dve", lambda e, n=n, c0=c0: e.tensor_scalar(out=ss[0:n, c0 + 1:c0 + 2], in0=ss[0:n, c0:c0 + 1],
                                                        scalar1=1.0 / 2048.0, scalar2=EPS, op0=ALU.mult, op1=ALU.add),
             reads=[tagp + "ss"], writes=[tagp + "ss"])
        p.op("act", lambda e, n=n, c0=c0: e.activation(out=ss[0:n, c0:c0 + 1], in_=ss[0:n, c0 + 1:c0 + 2], func=AF.Sqrt),
             reads=[tagp + "ss"], writes=[tagp + "ss"])
        p.op("dve", lambda e, n=n, c0=c0: e.reciprocal(out=ss[0:n, c0 + 1:c0 + 2], in_=ss[0:n, c0:c0 + 1]),
             reads=[tagp + "ss"], writes=[tagp + "ss"])
        p.op("dve", lambda e, x_=x_, n=n, c0=c0, g=g: e.scalar_tensor_tensor(
            out=tmp[0:n, :], in0=x_[0:n, :], scalar=ss[0:n, c0 + 1:c0 + 2], in1=A[g][0:n, :],
            op0=ALU.mult, op1=ALU.mult), reads=[xn, tagp + "ss", "A%d" % g], writes=[tagp + "tmp"])
        p.op("dve", lambda e, h_=h_, n=n, g=g: e.tensor_tensor(out=h_[0:n, :], in0=tmp[0:n, :], in1=Bv[g][0:n, :], op=ALU.add),
             reads=[tagp + "tmp", "B%d" % g], writes=[hn])
        for k4 in range(4):
            pt = pT[(ti * 4 + k4) % 2]
            pn = tagp + "pT%d" % ((ti * 4 + k4) % 2)
            for kk in range(4):
                kc = k4 * 4 + kk
                p.op("pe", lambda e, pt=pt, kk=kk, h_=h_, n=n, kc=kc: e.transpose(
                    out=pt[:, kk, 0:n], in_=h_[0:n, kc * 128:(kc + 1) * 128], identity=ident[0:n, 0:n]),
                    reads=[hn, tagp + "ident"], writes=[pn], inc=(kk == 3))
            eng = "act" if k4 % 2 == 0 else "dve"
            if eng == "act":
                p.op("act", lambda e, pt=pt, k4=k4, r0=r0, n=n: e.copy(out=hT[:, k4 * 4:(k4 + 1) * 4, r0:r0 + n], in_=pt[:, :, 0:n]),
                     reads=[pn], writes=["hT"])
            else:
                p.op("dve", lambda e, pt=pt, k4=k4, r0=r0, n=n: e.tensor_copy(out=hT[:, k4 * 4:(k4 + 1) * 4, r0:r0 + n], in_=pt[:, :, 0:n]),
                     reads=[pn], writes=["hT"])


def load_mod_vectors(p, mvec, idx_w, idx_sc, idx_sh, names):
    nw = p.sb("nw", [128, 2048])
    p.dma("sp", nw[:], mvec[idx_w, :].partition_broadcast(128), writes=["nw"])
    A, Bv = [], []
    for g in range(len(idx_sc)):
        a = p.sb("A%d" % g, [128, 2048])
        b = p.sb("B%d" % g, [128, 2048])
        p.dma("sp", a[:], mvec[idx_sc[g], :].partition_broadcast(128), writes=["A%d" % g])
        p.dma("sp", b[:], mvec[idx_sh[g], :].partition_broadcast(128), writes=["B%d" % g])
        p.op("dve", lambda e, a=a: e.scalar_tensor_tensor(out=a[:], in0=a[:], scalar=1.0, in1=nw[:], op0=ALU.add, op1=ALU.mult),
             reads=["A%d" % g, "nw"], writes=["A%d" % g])
        A.append(a)
        Bv.append(b)
    return A, Bv


def build_p1(RL, RC, NCOL):
    p = Prog()
    R = RL + RC
    xr = p.dram("xr", [R, 2048], F32, "ExternalInput")
    mvec = p.dram("mvec", [5, 2048], F32, "ExternalInput")
    w = p.dram("w", [2048, NCOL], F32, "ExternalInput")
    proj = p.dram("proj", [R, NCOL], F32, "ExternalOutput")
    tiles = row_tiles(RL, RC)
    A, Bv = load_mod_vectors(p, mvec, 0, [1, 3], [2, 4], None)
    hT = p.sb("hT", [128, 16, R], BF16)
    pT = [p.ps("pT%d" % i, [128, 4, 128], BF16) for i in range(2)]
    emit_norm_mod_T(p, xr, tiles, A, Bv, hT, pT, "")
    wt = [p.sb("wt%d" % i, [128, 16, 512], BF16) for i in range(2)]
    nt = len(tiles)
    st = [p.sb("st%d" % i, [128, nt, 512]) for i in range(2)]
    pacc = [p.ps("pacc%d" % i, [128, 512]) for i in range(4)]
    ncol_t = (NCOL + 511) // 512
    ev = 0
    for j in range(ncol_t):
        c0 = j * 512
        cw = min(512, NCOL - c0)
        w_ = wt[j % 2]
        wn = "wt%d" % (j % 2)
        s_ = st[j % 2]
        sn = "st%d" % (j % 2)
        p.dma("pool", w_[:, :, 0:cw], w[:, c0:c0 + cw].rearrange("(kc p) c -> p kc c", p=128), writes=[wn])
        for ti, (r0, n, g) in enumerate(tiles):
            pa = pacc[ev % 4]
            pn = "pacc%d" % (ev % 4)
            for kc in range(16):
                p.mm(pa[0:n, 0:cw], hT[:, kc, r0:r0 + n], w_[:, kc, 0:cw], start=(kc == 0), stop=(kc == 15),
                     reads=["hT", wn], writes=[pn])
            if ev % 2 == 0:
                p.op("act", lambda e, pa=pa, s_=s_, ti=ti, n=n, cw=cw: e.copy(out=s_[0:n, ti, 0:cw], in_=pa[0:n, 0:cw]),
                     reads=[pn], writes=[sn])
            else:
                p.op("dve", lambda e, pa=pa, s_=s_, ti=ti, n=n, cw=cw: e.tensor_copy(out=s_[0:n, ti, 0:cw], in_=pa[0:n, 0:cw]),
                     reads=[pn], writes=[sn])
            ev += 1
        nfull = sum(1 for (_, n, _) in tiles if n == 128)
        full_ok = all(tiles[i][1] == 128 for i in range(nfull))
        assert full_ok
        if nfull:
            p.dma("sp", proj[0:nfull * 128, c0:c0 + cw].rearrange("(t p) c -> p t c", p=128), s_[:, 0:nfull, 0:cw],
                  reads=[sn], key=sn + "_o", is_output=True)
        for ti in range(nfull, nt):
            r0, n, g = tiles[ti]
            p.dma("sp", proj[r0:r0 + n, c0:c0 + cw], s_[0:n, ti, 0:cw], reads=[sn], key=sn + "_o", is_output=True)
    return p.finish()


def make_consts64(p):
    c = {}
    ones = p.sb("c_ones", [128, 128])
    p.op("pool", lambda e: e.memset(ones[:], 1.0), writes=["c_ones"])
    negones = p.sb("c_negones", [64, 64])
    p.op("pool", lambda e: e.memset(negones[:], -1.0), writes=["c_negones"])
    tri = p.sb("c_tri", [64, 64])
    p.op("pool", lambda e: e.affine_select(out=tri[:], in_=ones[0:64, 0:64], pattern=[[1, 64]], compare_op=ALU.is_ge,
                                           fill=0.0, base=0, channel_multiplier=-1), reads=["c_ones"], writes=["c_tri"])
    stri = p.sb("c_stri", [64, 64])
    p.op("pool", lambda e: e.affine_select(out=stri[:], in_=ones[0:64, 0:64], pattern=[[1, 64]], compare_op=ALU.is_gt,
                                           fill=0.0, base=0, channel_multiplier=-1), reads=["c_ones"], writes=["c_stri"])
    ident = p.sb("c_ident", [128, 128])
    p.op("pool", lambda e: e.affine_select(out=ident[:], in_=ones[:], pattern=[[-1, 128]], compare_op=ALU.is_equal,
                                           fill=0.0, base=0, channel_multiplier=1), reads=["c_ones"], writes=["c_ident"])
    lowI = p.sb("c_lowI", [64, 64])
    p.op("pool", lambda e: e.affine_select(out=lowI[:], in_=ones[0:64, 0:64], pattern=[[-1, 64]], compare_op=ALU.is_ge,
                                           fill=0.0, base=0, channel_multiplier=1), reads=["c_ones"], writes=["c_lowI"])
    lowS = p.sb("c_lowS", [64, 64])
    p.op("pool", lambda e: e.affine_select(out=lowS[:], in_=ones[0:64, 0:64], pattern=[[-1, 64]], compare_op=ALU.is_gt,
                                           fill=0.0, base=0, channel_multiplier=1), reads=["c_ones"], writes=["c_lowS"])
    c.update(ones=ones, negones=negones, tri=tri, stri=stri, ident=ident, lowI=lowI, lowS=lowS)
    c["names"] = ["c_ones", "c_negones", "c_tri", "c_stri", "c_ident", "c_lowI", "c_lowS"]
    return c


def build_gla(T):
    NCH = T // 64
    p = Prog()
    qT_d = p.dram("qT", [2, 64, T], F32, "ExternalInput")
    kT_d = p.dram("kT", [2, 64, T], F32, "ExternalInput")
    kt_d = p.dram("kt", [2, 64, NCH, 64], F32, "ExternalInput")
    vt_d = p.dram("vt", [2, 64, NCH, 128], F32, "ExternalInput")
    gl_d = p.dram("glrT", [2, 17, T], F32, "ExternalInput")
    gw_d = p.dram("gwb", [2, 17, 64], F32, "ExternalInput")
    o_d = p.dram("o", [2, 64, NCH, 128], F32, "ExternalOutput")
    C = make_consts64(p)
    cn = C["names"]
    qT = p.sb("qT", [64, T])
    kT = p.sb("kT", [64, T])
    kt = p.sb("kt", [64, NCH, 64])
    vt = p.sb("vt", [64, NCH, 128])
    glrT = p.sb("glrT", [17, T])
    gwb = p.sb("gwb", [17, 64])
    ost = p.sb("ost", [64, NCH, 128])
    S = [p.sb("S%d" % i, [64, 128]) for i in range(2)]
    NR = 3
    tmp = [p.sb("tm%d" % i, [64, 10, 64]) for i in range(NR)]
    bLA = p.ps("bLA", [128, 512])
    bBT = p.ps("bBT", [128, 512])
    bAT = p.ps("bAT", [128, 512])
    bO = [p.ps("bO%d" % i, [128, 512]) for i in range(2)]
    bU = [p.ps("bU%d" % i, [128, 512]) for i in range(2)]
    for z in range(2):
        p.dma("sp", qT[:], qT_d[z], writes=["qT"])
        p.dma("sp", kT[:], kT_d[z], writes=["kT"])
        p.dma("sp", kt[:], kt_d[z], writes=["kt"])
        p.dma("sp", vt[:], vt_d[z], writes=["vt"])
        p.dma("sp", glrT[:], gl_d[z], writes=["glrT"])
        p.dma("sp", gwb[:], gw_d[z], writes=["gwb"])
        p.op("dve", lambda e: e.memset(S[0][:], 0.0), writes=["S0"])
        for n in range(NCH):
            r = n % NR
            t_ = tmp[r]
            tn = lambda k, r=r: "tm%d_%s" % (r, k)
            _pn = {"la": "bLA", "BT": "bBT", "blT": "bBT", "Bm": "bBT", "att": "bAT"}
            an = lambda k: _pn[k]
            bn = lambda k, n=n: ("bO%d" % (n % 2)) if k == "o" else ("bU%d" % (n % 2))
            Sc, Sn_ = S[n % 2], S[(n + 1) % 2]
            Scn, Snn = "S%d" % (n % 2), "S%d" % ((n + 1) % 2)
            cs = slice(n * 64, (n + 1) * 64)
            e1, sp_, E1T, E2T, E3, qdT, kdT, kg, attT = [t_[:, i, :] for i in range(9)]
            dec = t_[:, 9, 0:1]
            la_ps, BT_ps, blT_ps, Bm_ps, att_ps = bLA[0:64, 0:64], bBT[0:64, 64:128], bBT[0:64, 128:130], bBT[0:64, 192:256], bAT[0:64, 256:320]
            o_ps, u_ps = bO[n % 2][0:64, 0:128], bU[n % 2][0:64, 128:256]
            p.mm(la_ps, glrT[:, cs], gwb[:], True, True, reads=["glrT", "gwb"], writes=[an("la")])
            p.op("act", lambda e, e1=e1, la_ps=la_ps: e.activation(out=e1, in_=la_ps, func=AF.Exp, scale=-1.0),
                 reads=[an("la")], writes=[tn("e1")])
            p.op("act", lambda e, e1=e1, sp_=sp_: e.activation(out=sp_, in_=e1, func=AF.Ln, bias=1.0),
                 reads=[tn("e1")], writes=[tn("sp")])
            p.mm(BT_ps, sp_, C["tri"][:], True, True, reads=[tn("sp"), "c_tri"], writes=[an("BT")])
            p.mm(blT_ps, sp_, C["ones"][0:64, 0:2], True, True, reads=[tn("sp"), "c_ones"], writes=[an("blT")])
            p.mm(Bm_ps, C["tri"][:], sp_, True, False, reads=[tn("sp"), "c_tri"], writes=[an("Bm")])
            p.mm(Bm_ps, C["negones"][:], sp_, False, True, reads=[tn("sp"), "c_negones"], writes=[an("Bm")])
            p.op("act", lambda e, E1T=E1T, BT_ps=BT_ps: e.activation(out=E1T, in_=BT_ps, func=AF.Exp, scale=-1.0 / 16),
                 reads=[an("BT")], writes=[tn("E1T")])
            p.op("act", lambda e, E2T=E2T, BT_ps=BT_ps: e.activation(out=E2T, in_=BT_ps, func=AF.Exp, scale=1.0 / 16),
                 reads=[an("BT")], writes=[tn("E2T")])
            p.op("act", lambda e, E3=E3, Bm_ps=Bm_ps: e.activation(out=E3, in_=Bm_ps, func=AF.Exp, scale=1.0 / 16),
                 reads=[an("Bm")], writes=[tn("E3")])
            p.op("act", lambda e, dec=dec, blT_ps=blT_ps: e.activation(out=dec, in_=blT_ps[:, 0:1], func=AF.Exp, scale=-1.0 / 16),
                 reads=[an("blT")], writes=[tn("dec")])
            p.op("dve", lambda e, qdT=qdT, E1T=E1T, cs=cs: e.scalar_tensor_tensor(
                out=qdT, in0=qT[:, cs], scalar=0.125, in1=E1T, op0=ALU.mult, op1=ALU.mult),
                reads=["qT", tn("E1T")], writes=[tn("qdT")])
            p.op("dve", lambda e, kdT=kdT, E2T=E2T, cs=cs: e.tensor_tensor(out=kdT, in0=kT[:, cs], in1=E2T, op=ALU.mult),
                 reads=["kT", tn("E2T")], writes=[tn("kdT")])
            p.op("dve", lambda e, kg=kg, E3=E3, n=n: e.tensor_tensor(out=kg, in0=kt[:, n, :], in1=E3, op=ALU.mult),
                 reads=["kt", tn("E3")], writes=[tn("kg")])
            p.mm(att_ps, kdT, qdT, True, True, reads=[tn("kdT"), tn("qdT")], writes=[an("att")])
            p.op("dve", lambda e, attT=attT, att_ps=att_ps: e.tensor_tensor(out=attT, in0=att_ps, in1=C["tri"][:], op=ALU.mult),
                 reads=[an("att"), "c_tri"], writes=[tn("attT")])
            p.mm(o_ps, attT, vt[:, n, :], True, False, reads=[tn("attT"), "vt"], writes=[bn("o")])
            p.mm(o_ps, qdT, Sc[:], False, True, reads=[tn("qdT"), Scn], writes=[bn("o")])
            p.op("act", lambda e, o_ps=o_ps, n=n: e.copy(out=ost[:, n, :], in_=o_ps), reads=[bn("o")], writes=["ost"])
            p.mm(u_ps, kg, vt[:, n, :], True, True, reads=[tn("kg"), "vt"], writes=[bn("u")])
            p.op("dve", lambda e, Sn_=Sn_, Sc=Sc, dec=dec, u_ps=u_ps: e.scalar_tensor_tensor(
                out=Sn_[:], in0=Sc[:], scalar=dec, in1=u_ps, op0=ALU.mult, op1=ALU.add),
                reads=[Scn, tn("dec"), bn("u")], writes=[Snn])
        p.dma("sp", o_d[z], ost[:], reads=["ost"], key="o_out", is_output=True)
    return p.finish()


def build_gdn(TC, TL, NI):
    T = TC + TL
    NCH = T // 64
    p = Prog()
    x_d = p.dram("x", [NI, 3, 128, T], F32, "ExternalInput")
    cw_d = p.dram("cw", [NI, 128, 3, 5], F32, "ExternalInput")
    ba_d = p.dram("ba", [NI, 64, 2, NCH], F32, "ExternalInput")
    sc_d = p.dram("sc", [NI, 64, 2], F32, "ExternalInput")
    o_d = p.dram("o", [NI, 64, NCH, 128], F32, "ExternalOutput")
    C = make_consts64(p)
    ones, negones, tri, ident, lowI, lowS = C["ones"], C["negones"], C["tri"], C["ident"], C["lowI"], C["lowS"]
    X = [p.sb("X%d" % i, [128, T]) for i in range(3)]
    Y = [p.sb("Y%d" % i, [128, T]) for i in range(3)]
    cw = p.sb("cw", [128, 3, 5])
    ba = p.sb("ba", [64, 2, NCH])
    sc = p.sb("sc", [64, 2])
    beta = p.sb("beta", [64, NCH])
    nbeta = p.sb("nbeta", [64, NCH])
    graw = p.sb("graw", [64, NCH + 1])
    gtmp = p.sb("gtmp", [64, NCH])
    ea = p.sb("ea", [64, 1])
    ost = p.sb("ost", [64, NCH, 128])
    S = [p.sb("S%d" % i, [128, 128]) for i in range(2)]
    NR = 2
    t64 = [p.sb("t64_%d" % i, [64, 1424]) for i in range(NR)]
    t128 = [p.sb("t128_%d" % i, [128, 200]) for i in range(NR)]
    bT = p.ps("bT", [128, 512])
    bG = p.ps("bG", [128, 512])
    bK = p.ps("bK", [128, 512])
    bX = p.ps("bX", [128, 512])
    bSq = p.ps("bSq", [128, 512])
    bW = p.ps("bW", [128, 512])
    bV = p.ps("bV", [128, 512])
    bO = p.ps("bO", [128, 512])
    segs = [(0, TC), (TC, T)]
    for i in range(NI):
        for j in range(3):
            p.dma("sp", X[j][:], x_d[i, j], writes=["X%d" % j])
        p.dma("sp", cw[:], cw_d[i], writes=["cw"])
        p.dma("sp", ba[:], ba_d[i], writes=["ba"])
        p.dma("sp", sc[:], sc_d[i], writes=["sc"])
        for j in range(3):
            xj, yj, xn, yn = X[j], Y[j], "X%d" % j, "Y%d" % j
            p.op("dve", lambda e, xj=xj, yj=yj, j=j: e.tensor_scalar(out=yj[:], in0=xj[:], scalar1=cw[:, j, 2:3], scalar2=None,
                                                                    op0=ALU.mult), reads=[xn, "cw"], writes=[yn])
            for k in (0, 1, 3, 4):
                sh = k - 2
                for (a, b) in segs:
                    d0, d1 = max(a, a - sh), min(b, b - sh)
                    if d1 <= d0:
                        continue
                    p.op("dve", lambda e, xj=xj, yj=yj, j=j, k=k, d0=d0, d1=d1, sh=sh: e.scalar_tensor_tensor(
                        out=yj[:, d0:d1], in0=xj[:, d0 + sh:d1 + sh], scalar=cw[:, j, k:k + 1], in1=yj[:, d0:d1],
                        op0=ALU.mult, op1=ALU.add), reads=[xn, yn, "cw"], writes=[yn])
            p.op("act", lambda e, yj=yj: e.activation(out=yj[:], in_=yj[:], func=AF.Silu), reads=[yn], writes=[yn])
        for j in range(2):
            yj, yn, sq, sqn = Y[j], "Y%d" % j, X[j], "X%d" % j
            scale = (128.0 ** -0.5) if j == 0 else 1.0
            p.op("act", lambda e, yj=yj, sq=sq: e.activation(out=sq[:], in_=yj[:], func=AF.Square), reads=[yn], writes=[sqn])
            for c0 in range(0, T, 512):
                c1 = min(T, c0 + 512)
                w_ = c1 - c0
                p.mm(bT[:, 0:w_], ones[:, :], sq[:, c0:c1], True, True, reads=["c_ones", sqn], writes=["bT"])
                p.op("dve", lambda e, sq=sq, c0=c0, c1=c1, w_=w_: e.tensor_scalar(out=sq[:, c0:c1], in0=bT[:, 0:w_], scalar1=1e-6,
                                                                                 scalar2=None, op0=ALU.add),
                     reads=["bT"], writes=[sqn])
            p.op("act", lambda e, sq=sq: e.activation(out=sq[:], in_=sq[:], func=AF.Sqrt), reads=[sqn], writes=[sqn])
            p.op("dve", lambda e, sq=sq: e.reciprocal(out=sq[:], in_=sq[:]), reads=[sqn], writes=[sqn])
            p.op("dve", lambda e, yj=yj, sq=sq, scale=scale: e.scalar_tensor_tensor(
                out=yj[:], in0=yj[:], scalar=scale, in1=sq[:], op0=ALU.mult, op1=ALU.mult), reads=[yn, sqn], writes=[yn])
        p.op("act", lambda e: e.activation(out=beta[:], in_=ba[:, 0, :], func=AF.Sigmoid), reads=["ba"], writes=["beta"])
        p.op("dve", lambda e: e.tensor_scalar(out=nbeta[:], in0=beta[:], scalar1=-1.0, scalar2=None, op0=ALU.mult),
             reads=["beta"], writes=["nbeta"])
        p.op("act", lambda e: e.activation(out=gtmp[:], in_=ba[:, 1, :], func=AF.Exp, bias=sc[:, 1:2]), reads=["ba", "sc"], writes=["gtmp"])
        p.op("act", lambda e: e.activation(out=gtmp[:], in_=gtmp[:], func=AF.Ln, bias=1.0), reads=["gtmp"], writes=["gtmp"])
        p.op("act", lambda e: e.activation(out=ea[:], in_=sc[:, 0:1], func=AF.Exp), reads=["sc"], writes=["ea"])
        p.op("dve", lambda e: e.memset(graw[:], 0.0), writes=["graw"])
        p.op("dve", lambda e: e.tensor_scalar(out=graw[:, 0:NCH], in0=gtmp[:], scalar1=ea[:, 0:1], scalar2=-1.0, op0=ALU.mult, op1=ALU.mult),
             reads=["gtmp", "ea"], writes=["graw"])
        p.op("dve", lambda e: e.memset(S[0][:], 0.0), writes=["S0"])
        for n in range(NCH):
            r = n % NR
            a_, b_ = t64[r], t128[r]
            tn = lambda k, r=r: "t%d_%s" % (r, k)
            W1, ngd, Dm0, Dm, Dms, aqk, AT = [a_[:, 64 * q:64 * (q + 1)] for q in range(7)]
            XY = [a_[:, 448:576], a_[:, 576:704]]
            PP = [a_[:, 704:768], a_[:, 768:832]]
            kbgn, kd, vb, vnew = a_[:, 832:960], a_[:, 960:1088], a_[:, 1088:1216], a_[:, 1216:1344]
            gcol_sb, eg, ekd = a_[:, 1344:1345], a_[:, 1345:1346], a_[:, 1346:1347]
            egrow, qgT, wTn, egl = b_[:, 0:64], b_[:, 64:128], b_[:, 128:192], b_[:, 192:193]
            cs = slice(n * 64, (n + 1) * 64)
            qTn, kTn, vTn = Y[0][:, cs], Y[1][:, cs], Y[2][:, cs]
            gr2 = graw[:, n:n + 2]
            Sc, Sn_ = S[n % 2], S[(n + 1) % 2]
            Scn, Snn = "S%d" % (n % 2), "S%d" % ((n + 1) % 2)
            p.op("pe", lambda e, kTn=kTn: e.transpose(out=bT[0:64, 0:128], in_=kTn, identity=ident[:, :]),
                 reads=["Y1", "c_ident"], writes=["bT"], inc=False)
            p.op("pe", lambda e, vTn=vTn: e.transpose(out=bT[0:64, 128:256], in_=vTn, identity=ident[:, :]),
                 reads=["Y2", "c_ident"], writes=["bT"])
            p.mm(bG[0:64, 0:2], tri[:], gr2, True, True, reads=["c_tri", "graw"], writes=["bG"], inc=False)
            p.mm(bG[0:64, 2:4], tri[:], gr2, True, False, reads=["c_tri", "graw"], writes=["bG"], inc=False)
            p.mm(bG[0:64, 2:4], negones[:], gr2, False, True, reads=["c_negones", "graw"], writes=["bG"], inc=False)
            p.mm(bG[0:128, 4:6], ones[0:64, 0:128], gr2, True, True, reads=["c_ones", "graw"], writes=["bG"], inc=False)
            p.op("dve", lambda e, W1=W1, n=n: e.tensor_scalar(out=W1, in0=tri[:], scalar1=graw[:, n:n + 1], scalar2=None, op0=ALU.mult),
                 reads=["c_tri", "graw"], writes=[tn("W1")])
            p.mm(bG[0:64, 64:128], ones[0:64, 0:64], W1, True, True, reads=["c_ones", tn("W1")], writes=["bG"], inc=False)
            p.mm(bG[0:128, 128:192], ones[0:64, 0:128], W1, True, True, reads=["c_ones", tn("W1")], writes=["bG"])
            p.op("act", lambda e, gcol_sb=gcol_sb: e.copy(out=gcol_sb, in_=bG[0:64, 0:1]), reads=["bG"], writes=[tn("gcol")])
            p.op("act", lambda e, eg=eg: e.activation(out=eg, in_=bG[0:64, 0:1], func=AF.Exp), reads=["bG"], writes=[tn("eg")])
            p.op("act", lambda e, ekd=ekd: e.activation(out=ekd, in_=bG[0:64, 2:3], func=AF.Exp, scale=-1.0), reads=["bG"], writes=[tn("ekd")])
            p.op("act", lambda e, egl=egl: e.activation(out=egl, in_=bG[0:128, 4:5], func=AF.Exp), reads=["bG"], writes=[tn("egl")])
            p.op("act", lambda e, egrow=egrow: e.activation(out=egrow, in_=bG[0:128, 128:192], func=AF.Exp), reads=["bG"], writes=[tn("egrow")])
            p.op("dve", lambda e, ngd=ngd, gcol_sb=gcol_sb: e.scalar_tensor_tensor(
                out=ngd, in0=bG[0:64, 64:128], scalar=gcol_sb, in1=lowI[:], op0=ALU.subtract, op1=ALU.mult),
                reads=["bG", tn("gcol"), "c_lowI"], writes=[tn("ngd")])
            p.op("act", lambda e, Dm0=Dm0, ngd=ngd: e.activation(out=Dm0, in_=ngd, func=AF.Exp, scale=-1.0), reads=[tn("ngd")], writes=[tn("Dm0")])
            p.op("dve", lambda e, Dm=Dm, Dm0=Dm0: e.tensor_tensor(out=Dm, in0=Dm0, in1=lowI[:], op=ALU.mult), reads=[tn("Dm0"), "c_lowI"], writes=[tn("Dm")])
            p.op("dve", lambda e, Dms=Dms, Dm0=Dm0: e.tensor_tensor(out=Dms, in0=Dm0, in1=lowS[:], op=ALU.mult), reads=[tn("Dm0"), "c_lowS"], writes=[tn("Dms")])
            p.op("dve", lambda e, kbgn=kbgn, eg=eg, n=n: e.tensor_scalar(out=kbgn, in0=bT[0:64, 0:128], scalar1=nbeta[:, n:n + 1], scalar2=eg,
                                                                    op0=ALU.mult, op1=ALU.mult), reads=["bT", "nbeta", tn("eg")], writes=[tn("kbgn")])
            p.op("dve", lambda e, kd=kd, ekd=ekd: e.tensor_scalar(out=kd, in0=bT[0:64, 0:128], scalar1=ekd, scalar2=None, op0=ALU.mult),
                 reads=["bT", tn("ekd")], writes=[tn("kd")])
            p.op("dve", lambda e, vb=vb, n=n: e.tensor_scalar(out=vb, in0=bT[0:64, 128:256], scalar1=beta[:, n:n + 1], scalar2=None, op0=ALU.mult),
                 reads=["bT", "beta"], writes=[tn("vb")])
            p.mm(bK[0:64, 0:64], kTn, kTn, True, True, reads=["Y1"], writes=["bK"], inc=False)
            p.mm(bK[0:64, 64:128], qTn, kTn, True, True, reads=["Y0", "Y1"], writes=["bK"])
            xy0 = XY[0]
            p.op("dve", lambda e, xy0=xy0, Dms=Dms, n=n: e.scalar_tensor_tensor(
                out=xy0[:, 64:128], in0=bK[0:64, 0:64], scalar=nbeta[:, n:n + 1], in1=Dms, op0=ALU.mult, op1=ALU.mult),
                reads=["bK", "nbeta", tn("Dms")], writes=[tn("XY0")])
            p.op("dve", lambda e, aqk=aqk, Dm=Dm: e.tensor_tensor(out=aqk, in0=bK[0:64, 64:128], in1=Dm, op=ALU.mult),
                 reads=["bK", tn("Dm")], writes=[tn("aqk")])
            p.op("pe", lambda e, xy0=xy0: e.transpose(out=bX[0:64, 0:64], in_=xy0[:, 64:128], identity=ident[0:64, 0:64]),
                 reads=[tn("XY0"), "c_ident"], writes=["bX"], inc=False)
            p.op("pe", lambda e, aqk=aqk: e.transpose(out=bX[0:64, 64:128], in_=aqk, identity=ident[0:64, 0:64]),
                 reads=[tn("aqk"), "c_ident"], writes=["bX"])
            p.op("act", lambda e, xy0=xy0: e.copy(out=xy0[:, 0:64], in_=bX[0:64, 0:64]), reads=["bX"], writes=[tn("XY0")])
            p.op("act", lambda e, AT=AT: e.copy(out=AT, in_=bX[0:64, 64:128]), reads=["bX"], writes=[tn("AT")])
            p.op("dve", lambda e, xy0=xy0, P0=PP[0]: e.tensor_tensor(out=P0, in0=xy0[:, 0:64], in1=ident[0:64, 0:64], op=ALU.add),
                 reads=[tn("XY0"), "c_ident"], writes=[tn("P0")])
            for j in range(5):
                xc, xn_ = XY[j % 2], XY[(j + 1) % 2]
                xcn, xnn = tn("XY%d" % (j % 2)), tn("XY%d" % ((j + 1) % 2))
                pc, pn_ = PP[j % 2], PP[(j + 1) % 2]
                pcn, pnn = tn("P%d" % (j % 2)), tn("P%d" % ((j + 1) % 2))
                if j < 4:
                    p.mm(bSq[0:64, 0:64], xc[:, 64:128], xc[:, 0:64], True, True, reads=[xcn], writes=["bSq"], inc=False)
                p.mm(bSq[0:64, 64:128], xc[:, 0:64], xc[:, 64:128], True, True, reads=[xcn], writes=["bSq"])
                lo = 0 if j < 4 else 64
                if j % 2 == 0:
                    p.op("act", lambda e, xn_=xn_, lo=lo: e.copy(out=xn_[:, lo:128], in_=bSq[0:64, lo:128]), reads=["bSq"], writes=[xnn])
                else:
                    p.op("dve", lambda e, xn_=xn_, lo=lo: e.tensor_copy(out=xn_[:, lo:128], in_=bSq[0:64, lo:128]), reads=["bSq"], writes=[xnn])
                p.mm(bSq[0:64, 128:192], ident[0:64, 0:64], pc, True, False, reads=["c_ident", pcn], writes=["bSq"], inc=False)
                p.mm(bSq[0:64, 128:192], xn_[:, 64:128], pc, False, True, reads=[xnn, pcn], writes=["bSq"])
                if j % 2 == 0:
                    p.op("dve", lambda e, pn_=pn_: e.tensor_copy(out=pn_, in_=bSq[0:64, 128:192]), reads=["bSq"], writes=[pnn])
                else:
                    p.op("act", lambda e, pn_=pn_: e.copy(out=pn_, in_=bSq[0:64, 128:192]), reads=["bSq"], writes=[pnn])
            TT, TTn = PP[1], tn("P1")
            p.mm(bW[0:128, 0:64], kbgn, TT, True, True, reads=[tn("kbgn"), TTn], writes=["bW"])
            p.op("act", lambda e, wTn=wTn: e.copy(out=wTn, in_=bW[0:128, 0:64]), reads=["bW"], writes=[tn("wTn")])
            p.mm(bV[0:64, 0:128], TT, vb, True, False, reads=[TTn, tn("vb")], writes=["bV"], inc=False)
            p.mm(bV[0:64, 0:128], wTn, Sc[:], False, True, reads=[tn("wTn"), Scn], writes=["bV"])
            p.op("dve", lambda e, vnew=vnew: e.tensor_copy(out=vnew, in_=bV[0:64, 0:128]), reads=["bV"], writes=[tn("vnew")])
            p.op("dve", lambda e, qgT=qgT, qTn=qTn, egrow=egrow: e.tensor_tensor(out=qgT, in0=qTn, in1=egrow, op=ALU.mult),
                 reads=["Y0", tn("egrow")], writes=[tn("qgT")])
            p.mm(bO[0:64, 0:128], qgT, Sc[:], True, False, reads=[tn("qgT"), Scn], writes=["bO"], inc=False)
            p.mm(bO[0:64, 0:128], AT, vnew, False, True, reads=[tn("AT"), tn("vnew")], writes=["bO"])
            p.op("act", lambda e, n=n: e.copy(out=ost[:, n, :], in_=bO[0:64, 0:128]), reads=["bO"], writes=["ost"])
            p.mm(bW[0:128, 64:192], kd, vnew, True, True, reads=[tn("kd"), tn("vnew")], writes=["bW"])
            p.op("dve", lambda e, Sn_=Sn_, Sc=Sc, egl=egl: e.scalar_tensor_tensor(
                out=Sn_[:], in0=Sc[:], scalar=egl, in1=bW[0:128, 64:192], op0=ALU.mult, op1=ALU.add),
                reads=[Scn, tn("egl"), "bW"], writes=[Snn])
        p.dma("sp", o_d[i], ost[:], reads=["ost"], key="o_out", is_output=True)
    return p.finish()


def build_diff(TQ, TL, TC, NI, need_ctx, lambda_init):
    TK = TC + TL
    NKT = TK // 128
    NKC = TC // 128
    p = Prog()
    q_d = p.dram("q", [NI, 2, 2, 64, TQ], F32, "ExternalInput")
    k_d = p.dram("k", [NI, 2, 2, 64, TL], F32, "ExternalInput")
    kc_d = p.dram("kc", [NI, 2, 64, TC], F32, "ExternalInput")
    qc_d = p.dram("qc", [NI, 2, 64, TC], F32, "ExternalInput")
    v_d = p.dram("v", [NI, 128, NKT, 128], F32, "ExternalInput")
    csq_d = p.dram("csq", [NI, 2, 64, TQ], F32, "ExternalInput")
    csk_d = p.dram("csk", [2, 64, TL], F32, "ExternalInput")
    dl_d = p.dram("dl", [1, 386], F32, "ExternalInput")
    y_d = p.dram("y", [NI, 128, TQ // 128, 128], F32, "ExternalOutput")
    yc_d = p.dram("yc", [NI, 128, max(NKC, 1), 128], F32, "ExternalOutput")
    stA = p.sb("stA", [64, TL])
    stB = p.sb("stB", [64, TL])
    csk = p.sb("csk", [64, 2, TL])
    csq = p.sb("csq", [64, 2, TQ])
    kall = [p.sb("kall%d" % m, [64, TK], BF16) for m in range(2)]
    qr = [p.sb("qr%d" % m, [64, TQ], BF16) for m in range(2)]
    qcb = [p.sb("qcb%d" % m, [64, TC], BF16) for m in range(2)]
    vst = p.sb("vst", [128, NKT, 128])
    vaug = p.sb("vaug", [128, NKT, 129], BF16)
    eT = [p.sb("eT%d" % i, [128, 512], BF16) for i in range(3)]
    osb = [p.sb("osb%d" % m, [128, 4, 129]) for m in range(2)]
    yst = p.sb("yst", [128, TQ // 128, 128])
    ycst = p.sb("ycst", [128, max(NKC, 1), 128])
    dl = p.sb("dl", [128, 386])
    sm = p.sb("sm", [128, 16])
    scr = p.sb("scr", [128, 128])
    osc = p.sb("osc", [128, 128])
    nwd = p.sb("nwd", [128, 128])
    bS = [p.ps("bS%d" % i, [128, 512]) for i in range(2)]
    bA = [p.ps("bA%d" % i, [128, 512]) for i in range(4)]
    p.dma("sp", dl[:], dl_d[0, :].partition_broadcast(128), writes=["dl"])
    p.dma("sp", csk[:], csk_d.rearrange("c d t -> d c t"), writes=["csk"])
    p.op("dve", lambda e: e.scalar_tensor_tensor(out=scr[:, 0:64], in0=dl[:, 0:64], scalar=1.0, in1=dl[:, 64:128],
                                                 op0=ALU.mult, op1=ALU.mult, accum_out=sm[:, 0:1]), reads=["dl"], writes=["scr", "sm"])
    p.op("dve", lambda e: e.scalar_tensor_tensor(out=scr[:, 0:64], in0=dl[:, 128:192], scalar=1.0, in1=dl[:, 192:256],
                                                 op0=ALU.mult, op1=ALU.mult, accum_out=sm[:, 1:2]), reads=["dl"], writes=["scr", "sm"])
    p.op("act", lambda e: e.activation(out=sm[:, 2:4], in_=sm[:, 0:2], func=AF.Exp), reads=["sm"], writes=["sm"])
    p.op("dve", lambda e: e.tensor_tensor(out=sm[:, 4:5], in0=sm[:, 3:4], in1=sm[:, 2:3], op=ALU.subtract), reads=["sm"], writes=["sm"])
    p.op("dve", lambda e: e.tensor_scalar(out=sm[:, 5:6], in0=sm[:, 4:5], scalar1=dl[:, 384:385], scalar2=None, op0=ALU.subtract),
         reads=["sm"], writes=["sm"])
    nlam = sm[:, 5:6]
    p.op("dve", lambda e: e.tensor_scalar(out=nwd[:], in0=dl[:, 256:384], scalar1=dl[:, 385:386], scalar2=None, op0=ALU.mult),
         reads=["dl"], writes=["nwd"])

    def rope(dst, dstn, src_ap, ncol, cs, csn, eng="dve"):
        p.dma("sp", stA[:, 0:ncol], src_ap[0], writes=["stA"])
        p.dma("sp", stB[:, 0:ncol], src_ap[1], writes=["stB"])
        p.op(eng, lambda e: e.tensor_tensor(out=stA[:, 0:ncol], in0=stA[:, 0:ncol], in1=cs[:, 0, 0:ncol], op=ALU.mult),
             reads=["stA", csn], writes=["stA"])
        p.op(eng, lambda e: e.tensor_tensor(out=stB[:, 0:ncol], in0=stB[:, 0:ncol], in1=cs[:, 1, 0:ncol], op=ALU.mult),
             reads=["stB", csn], writes=["stB"])
        p.op(eng, lambda e: e.tensor_tensor(out=dst, in0=stA[:, 0:ncol], in1=stB[:, 0:ncol], op=ALU.add),
             reads=["stA", "stB"], writes=[dstn])

    cnt = {"s": 0}

    def attend(qsrc, qn, nq, nkt, outt, outn, ob):
        nj = nq // 128
        for m in range(2):
            for kt in range(nkt):
                s = cnt["s"]
                cnt["s"] += 1
                bs, bsn = bS[s % 2], "bS%d" % (s % 2)
                et, etn = eT[s % 3], "eT%d" % (s % 3)
                p.mm(bs[:, 0:nq], kall[m][:, kt * 128:(kt + 1) * 128], qsrc[m], True, True,
                     reads=["kall%d" % m, qn[m]], writes=[bsn])
                p.op("act", lambda e, et=et, bs=bs: e.activation(out=et[:, 0:nq], in_=bs[:, 0:nq], func=AF.Exp, scale=0.125),
                     reads=[bsn], writes=[etn])
                for j in range(nj):
                    p.mm(bA[j][:, 0:129], et[:, j * 128:(j + 1) * 128], vaug[:, kt, :], kt == 0, kt == nkt - 1,
                         reads=[etn, "vaug"], writes=["bA%d" % j])
            for j in range(nj):
                p.op("dve", lambda e, m=m, j=j: e.tensor_copy(out=osb[m][:, j, :], in_=bA[j][:, 0:129]),
                     reads=["bA%d" % j], writes=["osb%d" % m])
        for j in range(nj):
            p.op("dve", lambda e, j=j: e.reciprocal(out=sm[:, 6:7], in_=osb[0][:, j, 128:129]), reads=["osb0"], writes=["sm"])
            p.op("dve", lambda e, j=j: e.reciprocal(out=sm[:, 7:8], in_=osb[1][:, j, 128:129]), reads=["osb1"], writes=["sm"])
            p.op("dve", lambda e: e.tensor_tensor(out=sm[:, 8:9], in0=sm[:, 7:8], in1=nlam, op=ALU.mult), reads=["sm"], writes=["sm"])
            p.op("dve", lambda e, j=j: e.tensor_scalar(out=osc[:], in0=osb[0][:, j, 0:128], scalar1=sm[:, 6:7], scalar2=None, op0=ALU.mult),
                 reads=["osb0", "sm"], writes=["osc"])
            p.op("dve", lambda e, j=j: e.scalar_tensor_tensor(out=osc[:], in0=osb[1][:, j, 0:128], scalar=sm[:, 8:9], in1=osc[:],
                                                              op0=ALU.mult, op1=ALU.add), reads=["osb1", "sm", "osc"], writes=["osc"])
            p.op("dve", lambda e: e.scalar_tensor_tensor(out=scr[:], in0=osc[:], scalar=1.0, in1=osc[:], op0=ALU.mult, op1=ALU.mult,
                                                         accum_out=sm[:, 9:10]), reads=["osc"], writes=["scr", "sm"])
            p.op("dve", lambda e: e.tensor_scalar(out=sm[:, 10:11], in0=sm[:, 9:10], scalar1=1.0 / 128.0, scalar2=1e-5, op0=ALU.mult, op1=ALU.add),
                 reads=["sm"], writes=["sm"])
            p.op("act", lambda e: e.activation(out=sm[:, 11:12], in_=sm[:, 10:11], func=AF.Sqrt), reads=["sm"], writes=["sm"])
            p.op("dve", lambda e: e.reciprocal(out=sm[:, 12:13], in_=sm[:, 11:12]), reads=["sm"], writes=["sm"])
            p.op("dve", lambda e, j=j: e.scalar_tensor_tensor(out=outt[:, ob + j, :], in0=osc[:], scalar=sm[:, 12:13], in1=nwd[:],
                                                              op0=ALU.mult, op1=ALU.mult), reads=["osc", "sm", "nwd"], writes=[outn])

    for i in range(NI):
        p.dma("sp", csq[:], csq_d[i].rearrange("c d t -> d c t"), writes=["csq"])
        p.dma("sp", vst[:], v_d[i], writes=["vst"])
        p.op("pool", lambda e: e.memset(vaug[:, :, 128:129], 1.0), writes=["vaug"])
        p.op("pool", lambda e: e.tensor_copy(out=vaug[:, :, 0:128], in_=vst[:]), reads=["vst"], writes=["vaug"])
        for m in range(2):
            rope(kall[m][:, TC:TK], "kall%d" % m, k_d[i, m], TL, csk, "csk")
            p.dma("sp", stA[:, 0:TC], kc_d[i, m], writes=["stA"])
            p.op("dve", lambda e, m=m: e.tensor_copy(out=kall[m][:, 0:TC], in_=stA[:, 0:TC]), reads=["stA"], writes=["kall%d" % m])
            rope(qr[m][:], "qr%d" % m, q_d[i, m], TQ, csq, "csq")
            if need_ctx:
                p.dma("sp", stA[:, 0:TC], qc_d[i, m], writes=["stA"])
                p.op("dve", lambda e, m=m: e.tensor_copy(out=qcb[m][:], in_=stA[:, 0:TC]), reads=["stA"], writes=["qcb%d" % m])
        QB = min(512, TQ)
        for qb in range(TQ // QB):
            attend([qr[m][:, qb * QB:(qb + 1) * QB] for m in range(2)], ["qr0", "qr1"], QB, NKT, yst, "yst", qb * (QB // 128))
        p.dma("sp", y_d[i], yst[:], reads=["yst"], key="y_out", is_output=True)
        if need_ctx:
            attend([qcb[m][:] for m in range(2)], ["qcb0", "qcb1"], TC, NKC, ycst, "ycst", 0)
            p.dma("sp", yc_d[i], ycst[:], reads=["ycst"], key="yc_out", is_output=True)
    return p.finish()


def build_p3(RL, RC):
    R = RL + RC
    p = Prog()
    xr = p.dram("xr", [R, 2048], F32, "ExternalInput")
    go_d = p.dram("go", [2, R, 512], F32, "ExternalInput")
    do_d = p.dram("do", [2, R, 768], F32, "ExternalInput")
    og_d = p.dram("og", [R, 1280], F32, "ExternalInput")
    df_d = p.dram("df", [R, 768], F32, "ExternalInput")
    mT_d = p.dram("mT", [128, 16, 5], F32, "ExternalInput")
    g1_d = p.dram("g1", [2, 2048], F32, "ExternalInput")
    hn_d = p.dram("hn", [1, 256], F32, "ExternalInput")
    wo_d = p.dram("wo", [2048, 2048], F32, "ExternalInput")
    rw_d = p.dram("rw", [2048, 32], F32, "ExternalInput")
    rb_d = p.dram("rb", [1, 32], F32, "ExternalInput")
    xn_d = p.dram("xn", [R, 2048], F32, "ExternalOutput")
    h2T_d = p.dram("h2T", [2048, R], BF16, "ExternalOutput")
    G_d = p.dram("G", [R, 32], F32, "ExternalOutput")
    tiles = row_tiles(RL, RC)
    wo = p.sb("wo", [128, 16, 2048], BF16)
    for j in range(4):
        p.dma("pool", wo[:, :, j * 512:(j + 1) * 512], wo_d[:, j * 512:(j + 1) * 512].rearrange("(kc p) c -> p kc c", p=128),
              writes=["wo"], key="wo%d" % j)
    ng = 2 if RC > 0 else 1
    g1b = [p.sb("g1b%d" % g, [128, 2048]) for g in range(ng)]
    for g in range(ng):
        p.dma("sp", g1b[g][:], g1_d[g, :].partition_broadcast(128), writes=["g1b%d" % g])
    hnb = p.sb("hnb", [128, 256])
    p.dma("sp", hnb[:], hn_d[0, :].partition_broadcast(128), writes=["hnb"])
    rbb = p.sb("rbb", [128, 32])
    p.dma("sp", rbb[:], rb_d[0, :].partition_broadcast(128), writes=["rbb"])
    rwt = p.sb("rwt", [128, 16, 32])
    p.dma("sp", rwt[:], rw_d.rearrange("(kc p) c -> p kc c", p=128), writes=["rwt"])
    mT = p.sb("mT", [128, 16, 5])
    p.dma("sp", mT[:], mT_d, writes=["mT"])
    AT = p.sb("AT2", [128, 2, 16])
    for g in range(ng):
        p.op("dve", lambda e, g=g: e.scalar_tensor_tensor(out=AT[:, g, :], in0=mT[:, :, 1 + 2 * g], scalar=1.0, in1=mT[:, :, 0],
                                                          op0=ALU.add, op1=ALU.mult), reads=["mT"], writes=["AT2"])
    identf = p.sb("identf", [128, 128])
    identb = p.sb("identb", [128, 128], BF16)
    onesf = p.sb("onesf", [128, 128])
    p.op("pool", lambda e: e.memset(onesf[:], 1.0), writes=["onesf"])
    p.op("pool", lambda e: e.affine_select(out=identf[:], in_=onesf[:], pattern=[[-1, 128]], compare_op=ALU.is_equal,
                                           fill=0.0, base=0, channel_multiplier=1), reads=["onesf"], writes=["identf"])
    p.op("pool", lambda e: e.tensor_copy(out=identb[:], in_=identf[:]), reads=["identf"], writes=["identb"])
    xt = [p.sb("xt%d" % i, [128, 2048]) for i in range(2)]
    xn = [p.sb("xn%d" % i, [128, 2048]) for i in range(2)]
    sqs = p.sb("sqs", [128, 2048], BF16)
    yn = p.sb("yn", [128, 2048])
    h2T = p.sb("h2T", [128, 16, 128])
    h2Tb = p.sb("h2Tb", [128, 16, 128], BF16)
    go = p.sb("go", [128, 2, 512])
    do = p.sb("do", [128, 2, 768])
    ogt = p.sb("ogt", [128, 1280])
    dft = p.sb("dft", [128, 768])
    osq = p.sb("osq", [128, 1280])
    ymix = p.sb("ymix", [128, 2048], BF16)
    ymT = p.sb("ymT", [128, 16, 128], BF16)
    sm = p.sb("sm", [128, 64])
    lg = p.sb("lg", [128, 32])
    ex = p.sb("ex", [128, 32])
    mk = p.sb("mk", [128, 32])
    Gt = p.sb("Gt", [128, 32])
    pT = [p.ps("pT%d" % i, [128, 4, 128], BF16) for i in range(2)]
    pacc = [p.ps("pacc%d" % i, [128, 512]) for i in range(2)]
    pF = [p.ps("pF%d" % i, [128, 4, 128]) for i in range(2)]
    pR = p.ps("pR", [128, 512])
    ev = 0
    for ti, (r0, n, g) in enumerate(tiles):
        x_, xnm = xt[ti % 2], "xt%d" % (ti % 2)
        xo, xon = xn[ti % 2], "xn%d" % (ti % 2)
        rs = slice(r0, r0 + n)
        p.dma("sp", x_[0:n, :], xr[rs, :], writes=[xnm])
        for z in range(2):
            p.dma("sp", go[0:n, z, :], go_d[z, rs, :], writes=["go"])
            p.dma("sp", do[0:n, z, :], do_d[z, rs, :], writes=["do"])
        p.dma("sp", ogt[0:n, :], og_d[rs, :], writes=["ogt"])
        p.dma("sp", dft[0:n, :], df_d[rs, :], writes=["dft"])
        p.op("act", lambda e, n=n: e.activation(out=ogt[0:n, :], in_=ogt[0:n, :], func=AF.Silu), reads=["ogt"], writes=["ogt"])
        p.op("dve", lambda e, n=n: e.tensor_tensor(out=go[0:n, 0, :], in0=go[0:n, 0, :], in1=go[0:n, 1, :], op=ALU.add), reads=["go"], writes=["go"])
        p.op("dve", lambda e, n=n: e.tensor_tensor(out=do[0:n, 0, :], in0=do[0:n, 0, :], in1=do[0:n, 1, :], op=ALU.add), reads=["do"], writes=["do"])
        p.op("dve", lambda e, n=n: e.tensor_tensor(out=osq[0:n, 0:512], in0=go[0:n, 0, :], in1=go[0:n, 0, :], op=ALU.mult), reads=["go"], writes=["osq"])
        p.op("dve", lambda e, n=n: e.tensor_tensor(out=osq[0:n, 512:1280], in0=do[0:n, 0, :], in1=do[0:n, 0, :], op=ALU.mult), reads=["do"], writes=["osq"])
        p.op("dve", lambda e, n=n: e.tensor_reduce(out=sm[0:n, 0:10], in_=osq[0:n, :].rearrange("p (h d) -> p h d", d=128), axis=AX.X, op=ALU.add),
             reads=["osq"], writes=["sm"])
        p.op("dve", lambda e, n=n: e.tensor_scalar(out=sm[0:n, 10:20], in0=sm[0:n, 0:10], scalar1=1.0 / 128.0, scalar2=EPS, op0=ALU.mult, op1=ALU.add),
             reads=["sm"], writes=["sm"])
        p.op("act", lambda e, n=n: e.activation(out=sm[0:n, 0:10], in_=sm[0:n, 10:20], func=AF.Sqrt), reads=["sm"], writes=["sm"])
        p.op("dve", lambda e, n=n: e.reciprocal(out=sm[0:n, 10:20], in_=sm[0:n, 0:10]), reads=["sm"], writes=["sm"])
        for h in range(10):
            src = go[0:n, 0, h * 128:(h + 1) * 128] if h < 4 else do[0:n, 0, (h - 4) * 128:(h - 3) * 128]
            nwc = hnb[0:n, 0:128] if h < 4 else hnb[0:n, 128:256]
            p.op("dve", lambda e, n=n, h=h, src=src, nwc=nwc: e.scalar_tensor_tensor(
                out=osq[0:n, h * 128:(h + 1) * 128], in0=src, scalar=sm[0:n, 10 + h:11 + h], in1=nwc, op0=ALU.mult, op1=ALU.mult),
                reads=["go", "do", "sm", "hnb"], writes=["osq"])
        p.op("dve", lambda e, n=n: e.tensor_tensor(out=ymix[0:n, 0:1280], in0=osq[0:n, :], in1=ogt[0:n, :], op=ALU.mult),
             reads=["osq", "ogt"], writes=["ymix"])
        p.op("pool", lambda e, n=n: e.tensor_copy(out=ymix[0:n, 1280:2048], in_=dft[0:n, :]), reads=["dft"], writes=["ymix"])
        for k4 in range(4):
            pt, pn = pT[k4 % 2], "pT%d" % (k4 % 2)
            for kk in range(4):
                kc = k4 * 4 + kk
                p.op("pe", lambda e, pt=pt, kk=kk, kc=kc, n=n: e.transpose(out=pt[:, kk, 0:n], in_=ymix[0:n, kc * 128:(kc + 1) * 128],
                                                                         identity=identb[0:n, 0:n]),
                     reads=["ymix", "identb"], writes=[pn], inc=(kk == 3))
            if k4 % 2 == 0:
                p.op("act", lambda e, pt=pt, k4=k4, n=n: e.copy(out=ymT[:, k4 * 4:(k4 + 1) * 4, 0:n], in_=pt[:, :, 0:n]), reads=[pn], writes=["ymT"])
            else:
                p.op("dve", lambda e, pt=pt, k4=k4, n=n: e.tensor_copy(out=ymT[:, k4 * 4:(k4 + 1) * 4, 0:n], in_=pt[:, :, 0:n]), reads=[pn], writes=["ymT"])
        for j in range(4):
            pa, pan = pacc[ev % 2], "pacc%d" % (ev % 2)
            ev += 1
            cs = slice(j * 512, (j + 1) * 512)
            for kc in range(16):
                p.mm(pa[0:n, :], ymT[:, kc, 0:n], wo[:, kc, cs], kc == 0, kc == 15, reads=["ymT", "wo"], writes=[pan])
            p.op("dve", lambda e, pa=pa, xo=xo, n=n, cs=cs, g=g: e.tensor_tensor(out=xo[0:n, cs], in0=pa[0:n, :], in1=g1b[g][0:n, cs], op=ALU.mult),
                 reads=[pan, "g1b%d" % g], writes=[xon])
            p.op("pool", lambda e, xo=xo, x_=x_, n=n, cs=cs: e.tensor_tensor(out=xo[0:n, cs], in0=xo[0:n, cs], in1=x_[0:n, cs], op=ALU.add),
                 reads=[xon, xnm], writes=[xon])
        p.dma("sp", xn_d[rs, :], xo[0:n, :], reads=[xon], key=xon + "_o", is_output=True)
        p.op("act", lambda e, xo=xo, n=n: e.activation(out=sqs[0:n, :], in_=xo[0:n, :], func=AF.Square, accum_out=sm[0:n, 20:21]),
             reads=[xon], writes=["sqs", "sm"])
        p.op("dve", lambda e, n=n: e.tensor_scalar(out=sm[0:n, 21:22], in0=sm[0:n, 20:21], scalar1=1.0 / 2048.0, scalar2=EPS, op0=ALU.mult, op1=ALU.add),
             reads=["sm"], writes=["sm"])
        p.op("act", lambda e, n=n: e.activation(out=sm[0:n, 22:23], in_=sm[0:n, 21:22], func=AF.Sqrt), reads=["sm"], writes=["sm"])
        p.op("dve", lambda e, n=n: e.reciprocal(out=sm[0:n, 23:24], in_=sm[0:n, 22:23]), reads=["sm"], writes=["sm"])
        p.op("dve", lambda e, xo=xo, n=n: e.tensor_scalar(out=yn[0:n, :], in0=xo[0:n, :], scalar1=sm[0:n, 23:24], scalar2=None, op0=ALU.mult),
             reads=[xon, "sm"], writes=["yn"])
        for k4 in range(4):
            pf, pfn = pF[k4 % 2], "pF%d" % (k4 % 2)
            for kk in range(4):
                kc = k4 * 4 + kk
                p.op("pe", lambda e, pf=pf, kk=kk, kc=kc, n=n: e.transpose(out=pf[:, kk, 0:n], in_=yn[0:n, kc * 128:(kc + 1) * 128],
                                                                         identity=identf[0:n, 0:n]),
                     reads=["yn", "identf"], writes=[pfn], inc=(kk == 3))
            for kk in range(4):
                kc = k4 * 4 + kk
                p.op("dve", lambda e, pf=pf, kk=kk, kc=kc, n=n, g=g: e.tensor_scalar(
                    out=h2T[:, kc, 0:n], in0=pf[:, kk, 0:n], scalar1=AT[:, g, kc:kc + 1], scalar2=mT[:, kc, 2 + 2 * g:3 + 2 * g],
                    op0=ALU.mult, op1=ALU.add), reads=[pfn, "AT2", "mT"], writes=["h2T"])
        p.op("act", lambda e, n=n: e.copy(out=h2Tb[:, :, 0:n], in_=h2T[:, :, 0:n]), reads=["h2T"], writes=["h2Tb"])
        p.dma("sp", h2T_d[:, rs].rearrange("(kc p) r -> p kc r", p=128), h2Tb[:, :, 0:n], reads=["h2Tb"], key="h2Tb_o", is_output=True)
        for kc in range(16):
            p.mm(pR[0:n, 0:32], h2T[:, kc, 0:n], rwt[:, kc, :], kc == 0, kc == 15, reads=["h2T", "rwt"], writes=["pR"])
        p.op("dve", lambda e, n=n: e.tensor_tensor(out=lg[0:n, :], in0=pR[0:n, 0:32], in1=rbb[0:n, :], op=ALU.add), reads=["pR", "rbb"], writes=["lg"])
        p.op("dve", lambda e, n=n: e.max(out=sm[0:n, 24:32], in_=lg[0:n, :]), reads=["lg"], writes=["sm"])
        p.op("dve", lambda e, n=n: e.tensor_scalar(out=sm[0:n, 32:33], in0=sm[0:n, 24:25], scalar1=-1.0, scalar2=None, op0=ALU.mult), reads=["sm"], writes=["sm"])
        p.op("dve", lambda e, n=n: e.tensor_scalar(out=mk[0:n, :], in0=lg[0:n, :], scalar1=sm[0:n, 27:28], scalar2=None, op0=ALU.is_ge),
             reads=["lg", "sm"], writes=["mk"])
        p.op("act", lambda e, n=n: e.activation(out=ex[0:n, :], in_=lg[0:n, :], func=AF.Exp, bias=sm[0:n, 32:33]), reads=["lg", "sm"], writes=["ex"])
        p.op("dve", lambda e, n=n: e.scalar_tensor_tensor(out=ex[0:n, :], in0=ex[0:n, :], scalar=1.0, in1=mk[0:n, :], op0=ALU.mult, op1=ALU.mult,
                                                          accum_out=sm[0:n, 33:34]), reads=["ex", "mk"], writes=["ex", "sm"])
        p.op("dve", lambda e, n=n: e.reciprocal(out=sm[0:n, 34:35], in_=sm[0:n, 33:34]), reads=["sm"], writes=["sm"])
        p.op("dve", lambda e, n=n: e.tensor_scalar(out=Gt[0:n, :], in0=ex[0:n, :], scalar1=sm[0:n, 34:35], scalar2=None, op0=ALU.mult),
             reads=["ex", "sm"], writes=["Gt"])
        p.dma("sp", G_d[rs, :], Gt[0:n, :], reads=["Gt"], key="Gt_o", is_output=True)
    return p.finish()


def build_p4(NT, TB=1024, NE=4, FA=2048):
    NJ = FA // 128
    p = Prog()
    h2T_d = p.dram("h2T", [2048, NT], BF16, "ExternalInput")
    Gl_d = p.dram("Gl", [NT, NE], F32, "ExternalInput")
    GlT_d = p.dram("GlT", [NE, NT], F32, "ExternalInput")
    w1_d = p.dram("w1", [NE, 2048, 2 * FA], F32, "ExternalInput")
    b1_d = p.dram("b1c", [128, NE, 2 * NJ], F32, "ExternalInput")
    w2_d = p.dram("w2", [NE, FA, 2048], F32, "ExternalInput")
    b2_d = p.dram("b2", [NE, 2048], F32, "ExternalInput")
    part_d = p.dram("part", [NT, 2048], F32, "ExternalOutput")
    blocks = [(t0, min(TB, NT - t0)) for t0 in range(0, NT, TB)]
    hT = p.sb("hT", [128, 16, TB], BF16)
    actT = p.sb("actT", [128, NJ, TB], BF16)
    acc = p.sb("acc", [128, TB // 128, 2048])
    wt1 = [p.sb("wt1_%d" % i, [128, 16, 256], BF16) for i in range(2)]
    wt2 = [p.sb("wt2_%d" % i, [128, NJ, 256], BF16) for i in range(2)]
    NRT = 2
    tg = [p.sb("tg%d" % i, [128, 512]) for i in range(NRT)]
    tl = [p.sb("tl%d" % i, [128, 512]) for i in range(NRT)]
    ts_ = [p.sb("ts%d" % i, [128, 512], BF16) for i in range(NRT)]
    tt_ = [p.sb("tt%d" % i, [128, 512], BF16) for i in range(NRT)]
    glt = p.sb("glt", [128, TB // 128, NE])
    glT = p.sb("glT", [NE, TB])
    b1c = p.sb("b1c", [128, NE, 2 * NJ])
    b2s = p.sb("b2s", [NE, 2048])
    pG = [p.ps("pG%d" % i, [128, 512]) for i in range(2)]
    pL = [p.ps("pL%d" % i, [128, 512]) for i in range(2)]
    pY = [p.ps("pY%d" % i, [128, 512]) for i in range(2)]
    pB = p.ps("pB", [128, 512])
    p.dma("sp", b1c[:], b1_d, writes=["b1c"])
    p.dma("sp", b2s[:], b2_d, writes=["b2s"])
    p.op("dve", lambda e: e.tensor_scalar(out=b1c[:, :, 1::2], in0=b1c[:, :, 1::2], scalar1=1.0, scalar2=None, op0=ALU.add),
         reads=["b1c"], writes=["b1c"])
    w1i = 0
    w2i = 0
    ev = 0
    ri = 0
    w1_tasks = [(ex, j) for _ in blocks for ex in range(NE) for j in range(NJ)]
    w2_tasks = [(ex, dt) for _ in blocks for ex in range(NE) for dt in range(8)]
    issued = {"w1": 0, "w2": 0}

    def issue_w1(k):
        if k != issued["w1"] or k >= len(w1_tasks):
            return
        ex, j = w1_tasks[k]
        p.dma("pool", wt1[k % 2][:], w1_d[ex][:, j * 256:(j + 1) * 256].rearrange("(kc p) c -> p kc c", p=128),
              writes=["wt1_%d" % (k % 2)])
        issued["w1"] += 1

    def issue_w2(k):
        if k != issued["w2"] or k >= len(w2_tasks):
            return
        ex, dt = w2_tasks[k]
        p.dma("pool", wt2[k % 2][:], w2_d[ex][:, dt * 256:(dt + 1) * 256].rearrange("(fc p) c -> p fc c", p=128),
              writes=["wt2_%d" % (k % 2)])
        issued["w2"] += 1

    issue_w1(0)
    issue_w2(0)
    for (t0, TBn) in blocks:
        ntile = TBn // 128
        nN = (TBn + 511) // 512
        p.dma("sp", hT[:, :, 0:TBn], h2T_d[:, t0:t0 + TBn].rearrange("(kc p) t -> p kc t", p=128), writes=["hT"])
        p.dma("sp", glt[:, 0:ntile, :], Gl_d[t0:t0 + TBn, :].rearrange("(t p) e -> p t e", p=128), writes=["glt"],
              allow_slow_non_contiguous=True)
        p.dma("sp", glT[:, 0:TBn], GlT_d[:, t0:t0 + TBn], writes=["glT"])
        for tt in range(ntile):
            for dt in range(4):
                p.mm(pB[:, :], glT[:, tt * 128:(tt + 1) * 128], b2s[:, dt * 512:(dt + 1) * 512], True, True, reads=["glT", "b2s"], writes=["pB"])
                if (tt * 4 + dt) % 2 == 0:
                    p.op("act", lambda e, tt=tt, dt=dt: e.copy(out=acc[:, tt, dt * 512:(dt + 1) * 512], in_=pB[:, :]), reads=["pB"], writes=["acc"])
                else:
                    p.op("dve", lambda e, tt=tt, dt=dt: e.tensor_copy(out=acc[:, tt, dt * 512:(dt + 1) * 512], in_=pB[:, :]), reads=["pB"], writes=["acc"])
        for ex in range(NE):
            for j in range(NJ):
                w_, wn = wt1[w1i % 2], "wt1_%d" % (w1i % 2)
                assert w1_tasks[w1i] == (ex, j)
                issue_w1(w1i)
                issue_w1(w1i + 1)
                w1i += 1
                for nt in range(nN):
                    c0 = nt * 512
                    cw = min(512, TBn - c0)
                    k = ev % 2
                    ev += 1
                    g_ps, gn = pG[k], "pG%d" % k
                    l_ps, ln = pL[k], "pL%d" % k
                    for kc in range(16):
                        p.mm(g_ps[:, 0:cw], w_[:, kc, 0:128], hT[:, kc, c0:c0 + cw], kc == 0, kc == 15, reads=[wn, "hT"], writes=[gn])
                    for kc in range(16):
                        p.mm(l_ps[:, 0:cw], w_[:, kc, 128:256], hT[:, kc, c0:c0 + cw], kc == 0, kc == 15, reads=[wn, "hT"], writes=[ln])
                    r = ri % NRT
                    ri += 1
                    g_, l_, s_, t_ = tg[r], tl[r], ts_[r], tt_[r]
                    p.op("dve", lambda e, g_=g_, g_ps=g_ps, ex=ex, j=j, cw=cw: e.tensor_scalar(
                        out=g_[:, 0:cw], in0=g_ps[:, 0:cw], scalar1=b1c[:, ex, 2 * j:2 * j + 1], scalar2=7.0, op0=ALU.add, op1=ALU.min),
                        reads=[gn, "b1c"], writes=["tg%d" % r])
                    p.op("act", lambda e, g_=g_, s_=s_, cw=cw: e.activation(out=s_[:, 0:cw], in_=g_[:, 0:cw], func=AF.Sigmoid, scale=1.702),
                         reads=["tg%d" % r], writes=["ts%d" % r])
                    p.op("dve", lambda e, l_=l_, l_ps=l_ps, ex=ex, j=j, cw=cw: e.tensor_scalar(
                        out=l_[:, 0:cw], in0=l_ps[:, 0:cw], scalar1=b1c[:, ex, 2 * j + 1:2 * j + 2], scalar2=8.0, op0=ALU.add, op1=ALU.min),
                        reads=[ln, "b1c"], writes=["tl%d" % r])
                    p.op("pool", lambda e, g_=g_, s_=s_, t_=t_, cw=cw: e.tensor_tensor(out=t_[:, 0:cw], in0=g_[:, 0:cw], in1=s_[:, 0:cw], op=ALU.mult),
                         reads=["tg%d" % r, "ts%d" % r], writes=["tt%d" % r])
                    p.op("dve", lambda e, l_=l_, t_=t_, j=j, c0=c0, cw=cw: e.scalar_tensor_tensor(
                        out=actT[:, j, c0:c0 + cw], in0=l_[:, 0:cw], scalar=-6.0, in1=t_[:, 0:cw], op0=ALU.max, op1=ALU.mult),
                        reads=["tl%d" % r, "tt%d" % r], writes=["actT"])
            for dt in range(8):
                w_, wn = wt2[w2i % 2], "wt2_%d" % (w2i % 2)
                assert w2_tasks[w2i] == (ex, dt)
                issue_w2(w2i)
                issue_w2(w2i + 1)
                w2i += 1
                for tt in range(ntile):
                    k = ev % 2
                    ev += 1
                    y_ps, yn_ = pY[k], "pY%d" % k
                    for fc in range(NJ):
                        p.mm(y_ps[:, 0:256], actT[:, fc, tt * 128:(tt + 1) * 128], w_[:, fc, :], fc == 0, fc == NJ - 1,
                             reads=["actT", wn], writes=[yn_])
                    p.op("dve", lambda e, y_ps=y_ps, tt=tt, dt=dt, ex=ex: e.scalar_tensor_tensor(
                        out=acc[:, tt, dt * 256:(dt + 1) * 256], in0=y_ps[:, 0:256], scalar=glt[:, tt, ex:ex + 1],
                        in1=acc[:, tt, dt * 256:(dt + 1) * 256], op0=ALU.mult, op1=ALU.add),
                        reads=[yn_, "glt", "acc"], writes=["acc"])
        p.dma("sp", part_d[t0:t0 + TBn, :].rearrange("(t p) d -> p t d", p=128), acc[:, 0:ntile, :], reads=["acc"], key="acc_o", is_output=True)
    return p.finish()


def build_p5(RL, RC, final):
    R = RL + RC
    p = Prog()
    xn_d = p.dram("xn", [R, 2048], F32, "ExternalInput")
    pt_d = p.dram("parts", [NCORES, R, 2048], F32, "ExternalInput")
    g2_d = p.dram("g2", [2, 2048], F32, "ExternalInput")
    fw_d = p.dram("fw", [1, 2048], F32, "ExternalInput")
    xo_d = p.dram("xo", [R, 2048], F32, "ExternalOutput")
    tiles = row_tiles(RL, RC)
    ng = 2 if RC > 0 else 1
    g2b = [p.sb("g2b%d" % g, [128, 2048]) for g in range(ng)]
    for g in range(ng):
        p.dma("sp", g2b[g][:], g2_d[g, :].partition_broadcast(128), writes=["g2b%d" % g])
    fwb = p.sb("fwb", [128, 2048])
    p.dma("sp", fwb[:], fw_d[0, :].partition_broadcast(128), writes=["fwb"])
    xt = [p.sb("xt%d" % i, [128, 2048]) for i in range(2)]
    acc = [p.sb("acc%d" % i, [128, 2048]) for i in range(2)]
    pb = [p.sb("pb%d" % i, [128, 2048]) for i in range(4)]
    sqs = p.sb("sqs", [128, 2048], BF16)
    sm = p.sb("sm", [128, 8])
    k = 0
    for ti, (r0, n, g) in enumerate(tiles):
        rs = slice(r0, r0 + n)
        x_, xnm = xt[ti % 2], "xt%d" % (ti % 2)
        a_, an = acc[ti % 2], "acc%d" % (ti % 2)
        p.dma("sp", x_[0:n, :], xn_d[rs, :], writes=[xnm])
        p.dma("sp", a_[0:n, :], pt_d[0, rs, :], writes=[an])
        for c in range(1, NCORES):
            b_, bn = pb[k % 4], "pb%d" % (k % 4)
            k += 1
            p.dma("sp", b_[0:n, :], pt_d[c, rs, :], writes=[bn])
            eng = "dve" if c % 2 == 1 else "pool"
            p.op(eng, lambda e, a_=a_, b_=b_, n=n: e.tensor_tensor(out=a_[0:n, :], in0=a_[0:n, :], in1=b_[0:n, :], op=ALU.add),
                 reads=[an, bn], writes=[an])
        p.op("dve", lambda e, a_=a_, n=n, g=g: e.tensor_tensor(out=a_[0:n, :], in0=a_[0:n, :], in1=g2b[g][0:n, :], op=ALU.mult),
             reads=[an, "g2b%d" % g], writes=[an])
        p.op("pool", lambda e, a_=a_, x_=x_, n=n: e.tensor_tensor(out=a_[0:n, :], in0=a_[0:n, :], in1=x_[0:n, :], op=ALU.add),
             reads=[an, xnm], writes=[an])
        if final:
            p.op("act", lambda e, a_=a_, n=n: e.activation(out=sqs[0:n, :], in_=a_[0:n, :], func=AF.Square, accum_out=sm[0:n, 0:1]),
                 reads=[an], writes=["sqs", "sm"])
            p.op("dve", lambda e, n=n: e.tensor_scalar(out=sm[0:n, 1:2], in0=sm[0:n, 0:1], scalar1=1.0 / 2048.0, scalar2=EPS, op0=ALU.mult, op1=ALU.add),
                 reads=["sm"], writes=["sm"])
            p.op("act", lambda e, n=n: e.activation(out=sm[0:n, 2:3], in_=sm[0:n, 1:2], func=AF.Sqrt), reads=["sm"], writes=["sm"])
            p.op("dve", lambda e, n=n: e.reciprocal(out=sm[0:n, 3:4], in_=sm[0:n, 2:3]), reads=["sm"], writes=["sm"])
            p.op("dve", lambda e, a_=a_, n=n: e.scalar_tensor_tensor(out=a_[0:n, :], in0=a_[0:n, :], scalar=sm[0:n, 3:4], in1=fwb[0:n, :],
                                                                    op0=ALU.mult, op1=ALU.mult), reads=[an, "sm", "fwb"], writes=[an])
        p.dma("sp", xo_d[rs, :], a_[0:n, :], reads=[an], key=an + "_o", is_output=True)
    return p.finish()


B_, S_, L_, D_ = 2, 4096, 256, 2048
T_ = S_ + L_
RL_, RC_ = S_ // 4, L_ // 4
NT_ = NCORES * (RL_ + RC_)
C_GLA_Q, C_GLA_K, C_GLA_V, C_GLA_OG, C_GLA_GLR = 0, 256, 512, 1024, 1536
C_GDN_Q, C_GDN_K, C_GDN_V, C_GDN_OG, C_GDN_BLR, C_GDN_ALR = 1568, 2336, 3104, 3872, 4640, 4652
C_DF_Q, C_DF_K, C_DF_V = 4664, 5432, 6200
NCOL_ = 6968
GRID_W_ = 64
_PERM = np.concatenate([np.arange(16, 32), np.arange(0, 16), np.arange(48, 64), np.arange(32, 48)])

_prog_cache = {}


def _get(name, fn, *args):
    key = (name,) + tuple(args)
    if key not in _prog_cache:
        _prog_cache[key] = fn(*args)
    return _prog_cache[key]


_DBG = {}
_KDEBUG = bool(os.environ.get("KDEBUG"))


def _run(nc, in_maps):
    t0 = time.time()
    res = run_bass_kernel_spmd(nc, in_maps, core_ids=list(range(NCORES)))
    if _KDEBUG:
        print("  launch %.1fs" % (time.time() - t0), flush=True)
    return res.results


def _f32(a):
    return np.ascontiguousarray(a, dtype=np.float32)


def _rope_tables(pos):
    row = (pos // GRID_W_).astype(np.float64)
    col = (pos % GRID_W_).astype(np.float64)
    inv = 1.0 / (10000.0 ** (np.arange(0, 32, 2) / 32.0))
    cos = np.zeros((64, len(pos)))
    sin = np.zeros((64, len(pos)))
    for half, pp in ((0, row), (1, col)):
        ang = pp[None, :] * inv[:, None]
        b = half * 32
        cos[b:b + 16] = np.cos(ang)
        cos[b + 16:b + 32] = np.cos(ang)
        sin[b:b + 16] = -np.sin(ang)
        sin[b + 16:b + 32] = np.sin(ang)
    return np.stack([cos, sin]).astype(np.float32)


def _scan_order(ctx_part, lat_part, z):
    if z == 1:
        ctx_part, lat_part = ctx_part[::-1], lat_part[::-1]
    return np.concatenate([ctx_part, lat_part], axis=0)


def _unscan(o, z):
    oc, ol = o[:L_], o[L_:]
    if z == 1:
        oc, ol = oc[::-1], ol[::-1]
    return oc, ol


def _rows(lat_b, ctx_b, qd):
    return np.concatenate([lat_b[qd * RL_:(qd + 1) * RL_], ctx_b[qd * RC_:(qd + 1) * RC_]], axis=0)


def _tT(v):
    return v.reshape(16, 128).T


def run_layer(l, x, ctx, mod, P, final):
    D = D_
    msl = lambda i: slice(i * D, (i + 1) * D)
    SH1, SC1, G1, SH2, SC2, G2 = [msl(i) for i in range(6)]
    mlat = [mod[b, l] for b in range(B_)]
    mctx = mod[2, l]
    nc = _get("p1", build_p1, RL_, RC_, NCOL_)
    w_in = _f32(P["w_in"][l])
    in_maps = []
    for c in range(NCORES):
        b, qd = c // 4, c % 4
        mvec = np.stack([P["norm1_w"][l], mlat[b][SC1], mlat[b][SH1], mctx[SC1], mctx[SH1]])
        in_maps.append({"xr": _f32(_rows(x[b], ctx[b], qd)), "mvec": _f32(mvec), "w": w_in})
    res = _run(nc, in_maps)
    proj_lat = [np.concatenate([res[4 * b + qd]["proj"][:RL_] for qd in range(4)], 0) for b in range(B_)]
    proj_ctx = [np.concatenate([res[4 * b + qd]["proj"][RL_:] for qd in range(4)], 0) for b in range(B_)]
    del res
    if _KDEBUG:
        _DBG["proj_lat%d" % l] = proj_lat
        _DBG["proj_ctx%d" % l] = proj_ctx
    NCH = T_ // 64
    nc = _get("gla", build_gla, T_)
    in_maps = []
    for c in range(NCORES):
        b, h = c // 4, c % 4
        pl, pc = proj_lat[b], proj_ctx[b]
        qs, ks, vs, gs, gw = [], [], [], [], []
        for z in range(2):
            sl = lambda c0, w: _scan_order(pc[:, c0:c0 + w], pl[:, c0:c0 + w], z)
            qs.append(sl(C_GLA_Q + h * 64, 64))
            ks.append(sl(C_GLA_K + h * 64, 64))
            vs.append(sl(C_GLA_V + h * 128, 128))
            gs.append(sl(C_GLA_GLR + z * 16, 16))
            gw.append(np.concatenate([P["gla_gate_w"][l][z][:, h * 64:(h + 1) * 64], P["gla_gate_b"][l][z][None, h * 64:(h + 1) * 64]], 0))
        q, k, v, glr = np.stack(qs), np.stack(ks), np.stack(vs), np.stack(gs)
        in_maps.append({
            "qT": _f32(q.transpose(0, 2, 1)), "kT": _f32(k.transpose(0, 2, 1)),
            "kt": _f32(k.reshape(2, NCH, 64, 64).transpose(0, 2, 1, 3)),
            "vt": _f32(v.reshape(2, NCH, 64, 128).transpose(0, 2, 1, 3)),
            "glrT": _f32(np.concatenate([glr.transpose(0, 2, 1), np.ones((2, 1, T_), np.float32)], 1)),
            "gwb": _f32(np.stack(gw)),
        })
    res = _run(nc, in_maps)
    gla_lat = np.zeros((B_, 2, S_, 512), np.float32)
    gla_ctx = np.zeros((B_, 2, L_, 512), np.float32)
    for c in range(NCORES):
        b, h = c // 4, c % 4
        o = res[c]["o"].transpose(0, 2, 1, 3).reshape(2, T_, 128)
        for z in range(2):
            oc, ol = _unscan(o[z], z)
            gla_lat[b, z, :, h * 128:(h + 1) * 128] = ol
            gla_ctx[b, z, :, h * 128:(h + 1) * 128] = oc
    del res
    nc = _get("gdn", build_gdn, L_, S_, 1)
    gdn_lat = np.zeros((B_, 2, S_, 768), np.float32)
    gdn_ctx = np.zeros((B_, 2, L_, 768), np.float32)
    for i in range(3):
        in_maps = []
        for c in range(NCORES):
            b = c // 4
            pl, pc = proj_lat[b], proj_ctx[b]
            kk = (c % 4) * 3 + i
            h, z = kk // 2, kk % 2
            sl = lambda c0, w: _scan_order(pc[:, c0:c0 + w], pl[:, c0:c0 + w], z)
            xs = np.stack([sl(C_GDN_Q + h * 128, 128).T, sl(C_GDN_K + h * 128, 128).T, sl(C_GDN_V + h * 128, 128).T])
            cw = np.stack([P["gdn_conv_w"][l][:, j * 768 + h * 128: j * 768 + (h + 1) * 128].T for j in range(3)], 1)
            if z == 1:
                cw = cw[:, :, ::-1]
            blr = sl(C_GDN_BLR + z * 6 + h, 1)[:, 0]
            alr = sl(C_GDN_ALR + z * 6 + h, 1)[:, 0]
            ba = np.stack([blr.reshape(NCH, 64).T, alr.reshape(NCH, 64).T], 1)
            sc = np.stack([np.full(64, P["gdn_a_log"][l][z, h]), np.full(64, P["gdn_dt_bias"][l][z, h])], 1)
            in_maps.append({"x": _f32(xs[None]), "cw": _f32(cw[None]), "ba": _f32(ba[None]), "sc": _f32(sc[None])})
        res = _run(nc, in_maps)
        for c in range(NCORES):
            b = c // 4
            kk = (c % 4) * 3 + i
            h, z = kk // 2, kk % 2
            o = res[c]["o"][0].transpose(1, 0, 2).reshape(T_, 128)
            oc, ol = _unscan(o, z)
            gdn_lat[b, z, :, h * 128:(h + 1) * 128] = ol
            gdn_ctx[b, z, :, h * 128:(h + 1) * 128] = oc
        del res
    TQ = S_ // 2
    NKT = T_ // 128
    nc = _get("diff", build_diff, TQ, S_, L_, 3, True, -1.0)
    lam0 = 0.8 - 0.6 * math.exp(-0.3 * l)
    csk = _rope_tables(np.arange(S_))
    csq = [_rope_tables(np.arange(S_)[hf * TQ:(hf + 1) * TQ]) for hf in range(2)]
    dl = _f32(np.concatenate([P["diff_lambda"][l].reshape(-1), P["diff_norm_w"][l], [lam0, 1.0 - lam0]])[None])
    in_maps = []
    for c in range(NCORES):
        b = c // 4
        pl, pc = proj_lat[b], proj_ctx[b]
        qi, ki, kci, qci, vi, csi = [], [], [], [], [], []
        for i in range(3):
            kk = (c % 4) * 3 + i
            h, hf = kk // 2, kk % 2
            ql = pl[:, C_DF_Q + h * 128: C_DF_Q + (h + 1) * 128].reshape(S_, 2, 64)
            kl = pl[:, C_DF_K + h * 128: C_DF_K + (h + 1) * 128].reshape(S_, 2, 64)
            qc = pc[:, C_DF_Q + h * 128: C_DF_Q + (h + 1) * 128].reshape(L_, 2, 64)
            kc = pc[:, C_DF_K + h * 128: C_DF_K + (h + 1) * 128].reshape(L_, 2, 64)
            vl = pl[:, C_DF_V + h * 128: C_DF_V + (h + 1) * 128]
            vc = pc[:, C_DF_V + h * 128: C_DF_V + (h + 1) * 128]
            qT = ql[hf * TQ:(hf + 1) * TQ].transpose(1, 2, 0)
            kT = kl.transpose(1, 2, 0)
            qi.append(np.stack([qT, qT[:, _PERM, :]], 1))
            ki.append(np.stack([kT, kT[:, _PERM, :]], 1))
            kci.append(kc.transpose(1, 2, 0))
            qci.append(qc.transpose(1, 2, 0))
            vi.append(np.concatenate([vc, vl], 0).reshape(NKT, 128, 128).transpose(1, 0, 2))
            csi.append(csq[hf])
        in_maps.append({"q": _f32(np.stack(qi)), "k": _f32(np.stack(ki)), "kc": _f32(np.stack(kci)), "qc": _f32(np.stack(qci)),
                        "v": _f32(np.stack(vi)), "csq": _f32(np.stack(csi)), "csk": csk, "dl": dl})
    res = _run(nc, in_maps)
    df_lat = np.zeros((B_, S_, 768), np.float32)
    df_ctx = np.zeros((B_, L_, 768), np.float32)
    for c in range(NCORES):
        b = c // 4
        for i in range(3):
            kk = (c % 4) * 3 + i
            h, hf = kk // 2, kk % 2
            y = res[c]["y"][i].transpose(1, 0, 2).reshape(TQ, 128)
            df_lat[b, hf * TQ:(hf + 1) * TQ, h * 128:(h + 1) * 128] = y
            if hf == 0:
                df_ctx[b, :, h * 128:(h + 1) * 128] = res[c]["yc"][i].transpose(1, 0, 2).reshape(L_, 128)
    del res
    if _KDEBUG:
        _DBG["gla_lat%d" % l], _DBG["gla_ctx%d" % l] = gla_lat, gla_ctx
        _DBG["gdn_lat%d" % l], _DBG["gdn_ctx%d" % l] = gdn_lat, gdn_ctx
        _DBG["df_lat%d" % l], _DBG["df_ctx%d" % l] = df_lat, df_ctx
    nc = _get("p3", build_p3, RL_, RC_)
    wo, rw, rb = _f32(P["w_out"][l]), _f32(P["router_w"][l]), _f32(P["router_b"][l][None])
    hn = _f32(np.concatenate([P["gla_norm_w"][l], P["gdn_norm_w"][l]])[None])
    in_maps = []
    for c in range(NCORES):
        b, qd = c // 4, c % 4
        og_l = np.concatenate([proj_lat[b][:, C_GLA_OG:C_GLA_OG + 512], proj_lat[b][:, C_GDN_OG:C_GDN_OG + 768]], 1)
        og_c = np.concatenate([proj_ctx[b][:, C_GLA_OG:C_GLA_OG + 512], proj_ctx[b][:, C_GDN_OG:C_GDN_OG + 768]], 1)
        mT = np.stack([_tT(P["norm2_w"][l]), _tT(mlat[b][SC2]), _tT(mlat[b][SH2]), _tT(mctx[SC2]), _tT(mctx[SH2])], 2)
        in_maps.append({
            "xr": _f32(_rows(x[b], ctx[b], qd)),
            "go": _f32(np.stack([_rows(gla_lat[b, z], gla_ctx[b, z], qd) for z in range(2)])),
            "do": _f32(np.stack([_rows(gdn_lat[b, z], gdn_ctx[b, z], qd) for z in range(2)])),
            "og": _f32(_rows(og_l, og_c, qd)), "df": _f32(_rows(df_lat[b], df_ctx[b], qd)),
            "mT": _f32(mT), "g1": _f32(np.stack([mlat[b][G1], mctx[G1]])), "hn": hn, "wo": wo, "rw": rw, "rb": rb,
        })
    res = _run(nc, in_maps)
    xn = [res[c]["xn"] for c in range(NCORES)]
    h2T = np.concatenate([res[c]["h2T"] for c in range(NCORES)], axis=1)
    G = np.concatenate([res[c]["G"] for c in range(NCORES)], axis=0)
    del res, proj_lat, proj_ctx
    if _KDEBUG:
        _DBG["xn%d" % l], _DBG["h2T%d" % l], _DBG["G%d" % l] = xn, h2T, G
    nc = _get("p4", build_p4, NT_, 1024, 4, 2048)
    in_maps = []
    for c in range(NCORES):
        es = slice(4 * c, 4 * c + 4)
        w1 = P["expert_w1"][l][es].reshape(4, 2048, 16, 128, 2).transpose(0, 1, 2, 4, 3).reshape(4, 2048, 4096)
        b1c = P["expert_b1"][l][es].reshape(4, 16, 128, 2).transpose(2, 0, 1, 3).reshape(128, 4, 32)
        Gl = G[:, es]
        in_maps.append({"h2T": h2T, "Gl": _f32(Gl), "GlT": _f32(Gl.T), "w1": _f32(w1), "b1c": _f32(b1c),
                        "w2": _f32(P["expert_w2"][l][es]), "b2": _f32(P["expert_b2"][l][es])})
    res = _run(nc, in_maps)
    del in_maps
    parts = [res[c]["part"] for c in range(NCORES)]
    del res
    if _KDEBUG:
        _DBG["parts%d" % l] = parts
    nc = _get("p5", build_p5, RL_, RC_, bool(final))
    R = RL_ + RC_
    fw = _f32(P["final_norm_w"][None])
    in_maps = []
    for c in range(NCORES):
        b = c // 4
        in_maps.append({"xn": xn[c], "parts": _f32(np.stack([parts[k][c * R:(c + 1) * R] for k in range(NCORES)])),
                        "g2": _f32(np.stack([mlat[b][G2], mctx[G2]])), "fw": fw})
    res = _run(nc, in_maps)
    x_new = np.stack([np.concatenate([res[4 * b + qd]["xo"][:RL_] for qd in range(4)], 0) for b in range(B_)])
    ctx_new = np.stack([np.concatenate([res[4 * b + qd]["xo"][RL_:] for qd in range(4)], 0) for b in range(B_)])
    return x_new, ctx_new


def kernel(**inputs):
    P = {k: np.asarray(v) for k, v in inputs.items()}
    x = _f32(P["x"])
    ctx = _f32(P["ctx"])
    mod = run_p0(_f32(P["c"]), _f32(P["c_ctx"]), _f32(P["w_mod"]), _f32(P["b_mod"]))
    for l in range(2):
        x, ctx = run_layer(l, x, ctx, mod, P, final=(l == 1))
    return np.ascontiguousarray(x, dtype=np.float32)
```
